# Optimizing a Trainium2 kernel written in Bass

```python
import math
import jax, jax.numpy as jnp
from jax import lax
import numpy as np

D_MODEL = 1024
BATCH = 4
SEQ = 4096
DEPTH = 1

PLE_DIM = 256
RET_HEADS = 8
RET_HEAD_DIM = 64
RET_WIDTH = RET_HEADS * RET_HEAD_DIM
RET_CHUNK = 128
ROPE_BASE = 10000.0
GN_EPS = 1e-6
NA_HEADS = 8
NA_HEAD_DIM = 64
NA_WIDTH = NA_HEADS * NA_HEAD_DIM
GRID_W = 64
NA_WIN_ROWS_MAX = 8
NA_WIN_COLS = 16
MIX_WIDTH = RET_WIDTH + NA_WIDTH
IN_PROJ_WIDTH = 4 * RET_WIDTH + 3 * NA_WIDTH
N_EXPERTS = 64
TOP_K = 8
N_GROUPS = 8
TOPK_GROUPS = 4
EXPERT_DIM = 256
SHARED_DIM = 256
ROUTED_SCALE = 2.5
MOE_BLOCK = 128
LN_EPS = 1e-5

kernel_name = "hybrid_retention_natten_moe_deepnorm"


def _layer_norm(x, g, b):
    xf = x.astype(jnp.float32)
    mu = jnp.mean(xf, -1, keepdims=True)
    var = jnp.mean(jnp.square(xf - mu), -1, keepdims=True)
    return ((xf - mu) * lax.rsqrt(var + LN_EPS) * g.astype(jnp.float32)
            + b.astype(jnp.float32)).astype(x.dtype)


def _rotary(t, pos):
    half = t.shape[-1] // 2
    inv = ROPE_BASE ** (-jnp.arange(half, dtype=jnp.float32) / half)
    ang = pos.astype(jnp.float32)[:, None] * inv[None, :]
    cos = jnp.cos(ang)[None, :, None, :].astype(t.dtype)
    sin = jnp.sin(ang)[None, :, None, :].astype(t.dtype)
    t1, t2 = t[..., :half], t[..., half:]
    return jnp.concatenate([t1 * cos - t2 * sin, t1 * sin + t2 * cos], -1)


def _retention_dir(q, k, v, log_gamma, strict):
    B, H, S, d = q.shape
    C = RET_CHUNK
    NC = S // C
    qc = q.reshape(B, H, NC, C, d)
    kc = k.reshape(B, H, NC, C, d)
    vc = v.reshape(B, H, NC, C, d)
    i = jnp.arange(C, dtype=jnp.float32)
    diff = i[:, None] - i[None, :]
    mask = (diff > 0) if strict else (diff >= 0)
    lg = log_gamma[:, None, None]
    d_intra = jnp.where(mask[None], jnp.exp(jnp.where(mask, diff, 0.0)[None] * lg), 0.0)
    scores = jnp.einsum('bhncd,bhnjd->bhncj', qc, kc) * d_intra[None, :, None]
    inner = jnp.einsum('bhncj,bhnje->bhnce', scores, vc)
    k_decay = jnp.exp((C - 1.0 - i)[None, :] * log_gamma[:, None])
    q_decay = jnp.exp((i + 1.0)[None, :] * log_gamma[:, None])
    chunk_decay = jnp.exp(C * log_gamma)[None, :, None, None]
    s_chunk = jnp.einsum('bhncd,hc,bhnce->nbhde', kc, k_decay, vc)

    def step(state, s_c):
        return chunk_decay * state + s_c, state

    _, prev = lax.scan(step, jnp.zeros_like(s_chunk[0]), s_chunk)
    cross = jnp.einsum('bhncd,hc,nbhde->bhnce', qc, q_decay, prev)
    return (inner + cross).reshape(B, H, S, d)


def _retention_group(rq, rk, rv, rg, pos, dec_f, dec_b, gn_gain):
    B, S, _ = rq.shape
    dt = rq.dtype
    q = _rotary(rq.reshape(B, S, RET_HEADS, RET_HEAD_DIM), pos)
    k = _rotary(rk.reshape(B, S, RET_HEADS, RET_HEAD_DIM), pos) * (RET_HEAD_DIM ** -0.5)
    v = rv.reshape(B, S, RET_HEADS, RET_HEAD_DIM)
    q, k, v = (t.transpose(0, 2, 1, 3).astype(jnp.float32) for t in (q, k, v))
    lg_f = jax.nn.log_sigmoid(dec_f.astype(jnp.float32))
    lg_b = jax.nn.log_sigmoid(dec_b.astype(jnp.float32))
    y_f = _retention_dir(q, k, v, lg_f, False)
    y_b = _retention_dir(q[:, :, ::-1], k[:, :, ::-1], v[:, :, ::-1], lg_b, True)[:, :, ::-1]
    y = y_f + y_b
    mu = jnp.mean(y, -1, keepdims=True)
    var = jnp.mean(jnp.square(y - mu), -1, keepdims=True)
    y = (y - mu) * lax.rsqrt(var + GN_EPS)
    y = y.transpose(0, 2, 1, 3).reshape(B, S, RET_WIDTH) * gn_gain.astype(jnp.float32)
    return jax.nn.silu(rg) * y.astype(dt)


def _neighbour_index(rows):
    wr = min(NA_WIN_ROWS_MAX, rows)
    wc = NA_WIN_COLS
    r = np.arange(rows)
    c = np.arange(GRID_W)
    rs = np.clip(r - wr // 2, 0, rows - wr)
    cs = np.clip(c - wc // 2, 0, GRID_W - wc)
    key_r = rs[:, None] + np.arange(wr)[None, :]
    key_c = cs[:, None] + np.arange(wc)[None, :]
    idx = key_r[:, None, :, None] * GRID_W + key_c[None, :, None, :]
    br = key_r - r[:, None] + (NA_WIN_ROWS_MAX - 1)
    bc = key_c - c[:, None] + (NA_WIN_COLS - 1)
    return (jnp.asarray(idx.reshape(rows, GRID_W, wr * wc), jnp.int32),
            jnp.asarray(br, jnp.int32), bc)


def _neighbourhood_attention(nq, nk, nv, rpb):
    B, S, _ = nq.shape
    H, d = NA_HEADS, NA_HEAD_DIM
    rows = S // GRID_W
    idx, br, bc = _neighbour_index(rows)
    qg = nq.reshape(B, rows, GRID_W, H, d).transpose(1, 0, 3, 2, 4)
    kh = nk.reshape(B, S, H, d).transpose(0, 2, 1, 3)
    vh = nv.reshape(B, S, H, d).transpose(0, 2, 1, 3)
    scale = d ** -0.5

    def row_block(args):
        q_r, idx_r, br_r = args
        k_r = kh[:, :, idx_r]
        v_r = vh[:, :, idx_r]
        bias = rpb[:, br_r][:, :, bc]
        bias = bias.transpose(0, 2, 1, 3).reshape(H, GRID_W, -1)
        s = (jnp.einsum('bhqd,bhqnd->bhqn', q_r, k_r).astype(jnp.float32) * scale
             + bias.astype(jnp.float32)[None])
        a = jax.nn.softmax(s, axis=-1).astype(v_r.dtype)
        return jnp.einsum('bhqn,bhqnd->bhqd', a, v_r)

    out = lax.map(row_block, (qg, idx, br))
    return out.transpose(1, 0, 3, 2, 4).reshape(B, S, NA_WIDTH)


def _hybrid_mixer(x, pos, w_in, dec_f, dec_b, gn_gain, rpb, w_out):
    proj = x @ w_in
    cuts = list(np.cumsum([RET_WIDTH] * 4 + [NA_WIDTH] * 2))
    rq, rk, rv, rg, nq, nk, nv = jnp.split(proj, cuts, axis=-1)
    ret_out = _retention_group(rq, rk, rv, rg, pos, dec_f, dec_b, gn_gain)
    na_out = _neighbourhood_attention(nq, nk, nv, rpb)
    return jnp.concatenate([ret_out, na_out], axis=-1) @ w_out


def _swiglu(h, w_gu, w_down):
    g, u = jnp.split(h @ w_gu, 2, axis=-1)
    return (jax.nn.silu(g) * u) @ w_down


def _route(xt, w_router, router_bias):
    T = xt.shape[0]
    scores = jax.nn.sigmoid(xt.astype(jnp.float32) @ w_router.astype(jnp.float32))
    biased = scores + router_bias.astype(jnp.float32)[None]
    grp = biased.reshape(T, N_GROUPS, N_EXPERTS // N_GROUPS)
    grp_score = lax.top_k(grp, 2)[0].sum(-1)
    _, top_g = lax.top_k(grp_score, TOPK_GROUPS)
    gmask = jnp.any(top_g[..., None] == jnp.arange(N_GROUPS)[None, None, :], axis=1)
    emask = jnp.repeat(gmask, N_EXPERTS // N_GROUPS, axis=-1)
    _, idx = lax.top_k(jnp.where(emask, biased, -jnp.inf), TOP_K)
    w = jnp.take_along_axis(scores, idx, axis=-1)
    w = w / jnp.sum(w, -1, keepdims=True) * ROUTED_SCALE
    return idx, w


def _routed_experts(xt, idx, w, w_gu, w_down):
    T, D = xt.shape
    A = T * TOP_K
    nb = -(-A // MOE_BLOCK) + N_EXPERTS
    n_slots = nb * MOE_BLOCK
    flat_e = idx.reshape(-1).astype(jnp.int32)
    flat_t = jnp.repeat(jnp.arange(T, dtype=jnp.int32), TOP_K)
    flat_w = w.reshape(-1)
    order = jnp.argsort(flat_e)
    e_sorted = flat_e[order]
    counts = jnp.bincount(flat_e, length=N_EXPERTS).astype(jnp.int32)
    start = jnp.cumsum(counts) - counts
    padded = (counts + MOE_BLOCK - 1) // MOE_BLOCK * MOE_BLOCK
    padded_end = jnp.cumsum(padded)
    padded_start = padded_end - padded
    dest = padded_start[e_sorted] + jnp.arange(A, dtype=jnp.int32) - start[e_sorted]
    slot_t = jnp.full((n_slots,), T, jnp.int32).at[dest].set(flat_t[order])
    slot_w = jnp.zeros((n_slots,), jnp.float32).at[dest].set(flat_w[order])
    block_e = jnp.minimum(
        jnp.searchsorted(padded_end, jnp.arange(nb, dtype=jnp.int32) * MOE_BLOCK, side='right'),
        N_EXPERTS - 1)
    x_pad = jnp.concatenate([xt, jnp.zeros((1, D), xt.dtype)], axis=0)

    def run_block(args):
        t_b, e_b = args
        return _swiglu(x_pad[t_b], w_gu[e_b], w_down[e_b])

    y = lax.map(run_block, (slot_t.reshape(nb, MOE_BLOCK), block_e)).reshape(n_slots, D)
    y = y * slot_w[:, None].astype(y.dtype)
    return jax.ops.segment_sum(y, slot_t, num_segments=T + 1)[:T]


def _moe(x, w_router, router_bias, w_e_gu, w_e_down, w_s_gu, w_s_down):
    B, S, D = x.shape
    xt = x.reshape(B * S, D)
    idx, w = _route(xt, w_router, router_bias)
    y = _routed_experts(xt, idx, w, w_e_gu, w_e_down) + _swiglu(xt, w_s_gu, w_s_down)
    return y.reshape(B, S, D)


def setup_inputs(seed: int = 0) -> dict:
    key = jax.random.key(seed)
    ks = jax.random.split(key, 20)
    f32 = jnp.float32
    beta = (8.0 * DEPTH) ** -0.25
    nrm = lambda k, s: jax.random.normal(k, s, f32)
    x = nrm(ks[0], (BATCH, SEQ, D_MODEL))
    p = nrm(ks[1], (DEPTH, BATCH, SEQ, PLE_DIM))
    col_scale = jnp.concatenate([
        jnp.ones((2 * RET_WIDTH,), f32), jnp.full((RET_WIDTH,), beta, f32),
        jnp.ones((RET_WIDTH,), f32), jnp.ones((2 * NA_WIDTH,), f32),
        jnp.full((NA_WIDTH,), beta, f32)])
    w_in = nrm(ks[2], (DEPTH, D_MODEL, IN_PROJ_WIDTH)) * (D_MODEL ** -0.5) * col_scale
    gamma0 = 1.0 - 2.0 ** (-5.0 - np.arange(RET_HEADS))
    logit0 = jnp.asarray(np.log(gamma0 / (1.0 - gamma0)), f32)
    ret_decay_fwd = logit0[None] + 0.01 * nrm(ks[3], (DEPTH, RET_HEADS))
    ret_decay_bwd = logit0[None] + 0.01 * nrm(ks[4], (DEPTH, RET_HEADS))
    ret_gn_gain = 1.0 + 0.02 * nrm(ks[5], (DEPTH, RET_WIDTH))
    na_rpb = 0.02 * nrm(ks[6], (DEPTH, NA_HEADS, 2 * NA_WIN_ROWS_MAX - 1, 2 * NA_WIN_COLS - 1))
    w_out = nrm(ks[7], (DEPTH, MIX_WIDTH, D_MODEL)) * (MIX_WIDTH ** -0.5) * beta
    ln1_gain = 1.0 + 0.02 * nrm(ks[8], (DEPTH, D_MODEL))
    ln1_bias = 0.02 * nrm(ks[9], (DEPTH, D_MODEL))
    w_router = nrm(ks[10], (DEPTH, D_MODEL, N_EXPERTS)) * (D_MODEL ** -0.5)
    router_bias = 0.01 * nrm(ks[11], (DEPTH, N_EXPERTS))
    w_expert_gu = nrm(ks[12], (DEPTH, N_EXPERTS, D_MODEL, 2 * EXPERT_DIM)) * (D_MODEL ** -0.5)
    w_expert_down = nrm(ks[13], (DEPTH, N_EXPERTS, EXPERT_DIM, D_MODEL)) * (EXPERT_DIM ** -0.5) * beta
    w_shared_gu = nrm(ks[14], (DEPTH, D_MODEL, 2 * SHARED_DIM)) * (D_MODEL ** -0.5)
    w_shared_down = nrm(ks[15], (DEPTH, SHARED_DIM, D_MODEL)) * (SHARED_DIM ** -0.5) * beta
    w_ple_proj = nrm(ks[16], (DEPTH, PLE_DIM, D_MODEL)) * (PLE_DIM ** -0.5) * beta
    w_ple_gate = nrm(ks[17], (DEPTH, D_MODEL, D_MODEL)) * (D_MODEL ** -0.5)
    ln2_gain = 1.0 + 0.02 * nrm(ks[18], (DEPTH, D_MODEL))
    ln2_bias = 0.02 * nrm(ks[19], (DEPTH, D_MODEL))
    return {"x": x, "p": p, "w_in": w_in, "ret_decay_fwd": ret_decay_fwd,
            "ret_decay_bwd": ret_decay_bwd, "ret_gn_gain": ret_gn_gain, "na_rpb": na_rpb,
            "w_out": w_out, "ln1_gain": ln1_gain, "ln1_bias": ln1_bias,
            "w_router": w_router, "router_bias": router_bias,
            "w_expert_gu": w_expert_gu, "w_expert_down": w_expert_down,
            "w_shared_gu": w_shared_gu, "w_shared_down": w_shared_down,
            "w_ple_proj": w_ple_proj, "w_ple_gate": w_ple_gate,
            "ln2_gain": ln2_gain, "ln2_bias": ln2_bias}


def reference(x, p, w_in, ret_decay_fwd, ret_decay_bwd, ret_gn_gain, na_rpb, w_out,
              ln1_gain, ln1_bias, w_router, router_bias, w_expert_gu, w_expert_down,
              w_shared_gu, w_shared_down, w_ple_proj, w_ple_gate, ln2_gain, ln2_bias):
    S = x.shape[1]
    alpha = (2.0 * DEPTH) ** 0.25
    pos = jnp.arange(S, dtype=jnp.int32)
    for i in range(DEPTH):
        mix = _hybrid_mixer(x, pos, w_in[i], ret_decay_fwd[i], ret_decay_bwd[i],
                            ret_gn_gain[i], na_rpb[i], w_out[i])
        x = _layer_norm(alpha * x + mix, ln1_gain[i], ln1_bias[i])
        ffn = _moe(x, w_router[i], router_bias[i], w_expert_gu[i], w_expert_down[i],
                   w_shared_gu[i], w_shared_down[i])
        ple = (p[i].astype(x.dtype) @ w_ple_proj[i]) * jax.nn.sigmoid(x @ w_ple_gate[i])
        x = _layer_norm(alpha * x + ffn + ple, ln2_gain[i], ln2_bias[i])
    return x
```

```python
import math
from contextlib import ExitStack

import numpy as np
import concourse.bass as bass
import concourse.mybir as mybir
from concourse.bass_utils import run_bass_kernel_spmd

F32 = mybir.dt.float32
BF16 = mybir.dt.bfloat16
U8 = mybir.dt.uint8
AF = mybir.ActivationFunctionType
ALU = mybir.AluOpType
AX = mybir.AxisListType
DT_SIZE = {F32: 4, BF16: 2, U8: 1}

NCORES = 8
D = 1024
S = 4096
TOK = 2048
NT = 16
NEXP = 64
ALPHA = 2.0 ** 0.25
LN_EPS = 1e-5
GN_EPS = 1e-6
NEG = -30000.0
NA_TYPES = {0: (0, 6, 0), 1: (1, 5, 6), 14: (14, 5, 16), 15: (14, 6, 21)}
for _i in range(2, 14):
    NA_TYPES[_i] = (_i, 5, 11)
NA_BLOCKS = 27


class Tile:
    __slots__ = ("name", "lw", "rd", "dsem", "excl")

    def __init__(self, name):
        self.name = name
        self.excl = False
        self.lw = None
        self.rd = []
        self.dsem = None


class Prog:
    ENGS = ("pe", "act", "dve", "pool", "sp")

    def __init__(self, nc, stack):
        self.nc = nc
        self.stack = stack
        self.streams = {e: [] for e in self.ENGS}
        self.sems = {}
        self.cnt = {}
        for e in self.ENGS:
            self.sems[e] = stack.enter_context(nc.semaphore("s_" + e))
            self.cnt[e] = 0
        self.seen = {e: {} for e in self.ENGS}
        self.ndsem = 0
        self.tiles = []

    def tile(self, name="t"):
        t = Tile(name)
        self.tiles.append(t)
        return t

    def tiles_n(self, n, name="t"):
        return [self.tile("%s%d" % (name, i)) for i in range(n)]

    def _dma_sem(self, t):
        if t.dsem is None:
            key = "d%d" % self.ndsem
            self.ndsem += 1
            self.sems[key] = self.stack.enter_context(self.nc.semaphore("s_" + key))
            self.cnt[key] = 0
            t.dsem = key
        return t.dsem

    def _waits(self, eng, reads, writes):
        need = {}

        def add(ev):
            if ev is None:
                return
            k, v = ev
            if need.get(k, 0) < v:
                need[k] = v
        for t in reads:
            add(t.lw)
            if t.excl:
                for ev in t.rd:
                    if ev[0] != eng:
                        add(ev)
        for t in writes:
            add(t.lw)
            for ev in t.rd:
                add(ev)
        out = []
        for k, v in need.items():
            if k == "pe" and eng == "pe":
                continue
            if self.seen[eng].get(k, 0) >= v:
                continue
            self.seen[eng][k] = v
            out.append((k, v))
        return out

    def op(self, eng, fn, reads=(), writes=()):
        waits = self._waits(eng, reads, writes)
        self.cnt[eng] += 1
        ev = (eng, self.cnt[eng])
        sems = self.sems

        def emit(e, waits=waits, fn=fn, semk=eng):
            for k, v in waits:
                e.wait_ge(sems[k], v)
            ins = fn(e)
            ins.then_inc(sems[semk], 1)
        self.streams[eng].append(emit)
        for t in reads:
            t.rd.append(ev)
        for t in writes:
            t.lw = ev
            t.rd = []
        return ev

    def dma(self, q, fn, reads=(), writes=(), sem_tile=None):
        st = sem_tile or (writes[0] if writes else reads[0])
        key = self._dma_sem(st)
        waits = self._waits(q, reads, writes)
        if self.cnt[key] > 0 and self.seen[q].get(key, 0) < self.cnt[key]:
            self.seen[q][key] = self.cnt[key]
            waits.append((key, self.cnt[key]))
        self.cnt[key] += 16
        ev = (key, self.cnt[key])
        sems = self.sems

        def emit(e, waits=waits, fn=fn, key=key):
            for k, v in waits:
                e.wait_ge(sems[k], v)
            ins = fn(e)
            ins.then_inc(sems[key], 16)
        self.streams[q].append(emit)
        for t in reads:
            t.rd.append(ev)
        for t in writes:
            t.lw = ev
            t.rd = []
        return ev

    def barrier(self):
        snap = {k: v for k, v in self.cnt.items() if v > 0}
        sems = self.sems
        for eng in self.ENGS:
            waits = []
            for k, v in snap.items():
                if k == eng:
                    continue
                if self.seen[eng].get(k, 0) >= v:
                    continue
                self.seen[eng][k] = v
                waits.append((k, v))

            def emit(e, waits=waits):
                for k, v in waits:
                    e.wait_ge(sems[k], v)
            self.streams[eng].append(emit)
        for t in self.tiles:
            t.rd = []

    def final_wait(self, eng="sp"):
        snap = {k: v for k, v in self.cnt.items() if v > 0}
        sems = self.sems

        def emit(e):
            for k, v in snap.items():
                if k == eng:
                    continue
                e.wait_ge(sems[k], v)
        self.streams[eng].append(emit)

    def emit_all(self):
        nc = self.nc
        streams = self.streams
        with nc.Block() as block:
            @block.tensor
            def _(e):
                for f in streams["pe"]:
                    f(e)

            @block.scalar
            def _(e):
                for f in streams["act"]:
                    f(e)

            @block.vector
            def _(e):
                for f in streams["dve"]:
                    f(e)

            @block.gpsimd
            def _(e):
                for f in streams["pool"]:
                    f(e)

            @block.sync
            def _(e):
                for f in streams["sp"]:
                    f(e)


class Arena:
    def __init__(self, nc, stack, nbytes):
        self.t = stack.enter_context(nc.sbuf_tensor("arena", [128, nbytes], U8))
        self.n = nbytes
        self.off = 0
        self.marks = []
        self.peak = 0

    def mark(self):
        self.marks.append(self.off)

    def release(self):
        self.off = self.marks.pop()

    def alloc(self, free_shape, dtype):
        n = int(np.prod(free_shape)) * DT_SIZE[dtype]
        n_al = (n + 63) // 64 * 64
        assert self.off + n_al <= self.n, ("SBUF arena overflow", self.off, n_al, self.n)
        ap = self.t[:, self.off:self.off + n].bitcast(dtype)
        self.off += n_al
        self.peak = max(self.peak, self.off)
        if len(free_shape) == 2:
            ap = ap.rearrange("p (a b) -> p a b", b=free_shape[1])
        elif len(free_shape) == 3:
            ap = ap.rearrange("p (a b c) -> p a b c", b=free_shape[1], c=free_shape[2])
        return ap


def bc_mid(ap2, n):
    p, f = ap2.shape
    return ap2.unsqueeze(1).to_broadcast([p, n, f])


def bc_last(ap2, n):
    p, a = ap2.shape
    return ap2.unsqueeze(2).to_broadcast([p, a, n])


def build_program(stage="full"):
    nc = bass.Bass("TRN2", target_bir_lowering=False)

    def din(name, shape):
        return nc.dram_tensor(name, list(shape), F32, kind="ExternalInput").ap()

    xT = din("xT", [D, TOK]); xo = din("xo", [D, TOK]); xh = din("xh", [D, 512])
    xr = din("xr", [TOK, D]); pT = din("pT", [256, TOK])
    w_in = din("w_in", [D, 3584]); w_out = din("w_out", [D, D]); w_router = din("w_router", [D, NEXP])
    w_egu = din("w_egu", [NEXP, D, 512]); w_edn = din("w_edn", [NEXP, 256, D])
    w_sgu = din("w_sgu", [D, 512]); w_sdn = din("w_sdn", [256, D])
    w_pp = din("w_pp", [256, D]); w_pg = din("w_pg", [D, D])
    cc_own = din("cc_own", [128, NT * 64]); ss_own = din("ss_own", [128, NT * 64])
    cc_oth = din("cc_oth", [128, NT * 64]); ss_oth = din("ss_oth", [128, NT * 64])
    dist = din("dist", [128, NT])
    dec_f = din("dec_f", [1, 8]); dec_b = din("dec_b", [1, 8])
    decfT = din("decfT", [2, 4]); decbT = din("decbT", [2, 4]); flag = din("flag", [1, 1])
    epos = din("epos", [128, 128]); eneg = din("eneg", [128, 128])
    iota1 = din("iota1", [128, 128]); iota2 = din("iota2", [128, 128])
    c127 = din("c127", [128, 1]); cj = din("cj", [128, 1])
    gn_gain = din("gn_gain", [1, 512]); natab = din("natab", [8, 128, NA_BLOCKS * 128])
    ln1_g = din("ln1_g", [1, D]); ln1_b = din("ln1_b", [1, D])
    ln2_g = din("ln2_g", [1, D]); ln2_b = din("ln2_b", [1, D]); rbias = din("rbias", [1, NEXP])
    out = nc.dram_tensor("out", [TOK, D], F32, kind="ExternalOutput").ap()
    GT = nc.dram_tensor("GT", [NEXP, TOK], F32, kind="Internal").ap()
    base = nc.dram_tensor("base", [TOK, D], F32, kind="Internal").ap()
    cat = nc.dram_tensor("cat", [TOK, D], BF16, kind="Internal").ap()
    dbg = {}
    if stage != "full":
        dbg["ret"] = nc.dram_tensor("dbg_ret", [TOK, 512], F32, kind="ExternalOutput").ap()
        dbg["na"] = nc.dram_tensor("dbg_na", [TOK, 512], F32, kind="ExternalOutput").ap()
        dbg["x1"] = nc.dram_tensor("dbg_x1", [TOK, D], F32, kind="ExternalOutput").ap()
        dbg["gate"] = nc.dram_tensor("dbg_gate", [TOK, NEXP], F32, kind="ExternalOutput").ap()
        dbg["misc"] = nc.dram_tensor("dbg_misc", [128, 64], F32, kind="ExternalOutput").ap()

    with ExitStack() as st:
        P = Prog(nc, st)
        A = Arena(nc, st, 206 * 1024)
        PS = [st.enter_context(nc.psum_tensor("ps%d" % i, [128, 512], F32))[:, :] for i in range(8)]
        PSB = [p.bitcast(BF16) for p in PS]
        t_ps = P.tiles_n(8, "ps")
        for t_ in t_ps:
            t_.excl = True

        ident_f = A.alloc([128], F32); ident_b = A.alloc([128], BF16)
        t_ident = P.tile("ident")

        P.op("pool", lambda e: e.memset(ident_f, 0.0), writes=[t_ident])
        P.op("pool", lambda e: e.affine_select(out=ident_f, in_=ident_f, pattern=[[-1, 128]], compare_op=ALU.not_equal,
                                               fill=1.0, base=0, channel_multiplier=1), reads=[t_ident], writes=[t_ident])
        P.op("dve", lambda e: e.tensor_copy(out=ident_b, in_=ident_f), reads=[t_ident], writes=[t_ident])

        cst = A.alloc([8], F32)
        t_cst = P.tile("cst")

        for (p0, p1, c0, c1, val) in ((0, 128, 0, 1, math.log(0.125)), (0, 128, 1, 2, 1.0), (0, 128, 2, 3, 0.0),
                                      (0, 128, 3, 4, GN_EPS), (0, 128, 4, 5, LN_EPS), (0, 64, 5, 6, 1.0),
                                      (64, 128, 5, 6, 0.0), (0, 64, 6, 7, 0.0), (64, 128, 6, 7, 1.0)):
            P.op("pool", lambda e, p0=p0, p1=p1, c0=c0, c1=c1, val=val: e.memset(cst[p0:p1, c0:c1], val), writes=[t_cst])

        t_cat = [P.tiles_n(NT, "catR"), P.tiles_n(NT, "catN")]

        A.mark()
        w_ret = A.alloc([8, 2048], BF16)
        t_wret = P.tiles_n(4, "wret")
        for g in range(4):
            P.dma("pool", lambda e, g=g: e.dma_start(
                out=w_ret[:, :, g * 512:(g + 1) * 512],
                in_=w_in[:, g * 512:(g + 1) * 512].rearrange("(kc k) n -> k kc n", k=128)), writes=[t_wret[g]])
        ccX = A.alloc([NT, 64], F32); ssX = A.alloc([NT, 64], F32)
        ccO, ssO = ccX, ssX
        t_rotl = P.tiles_n(2, "rot")

        def load_rot(c_src, s_src):
            for i_, (dst, src) in enumerate(((ccX, c_src), (ssX, s_src))):
                P.dma("sp", lambda e, dst=dst, src=src: e.dma_start(out=dst.rearrange("p a b -> p (a b)"), in_=src),
                      writes=[t_rotl[i_]])
        load_rot(cc_oth, ss_oth)
        small = A.alloc([256], F32)
        t_small = P.tile("small")
        sm = lambda a, b: small[:, a:b]
        lds = [(sm(0, 8), dec_f.partition_broadcast(128)), (sm(8, 16), dec_b.partition_broadcast(128)),
               (small[0:64, 16:20], decfT[0:1, :].partition_broadcast(64)),
               (small[64:128, 16:20], decfT[1:2, :].partition_broadcast(64)),
               (small[0:64, 20:24], decbT[0:1, :].partition_broadcast(64)),
               (small[64:128, 20:24], decbT[1:2, :].partition_broadcast(64)),
               (sm(24, 25), flag.partition_broadcast(128)), (sm(96, 97), c127), (sm(97, 98), cj),
               (sm(100, 116), dist)]
        t_smld = P.tiles_n(len(lds), "smld")
        for i_, (dst, src) in enumerate(lds):
            P.dma("sp", lambda e, dst=dst, src=src: e.dma_start(out=dst, in_=src), writes=[t_smld[i_]])
        eposT = A.alloc([128], F32); enegT = A.alloc([128], F32)
        io1 = A.alloc([128], F32); io2 = A.alloc([128], F32)
        gnb = A.alloc([512], F32)
        t_tabl = P.tiles_n(5, "tabl")
        t_gc = P.tile("gc")
        for i_, (dst, src) in enumerate(((eposT, epos), (enegT, eneg), (io1, iota1), (io2, iota2),
                                         (gnb, gn_gain.partition_broadcast(128)))):
            P.dma("sp", lambda e, dst=dst, src=src: e.dma_start(out=dst, in_=src), writes=[t_tabl[i_]])

        P.op("act", lambda e: e.activation(out=sm(32, 56), in_=sm(0, 24), func=AF.Exp, scale=-1.0),
             reads=t_smld, writes=[t_small])
        P.op("act", lambda e: e.activation(out=sm(32, 56), in_=sm(32, 56), func=AF.Ln, bias=cst[:, 1:2], scale=1.0),
             reads=[t_small, t_cst], writes=[t_small])
        P.op("dve", lambda e: e.tensor_scalar(out=sm(32, 56), in0=sm(32, 56), scalar1=-1.0, scalar2=None, op0=ALU.mult),
             reads=[t_small], writes=[t_small])
        P.op("dve", lambda e: e.tensor_scalar(out=sm(25, 26), in0=sm(24, 25), scalar1=-1.0, scalar2=1.0,
                                              op0=ALU.mult, op1=ALU.add), reads=[t_small], writes=[t_small])
        P.op("dve", lambda e: e.tensor_scalar(out=sm(56, 64), in0=sm(32, 40), scalar1=sm(24, 25), scalar2=None,
                                              op0=ALU.mult), reads=[t_small], writes=[t_small])
        P.op("dve", lambda e: e.scalar_tensor_tensor(out=sm(56, 64), in0=sm(40, 48), scalar=sm(25, 26), in1=sm(56, 64),
                                                     op0=ALU.mult, op1=ALU.add), reads=[t_small], writes=[t_small])
        P.op("act", lambda e: e.activation(out=sm(64, 72), in_=sm(32, 40), func=AF.Exp, scale=sm(96, 97)),
             reads=[t_small], writes=[t_small])
        P.op("act", lambda e: e.activation(out=sm(72, 80), in_=sm(40, 48), func=AF.Exp, scale=sm(97, 98)),
             reads=[t_small], writes=[t_small])
        P.op("act", lambda e: e.activation(out=sm(80, 88), in_=sm(48, 56), func=AF.Exp, scale=128.0),
             reads=[t_small], writes=[t_small])
        gcfB = A.alloc([4, 64], F32); gcbB = A.alloc([4, 64], F32)
        P.op("dve", lambda e: e.tensor_copy(out=gcfB, in_=bc_last(sm(80, 84), 64)), reads=[t_small], writes=[t_gc])
        P.op("dve", lambda e: e.tensor_copy(out=gcbB, in_=bc_last(sm(84, 88), 64)), reads=[t_small], writes=[t_gc])
        Mmask = A.alloc([8, 128], BF16)
        mtmp = A.alloc([128], F32)
        t_M = P.tile("M"); t_mtmp = P.tile("mtmp")
        for h in range(8):
            P.op("dve", lambda e, h=h: e.tensor_scalar(out=mtmp, in0=eposT, scalar1=sm(32 + h, 33 + h), scalar2=None,
                                                       op0=ALU.mult), reads=[t_small] + t_tabl, writes=[t_mtmp])
            P.op("dve", lambda e, h=h: e.scalar_tensor_tensor(out=mtmp, in0=enegT, scalar=sm(40 + h, 41 + h), in1=mtmp,
                                                              op0=ALU.mult, op1=ALU.add),
                 reads=[t_small, t_mtmp] + t_tabl, writes=[t_mtmp])
            P.op("act", lambda e, h=h: e.activation(out=Mmask[:, h, :], in_=mtmp, func=AF.Exp, bias=cst[:, 0:1], scale=1.0),
                 reads=[t_mtmp, t_cst], writes=[t_M])
        Df = A.alloc([4, 128], BF16); Db = A.alloc([4, 128], BF16)
        t_D = P.tile("D")
        for pr in range(4):
            P.op("act", lambda e, pr=pr: e.activation(out=Df[:, pr, :], in_=io1, func=AF.Exp, bias=cst[:, 0:1],
                                                      scale=sm(48 + pr, 49 + pr)), reads=[t_small, t_cst] + t_tabl, writes=[t_D])
            P.op("act", lambda e, pr=pr: e.activation(out=Db[:, pr, :], in_=io2, func=AF.Exp, bias=cst[:, 0:1],
                                                      scale=sm(52 + pr, 53 + pr)), reads=[t_small, t_cst] + t_tabl, writes=[t_D])
        if "misc" in dbg:
            P.dma("sp", lambda e: e.dma_start(out=dbg["misc"][:, 0:56], in_=sm(32, 88)), reads=[t_small], sem_tile=P.tile("dm"))

        if stage == "ret0":
            P.final_wait("sp")
            P.emit_all()
            return nc
        qT = A.alloc([4, TOK], BF16); kT = A.alloc([4, TOK], BF16)
        vS = A.alloc([NT, 512], BF16); GS = A.alloc([NT, 512], BF16)
        SfB = A.alloc([NT, 256], BF16); SbB = A.alloc([NT, 256], BF16)
        UbS = A.alloc([NT, 256], BF16)
        Sst = A.alloc([2, 256], F32)
        t_qT = P.tiles_n(NT, "qT"); t_kT = P.tiles_n(NT, "kT"); t_v = P.tiles_n(NT, "v"); t_G = P.tiles_n(NT, "G")
        t_SfB = P.tiles_n(NT, "SfB"); t_SbB = P.tiles_n(NT, "SbB"); t_Ub = P.tiles_n(NT, "Ub")
        t_Sf = P.tile("Sf"); t_Sb = P.tile("Sb")
        A.mark()
        xs = [A.alloc([8, 256], BF16) for _ in range(2)]; t_xs = P.tiles_n(2, "xs")
        q_tm = [A.alloc([512], BF16) for _ in range(2)]; k_tm = [A.alloc([512], BF16) for _ in range(2)]
        t_qtm = P.tiles_n(2, "qtm"); t_ktm = P.tiles_n(2, "ktm")
        rA = [A.alloc([512], F32) for _ in range(2)]; rB = [A.alloc([512], F32) for _ in range(2)]
        t_rA = P.tiles_n(2, "rA"); t_rB = P.tiles_n(2, "rB")
        vfb = [A.alloc([4, 256], BF16) for _ in range(2)]; t_vfb = P.tiles_n(2, "vfb")
        wo = [A.alloc([8], F32) for _ in range(2)]; t_wo = P.tiles_n(2, "wo")
        Ud = [A.alloc([4, 2, 64], F32) for _ in range(2)]; t_Ud = P.tiles_n(2, "Ud")
        Uf = [A.alloc([2, 512], F32) for _ in range(2)]; t_Uf = P.tiles_n(2, "Uf")

        def v3(ap):
            return ap.rearrange("p (h d) -> p h d", d=64)

        def inproj(ps_i, xs_ap, wcols, reads):
            def f(e):
                ins = None
                for kc in range(8):
                    ins = e.matmul(PS[ps_i], lhsT=xs_ap(kc), rhs=w_ret[:, kc, wcols * 512:(wcols + 1) * 512],
                                   start=(kc == 0), stop=(kc == 7))
                return ins
            P.op("pe", f, reads=reads, writes=[t_ps[ps_i]])

        def rotary(ps_i, cc, ss, t, dst, t_dst, par):
            src = v3(PS[ps_i])
            a3 = v3(rA[par]); b3 = v3(rB[par])
            P.op("dve", lambda e: e.tensor_tensor(out=a3, in0=src, in1=bc_mid(cc[:, t, :], 8), op=ALU.mult),
                 reads=[t_ps[ps_i]] + t_rotl, writes=[t_rA[par]])
            P.op("dve", lambda e: e.tensor_tensor(out=b3[:, :, 0:32], in0=src[:, :, 32:64],
                                                  in1=bc_mid(ss[:, t, 0:32], 8), op=ALU.mult),
                 reads=[t_ps[ps_i]] + t_rotl, writes=[t_rB[par]])
            P.op("dve", lambda e: e.tensor_tensor(out=b3[:, :, 32:64], in0=src[:, :, 0:32],
                                                  in1=bc_mid(ss[:, t, 32:64], 8), op=ALU.mult),
                 reads=[t_ps[ps_i]] + t_rotl, writes=[t_rB[par]])
            P.op("pool", lambda e: e.tensor_tensor(out=dst, in0=rA[par], in1=rB[par], op=ALU.add),
                 reads=[t_rA[par], t_rB[par]], writes=[t_dst])

        sinB = [PS[4 + pr][:, 0:128] for pr in range(4)]
        for t in range(NT):
            ch, sub = divmod(t, 2)
            par = t % 2
            if sub == 0:
                P.dma("pool", lambda e, ch=ch: e.dma_start(
                    out=xs[ch % 2], in_=xo[:, ch * 256:(ch + 1) * 256].rearrange("(kc k) n -> k kc n", k=128)),
                    writes=[t_xs[ch % 2]])
            xs_ap = lambda kc, ch=ch, sub=sub: xs[ch % 2][:, kc, sub * 128:(sub + 1) * 128]
            inproj(0, xs_ap, 1, [t_xs[ch % 2], t_wret[1]])
            inproj(1, xs_ap, 2, [t_xs[ch % 2], t_wret[2]])
            rotary(0, ccX, ssX, t, k_tm[par], t_ktm[par], par)
            P.op("act", lambda e, t=t, par=par: e.activation(out=wo[par], in_=sm(56, 64), func=AF.Exp,
                                                             scale=sm(100 + t, 101 + t)),
                 reads=[t_small], writes=[t_wo[par]])
            P.op("dve", lambda e, par=par: e.tensor_tensor(
                out=vfb[par].rearrange("p a b -> p (a b)")[:, 0:512].rearrange("p (h d) -> p h d", d=64),
                in0=v3(PS[1]), in1=bc_last(wo[par], 64), op=ALU.mult),
                reads=[t_ps[1], t_wo[par]], writes=[t_vfb[par]])

            def fU(e, t=t, par=par):
                ins = None
                vw = vfb[par].rearrange("p a b -> p (a b)")
                for pr in range(4):
                    ins = e.matmul(sinB[pr], lhsT=k_tm[par][:, pr * 128:(pr + 1) * 128],
                                   rhs=vw[:, pr * 128:(pr + 1) * 128], start=(t == 0), stop=(t == NT - 1))
                return ins
            P.op("pe", fU, reads=[t_ktm[par], t_vfb[par]], writes=t_ps[4:8])
        S3 = Sst.rearrange("p a (b c) -> p a b c", c=64)
        for hp in range(2):
            pl, ph = hp * 64, hp * 64 + 64
            for pr in range(4):
                P.op("dve", lambda e, pl=pl, ph=ph, pr=pr: e.tensor_scalar(
                    out=S3[pl:ph, 0, pr, :], in0=sinB[pr][pl:ph, pl:ph], scalar1=small[pl:ph, 24:25], scalar2=None, op0=ALU.mult),
                    reads=[t_ps[4 + pr], t_small], writes=[t_Sf])
                P.op("dve", lambda e, pl=pl, ph=ph, pr=pr: e.tensor_scalar(
                    out=S3[pl:ph, 1, pr, :], in0=sinB[pr][pl:ph, pl:ph], scalar1=small[pl:ph, 25:26], scalar2=None, op0=ALU.mult),
                    reads=[t_ps[4 + pr], t_small], writes=[t_Sb])

        if stage == "ret1":
            P.final_wait("sp")
            P.emit_all()
            return nc
        load_rot(cc_own, ss_own)
        for t in range(NT):
            ch, sub = divmod(t, 2)
            par = t % 2
            if sub == 0:
                P.dma("pool", lambda e, ch=ch: e.dma_start(
                    out=xs[ch % 2], in_=xT[:, ch * 256:(ch + 1) * 256].rearrange("(kc k) n -> k kc n", k=128)),
                    writes=[t_xs[ch % 2]])
            xs_ap = lambda kc, ch=ch, sub=sub: xs[ch % 2][:, kc, sub * 128:(sub + 1) * 128]
            for g in range(4):
                inproj(g, xs_ap, g, [t_xs[ch % 2], t_wret[g]])
            rotary(0, ccO, ssO, t, q_tm[par], t_qtm[par], par)
            rotary(1, ccO, ssO, t, k_tm[par], t_ktm[par], par)
            P.op("act", lambda e, t=t: e.activation(func=AF.Copy, out=vS[:, t, :], in_=PS[2]), reads=[t_ps[2]], writes=[t_v[t]])
            vfl = vfb[par].rearrange("p a b -> p (a b)")
            for dr, col in ((0, 64), (1, 72)):
                P.op("dve", lambda e, dr=dr, col=col, vfl=vfl: e.tensor_tensor(
                    out=v3(vfl[:, dr * 512:(dr + 1) * 512]), in0=v3(PS[2]), in1=bc_last(small[:, col:col + 8], 64),
                    op=ALU.mult), reads=[t_ps[2], t_small], writes=[t_vfb[par]])
            P.op("act", lambda e, t=t: e.activation(out=GS[:, t, :], in_=PS[3], func=AF.Silu),
                 reads=[t_ps[3]], writes=[t_G[t]])
            P.op("pool", lambda e, t=t: e.tensor_tensor(out=GS[:, t, :], in0=GS[:, t, :], in1=gnb, op=ALU.mult),
                 reads=[t_G[t]] + t_tabl, writes=[t_G[t]])

            def fT(e, par=par):
                ins = None
                for pr in range(4):
                    ins = e.transpose(out=PSB[4][:, pr * 128:(pr + 1) * 128], in_=q_tm[par][:, pr * 128:(pr + 1) * 128],
                                      identity=ident_b)
                for pr in range(4):
                    ins = e.transpose(out=PSB[7][:, pr * 128:(pr + 1) * 128],
                                      in_=k_tm[par][:, pr * 128:(pr + 1) * 128], identity=ident_b)
                return ins
            P.op("pe", fT, reads=[t_qtm[par], t_ktm[par], t_ident], writes=[t_ps[4], t_ps[7]])
            P.op("dve", lambda e, t=t: e.tensor_copy(out=qT[:, :, t * 128:(t + 1) * 128],
                                                     in_=PSB[4][:, 0:512].rearrange("p (a b) -> p a b", b=128)),
                 reads=[t_ps[4]], writes=[t_qT[t]])
            P.op("dve", lambda e, t=t: e.tensor_copy(out=kT[:, :, t * 128:(t + 1) * 128],
                                                     in_=PSB[7][:, 0:512].rearrange("p (a b) -> p a b", b=128)),
                 reads=[t_ps[7]], writes=[t_kT[t]])

            def fU2(e, par=par):
                ins = None
                vfl = vfb[par].rearrange("p a b -> p (a b)")
                for pr in range(4):
                    bank = PS[5 + pr // 2]
                    for dr in range(2):
                        c0 = (pr % 2) * 256 + dr * 128
                        ins = e.matmul(bank[:, c0:c0 + 128], lhsT=k_tm[par][:, pr * 128:(pr + 1) * 128],
                                       rhs=vfl[:, dr * 512 + pr * 128:dr * 512 + (pr + 1) * 128], start=True, stop=True)
                return ins
            P.op("pe", fU2, reads=[t_ktm[par], t_vfb[par]], writes=[t_ps[5], t_ps[6]])
            P.op("act", lambda e, t=t: e.activation(func=AF.Copy, out=SfB[:, t, :], in_=Sst[:, 0, :]), reads=[t_Sf], writes=[t_SfB[t]])
            P.op("dve", lambda e: e.tensor_tensor(out=Sst[:, 0, :], in0=Sst[:, 0, :],
                                                  in1=gcfB.rearrange("p a b -> p (a b)"), op=ALU.mult),
                 reads=[t_Sf, t_gc], writes=[t_Sf])
            for bk in range(2):
                P.op("dve", lambda e, bk=bk, par=par: e.tensor_copy(out=Uf[par][:, bk, :], in_=PS[5 + bk]),
                     reads=[t_ps[5 + bk]], writes=[t_Uf[par]])
                Ub4 = Uf[par][:, bk, :].rearrange("p (a s c) -> p a s c", s=2, c=128)
                ud = Ud[par][:, 2 * bk:2 * bk + 2, :, :]
                for a_ in range(2):
                    P.op("dve", lambda e, Ub4=Ub4, ud=ud, a_=a_: e.tensor_scalar(
                        out=ud[:, a_], in0=Ub4[:, a_, :, 64:128], scalar1=cst[:, 6:7], scalar2=None, op0=ALU.mult),
                        reads=[t_Uf[par], t_cst], writes=[t_Ud[par]])
                    P.op("dve", lambda e, Ub4=Ub4, ud=ud, a_=a_: e.scalar_tensor_tensor(
                        out=ud[:, a_], in0=Ub4[:, a_, :, 0:64], scalar=cst[:, 5:6], in1=ud[:, a_], op0=ALU.mult, op1=ALU.add),
                        reads=[t_Uf[par], t_cst, t_Ud[par]], writes=[t_Ud[par]])
            P.op("dve", lambda e, par=par: e.tensor_tensor(out=S3[:, 0], in0=S3[:, 0], in1=Ud[par][:, :, 0, :], op=ALU.add),
                 reads=[t_Sf, t_Ud[par]], writes=[t_Sf])
            P.op("pool", lambda e, par=par, t=t: e.tensor_copy(out=UbS[:, t, :].rearrange("p (a c) -> p a c", c=64),
                                                               in_=Ud[par][:, :, 1, :]),
                 reads=[t_Ud[par]], writes=[t_Ub[t]])
        for t in range(NT - 1, -1, -1):
            P.op("act", lambda e, t=t: e.activation(func=AF.Copy, out=SbB[:, t, :], in_=Sst[:, 1, :]), reads=[t_Sb], writes=[t_SbB[t]])
            if t > 0:
                P.op("dve", lambda e: e.tensor_tensor(out=Sst[:, 1, :], in0=Sst[:, 1, :],
                                                      in1=gcbB.rearrange("p a b -> p (a b)"), op=ALU.mult),
                     reads=[t_Sb, t_gc], writes=[t_Sb])
                P.op("dve", lambda e, t=t: e.tensor_tensor(out=Sst[:, 1, :], in0=Sst[:, 1, :], in1=UbS[:, t, :], op=ALU.add),
                     reads=[t_Sb, t_Ub[t]], writes=[t_Sb])

        P.barrier()
        A.release()
        if stage == "ret2":
            P.final_wait("sp")
            P.emit_all()
            return nc
        sT_sb = [A.alloc([8, 128], BF16) for _ in range(2)]; t_sT = P.tiles_n(2, "sT")
        qfT = [A.alloc([4, 128], BF16) for _ in range(2)]; qbT = [A.alloc([4, 128], BF16) for _ in range(2)]
        t_qf = P.tiles_n(2, "qf"); t_qb = P.tiles_n(2, "qb")
        sq = [A.alloc([512], F32) for _ in range(2)]; t_sq = P.tiles_n(2, "sq")
        gst = [A.alloc([64], F32) for _ in range(2)]; t_gst = P.tiles_n(2, "gst")
        yc = [A.alloc([512], F32) for _ in range(2)]; t_yc = P.tiles_n(2, "yc")
        retb = [A.alloc([512], BF16) for _ in range(2)]; t_retb = P.tiles_n(2, "retb")
        retf = [A.alloc([512], F32) for _ in range(2)] if "ret" in dbg else None
        ret_pending = []
        for t in range(NT):
            par = t % 2
            b0 = 4 * par
            tsl = slice(t * 128, (t + 1) * 128)

            def fS(e, b0=b0, tsl=tsl):
                ins = None
                for h in range(8):
                    pr, hp = divmod(h, 2)
                    pl, ph = hp * 64, hp * 64 + 64
                    ins = e.matmul(PS[b0 + hp][:, pr * 128:pr * 128 + 128], lhsT=kT[pl:ph, pr, tsl],
                                   rhs=qT[pl:ph, pr, tsl], start=True, stop=True)
                return ins
            P.op("pe", fS, reads=[t_kT[t], t_qT[t]], writes=[t_ps[b0], t_ps[b0 + 1]])
            for bk in range(2):
                P.op("dve", lambda e, bk=bk, b0=b0, par=par: e.tensor_tensor(
                    out=sT_sb[par].rearrange("p (a s) b -> p a s b", s=2)[:, :, bk, :],
                    in0=PS[b0 + bk].rearrange("p (a b) -> p a b", b=128),
                    in1=Mmask.rearrange("p (a s) b -> p a s b", s=2)[:, :, bk, :], op=ALU.mult),
                    reads=[t_ps[b0 + bk], t_M], writes=[t_sT[par]])
            P.op("pool", lambda e, par=par, tsl=tsl: e.tensor_tensor(out=qfT[par], in0=qT[:, :, tsl], in1=Df, op=ALU.mult),
                 reads=[t_qT[t], t_D], writes=[t_qf[par]])
            P.op("pool", lambda e, par=par, tsl=tsl: e.tensor_tensor(out=qbT[par], in0=qT[:, :, tsl], in1=Db, op=ALU.mult),
                 reads=[t_qT[t], t_D], writes=[t_qb[par]])

            def ret_tail(b0=b0, par=par, t=t, tsl=tsl):
                def fY(e, b0=b0, par=par, t=t):
                    ins = None
                    for h in range(8):
                        pr, hp = divmod(h, 2)
                        pl, ph = hp * 64, hp * 64 + 64
                        o = PS[b0 + 2][:, h * 64:(h + 1) * 64]
                        e.matmul(o, lhsT=sT_sb[par][:, h, :], rhs=vS[:, t, h * 64:(h + 1) * 64], start=True, stop=False)
                        e.matmul(o, lhsT=qfT[par][pl:ph, pr, :], rhs=SfB[pl:ph, t, pr * 64:(pr + 1) * 64], start=False, stop=False)
                        ins = e.matmul(o, lhsT=qbT[par][pl:ph, pr, :], rhs=SbB[pl:ph, t, pr * 64:(pr + 1) * 64], start=False, stop=True)
                    return ins
                P.op("pe", fY, reads=[t_sT[par], t_v[t], t_qf[par], t_qb[par], t_SfB[t], t_SbB[t]], writes=[t_ps[b0 + 2]])
                y3 = v3(PS[b0 + 2])
                g = gst[par]
                P.op("dve", lambda e, y3=y3, g=g: e.tensor_reduce(out=g[:, 0:8], in_=y3, axis=AX.X, op=ALU.add),
                     reads=[t_ps[b0 + 2]], writes=[t_gst[par]])
                P.op("act", lambda e, b0=b0, par=par: e.activation(out=sq[par], in_=PS[b0 + 2], func=AF.Square),
                     reads=[t_ps[b0 + 2]], writes=[t_sq[par]])
                P.op("dve", lambda e, g=g, par=par: e.tensor_reduce(out=g[:, 8:16], in_=v3(sq[par]), axis=AX.X, op=ALU.add),
                     reads=[t_sq[par]], writes=[t_gst[par]])
                P.op("dve", lambda e, g=g: e.tensor_scalar(out=g[:, 16:24], in0=g[:, 0:8], scalar1=1.0 / 64, scalar2=None,
                                                           op0=ALU.mult), reads=[t_gst[par]], writes=[t_gst[par]])
                P.op("dve", lambda e, g=g: e.tensor_tensor(out=g[:, 24:32], in0=g[:, 16:24], in1=g[:, 16:24], op=ALU.mult),
                     reads=[t_gst[par]], writes=[t_gst[par]])
                P.op("dve", lambda e, g=g: e.scalar_tensor_tensor(out=g[:, 32:40], in0=g[:, 8:16], scalar=1.0 / 64,
                                                                  in1=g[:, 24:32], op0=ALU.mult, op1=ALU.subtract),
                     reads=[t_gst[par]], writes=[t_gst[par]])
                P.op("act", lambda e, g=g: e.activation(out=g[:, 40:48], in_=g[:, 32:40], func=AF.Ln, bias=cst[:, 3:4], scale=1.0),
                     reads=[t_gst[par], t_cst], writes=[t_gst[par]])
                P.op("act", lambda e, g=g: e.activation(out=g[:, 40:48], in_=g[:, 40:48], func=AF.Exp, scale=-0.5),
                     reads=[t_gst[par]], writes=[t_gst[par]])
                P.op("dve", lambda e, g=g, y3=y3, par=par: e.tensor_tensor(out=v3(yc[par]), in0=y3, in1=bc_last(g[:, 16:24], 64),
                                                                           op=ALU.subtract),
                     reads=[t_ps[b0 + 2], t_gst[par]], writes=[t_yc[par]])
                P.op("pool", lambda e, g=g, par=par: e.tensor_tensor(out=v3(yc[par]), in0=v3(yc[par]), in1=bc_last(g[:, 40:48], 64),
                                                                     op=ALU.mult), reads=[t_yc[par], t_gst[par]], writes=[t_yc[par]])
                P.op("pool", lambda e, par=par, t=t: e.tensor_tensor(out=retb[par], in0=yc[par], in1=GS[:, t, :], op=ALU.mult),
                     reads=[t_yc[par], t_G[t]], writes=[t_retb[par]])
                if "ret" in dbg:
                    P.op("dve", lambda e, par=par: e.tensor_copy(out=retf[par], in_=retb[par]), reads=[t_retb[par]], writes=[t_yc[par]])
                    P.dma("sp", lambda e, par=par, tsl=tsl: e.dma_start(out=dbg["ret"][tsl, :], in_=retf[par]),
                          reads=[t_yc[par]], sem_tile=t_yc[par])

                P.dma("sp", lambda e, par=par, tsl=tsl: e.dma_start(out=cat[tsl, 0:512], in_=retb[par]),
                      reads=[t_retb[par]], writes=[t_cat[0][t]], sem_tile=t_retb[par])

            ret_pending.append(ret_tail)
            if len(ret_pending) > 1:
                ret_pending.pop(0)()
        while ret_pending:
            ret_pending.pop(0)()
        P.barrier()
        A.release()
        if stage == "ret":
            P.final_wait("sp")
            P.emit_all()
            return nc

        A.mark()
        NKB = 20
        w_na = A.alloc([8, 1536], BF16); t_wna = P.tiles_n(3, "wna")
        for g in range(3):
            P.dma("pool", lambda e, g=g: e.dma_start(
                out=w_na[:, :, g * 512:(g + 1) * 512],
                in_=w_in[:, 2048 + g * 512:2048 + (g + 1) * 512].rearrange("(kc k) n -> k kc n", k=128)), writes=[t_wna[g]])
        nqT = A.alloc([4, TOK], BF16); nkT = A.alloc([4, NKB * 128], BF16)
        Vaug = A.alloc([NKB, 8 * 65], BF16)
        na_sb = A.alloc([NT, 512], BF16)
        t_nq = P.tiles_n(NT, "nq"); t_nk = P.tiles_n(NKB, "nk"); t_V = P.tiles_n(NKB, "V"); t_na = P.tiles_n(NT, "na")
        V4 = Vaug.rearrange("p a (h c) -> p a h c", c=65)
        for kb in range(NKB):
            P.op("pool", lambda e, kb=kb: e.memset(V4[:, kb, :, 64:65], 1.0), writes=[t_V[kb]])
        nxs = [A.alloc([8, 256], BF16) for _ in range(2)]; t_nxs = P.tiles_n(2, "xsn")
        nq_tm = [A.alloc([512], BF16) for _ in range(2)]; nk_tm = [A.alloc([512], BF16) for _ in range(2)]
        t_nqtm = P.tiles_n(2, "nqtm"); t_nktm = P.tiles_n(2, "nktm")

        def na_inproj(ps_i, xs_ap, g, reads):
            def f(e):
                ins = None
                for kc in range(8):
                    ins = e.matmul(PS[ps_i], lhsT=xs_ap(kc), rhs=w_na[:, kc, g * 512:(g + 1) * 512],
                                   start=(kc == 0), stop=(kc == 7))
                return ins
            P.op("pe", f, reads=reads, writes=[t_ps[ps_i]])

        chunks = [xh[:, 0:256]] + [xT[:, c * 256:(c + 1) * 256] for c in range(8)] + [xh[:, 256:512]]
        for kb in range(NKB):
            ch, sub = divmod(kb, 2)
            par = kb % 2
            own = 2 <= kb < 18
            if sub == 0:
                P.dma("pool", lambda e, ch=ch: e.dma_start(
                    out=nxs[ch % 2], in_=chunks[ch].rearrange("(kc k) n -> k kc n", k=128)), writes=[t_nxs[ch % 2]])
            xs_ap = lambda kc, ch=ch, sub=sub: nxs[ch % 2][:, kc, sub * 128:(sub + 1) * 128]
            if own:
                t = kb - 2
                na_inproj(0, xs_ap, 0, [t_nxs[ch % 2], t_wna[0]])
                P.op("act", lambda e, par=par: e.activation(func=AF.Copy, out=nq_tm[par], in_=PS[0]), reads=[t_ps[0]], writes=[t_nqtm[par]])

                def fTq(e, par=par):
                    ins = None
                    for pr in range(4):
                        ins = e.transpose(out=PSB[3][:, pr * 128:(pr + 1) * 128], in_=nq_tm[par][:, pr * 128:(pr + 1) * 128],
                                          identity=ident_b)
                    return ins
                P.op("pe", fTq, reads=[t_nqtm[par], t_ident], writes=[t_ps[3]])
                P.op("dve", lambda e, t=t: e.tensor_copy(out=nqT[:, :, t * 128:(t + 1) * 128],
                                                         in_=PSB[3][:, 0:512].rearrange("p (a b) -> p a b", b=128)),
                     reads=[t_ps[3]], writes=[t_nq[t]])
            na_inproj(1, xs_ap, 1, [t_nxs[ch % 2], t_wna[1]])
            na_inproj(2, xs_ap, 2, [t_nxs[ch % 2], t_wna[2]])
            P.op("act", lambda e, par=par: e.activation(func=AF.Copy, out=nk_tm[par], in_=PS[1]), reads=[t_ps[1]], writes=[t_nktm[par]])

            def fTk(e, par=par):
                ins = None
                for pr in range(4):
                    ins = e.transpose(out=PSB[4][:, pr * 128:(pr + 1) * 128], in_=nk_tm[par][:, pr * 128:(pr + 1) * 128],
                                      identity=ident_b)
                return ins
            P.op("pe", fTk, reads=[t_nktm[par], t_ident], writes=[t_ps[4]])
            P.op("dve", lambda e, kb=kb: e.tensor_copy(out=nkT[:, :, kb * 128:(kb + 1) * 128],
                                                       in_=PSB[4][:, 0:512].rearrange("p (a b) -> p a b", b=128)),
                 reads=[t_ps[4]], writes=[t_nk[kb]])
            P.op("dve", lambda e, kb=kb: e.tensor_copy(out=V4[:, kb, :, 0:64], in_=v3(PS[2])),
                 reads=[t_ps[2]], writes=[t_V[kb]])
        Est = [A.alloc([NA_BLOCKS * 128], F32) for _ in range(2)]; t_Est = P.tiles_n(2, "Est")
        Ebf = [A.alloc([NA_BLOCKS, 128], BF16) for _ in range(2)]; t_Ebf = P.tiles_n(2, "Ebf")
        nes = [A.alloc([768], BF16) for _ in range(2)]; t_nes = P.tiles_n(2, "nes")
        pTt = [A.alloc([768], BF16) for _ in range(2)]; t_pT = P.tiles_n(2, "pT")
        rcp = [A.alloc([2], F32) for _ in range(4)]; t_rcp = P.tiles_n(4, "rcp")
        na_pending = []
        for h in range(8):
            hp2 = h % 2
            pr, hp = divmod(h, 2)
            pl, ph = hp * 64, hp * 64 + 64
            P.dma("sp", lambda e, h=h, hp2=hp2: e.dma_start(out=Est[hp2], in_=natab[h]), writes=[t_Est[hp2]])
            P.op("act", lambda e, hp2=hp2: e.activation(out=Ebf[hp2].rearrange("p a b -> p (a b)"), in_=Est[hp2], func=AF.Exp),
                 reads=[t_Est[hp2]], writes=[t_Ebf[hp2]])
            for i in range(NT):
                kt0, nk, b0 = NA_TYPES[i]
                n = h * NT + i
                par = n % 2
                bX, bY = 2 * par, 2 * par + 1
                bO = 4 + n % 4
                qsl = slice(i * 128, (i + 1) * 128)

                def fS(e, kt0=kt0, nk=nk, bX=bX, bY=bY, pl=pl, ph=ph, pr=pr, qsl=qsl):
                    ins = None
                    for idx in range(nk):
                        kt = kt0 + idx
                        bank = PS[bX] if idx < 4 else PS[bY]
                        ins = e.matmul(bank[:, (idx % 4) * 128:(idx % 4) * 128 + 128], lhsT=nkT[pl:ph, pr, kt * 128:(kt + 1) * 128],
                                       rhs=nqT[pl:ph, pr, qsl], start=True, stop=True)
                    return ins
                P.op("pe", fS, reads=[t_nq[i]] + t_nk[kt0:kt0 + nk], writes=[t_ps[bX], t_ps[bY]])
                P.op("act", lambda e, par=par, bX=bX: e.activation(out=nes[par][:, 0:512], in_=PS[bX], func=AF.Exp, scale=0.125),
                     reads=[t_ps[bX]], writes=[t_nes[par]])
                w2 = (nk - 4) * 128
                P.op("act", lambda e, par=par, bY=bY, w2=w2: e.activation(out=nes[par][:, 512:512 + w2], in_=PS[bY][:, 0:w2],
                                                                        func=AF.Exp, scale=0.125),
                     reads=[t_ps[bY]], writes=[t_nes[par]])
                P.op("pool", lambda e, par=par, nk=nk, b0=b0, hp2=hp2: e.tensor_tensor(
                    out=pTt[par][:, 0:nk * 128], in0=nes[par][:, 0:nk * 128],
                    in1=Ebf[hp2][:, b0:b0 + nk, :].rearrange("p a b -> p (a b)"), op=ALU.mult),
                    reads=[t_nes[par], t_Ebf[hp2]], writes=[t_pT[par]])

                def na_tail(kt0=kt0, nk=nk, par=par, bO=bO, h=h, n=n, i=i):
                    def fO(e):
                        ins = None
                        for idx in range(nk):
                            ins = e.matmul(PS[bO][:, 0:65], lhsT=pTt[par][:, idx * 128:(idx + 1) * 128],
                                           rhs=Vaug[:, kt0 + idx, h * 65:(h + 1) * 65], start=(idx == 0), stop=(idx == nk - 1))
                        return ins
                    P.op("pe", fO, reads=[t_pT[par]] + t_V[kt0:kt0 + nk], writes=[t_ps[bO]])
                    r4 = n % 4
                    P.op("dve", lambda e: e.reciprocal(out=rcp[r4][:, 0:1], in_=PS[bO][:, 64:65]),
                         reads=[t_ps[bO]], writes=[t_rcp[r4]])
                    P.op("dve", lambda e: e.tensor_scalar(
                        out=na_sb[:, i, h * 64:(h + 1) * 64], in0=PS[bO][:, 0:64], scalar1=rcp[r4][:, 0:1], scalar2=None, op0=ALU.mult),
                        reads=[t_ps[bO], t_rcp[r4]], writes=[t_na[i]])
                na_pending.append(na_tail)
                if len(na_pending) > 1:
                    na_pending.pop(0)()
        while na_pending:
            na_pending.pop(0)()
        naf = [A.alloc([512], F32) for _ in range(2)] if "na" in dbg else None
        t_naf = P.tiles_n(2, "naf")
        for i in range(NT):
            tsl = slice(i * 128, (i + 1) * 128)
            P.dma("sp", lambda e, i=i, tsl=tsl: e.dma_start(out=cat[tsl, 512:1024], in_=na_sb[:, i, :]),
                  reads=[t_na[i]], writes=[t_cat[1][i]], sem_tile=P.tile("nast%d" % (i % 4)) if i >= 4 else P.tile("nast%d" % i))
            if "na" in dbg:
                P.op("dve", lambda e, i=i: e.tensor_copy(out=naf[i % 2], in_=na_sb[:, i, :]), reads=[t_na[i]], writes=[t_naf[i % 2]])
                P.dma("sp", lambda e, i=i, tsl=tsl: e.dma_start(out=dbg["na"][tsl, :], in_=naf[i % 2]),
                      reads=[t_naf[i % 2]], sem_tile=t_naf[i % 2])
        P.barrier()
        A.release()
        if stage == "na":
            P.final_wait("sp")
            P.emit_all()
            return nc

        x1T = A.alloc([8, TOK], BF16)
        t_x1T = P.tiles_n(NT, "x1T")
        t_GT = P.tiles_n(NT, "GT"); t_base = P.tiles_n(NT, "base")
        A.mark()
        w_o = A.alloc([8, 1024], BF16); w_pgt = A.alloc([8, 1024], BF16); w_ppt = A.alloc([2, 1024], BF16)
        pTb = A.alloc([2, TOK], BF16)
        w_rt = A.alloc([8, 64], F32)
        g1 = A.alloc([1024], F32); b1 = A.alloc([1024], F32); rb = A.alloc([64], F32)
        t_wl = P.tiles_n(9, "mw")
        P.dma("pool", lambda e: e.dma_start(out=w_o, in_=w_out.rearrange("(kc k) n -> k kc n", k=128)), writes=[t_wl[0]])
        P.dma("pool", lambda e: e.dma_start(out=pTb, in_=pT.rearrange("(kc k) n -> k kc n", k=128)), writes=[t_wl[1]])
        P.dma("pool", lambda e: e.dma_start(out=w_ppt, in_=w_pp.rearrange("(kc k) n -> k kc n", k=128)), writes=[t_wl[2]])
        P.dma("pool", lambda e: e.dma_start(out=w_pgt, in_=w_pg.rearrange("(kc k) n -> k kc n", k=128)), writes=[t_wl[3]])
        P.dma("sp", lambda e: e.dma_start(out=w_rt, in_=w_router.rearrange("(kc k) n -> k kc n", k=128)), writes=[t_wl[4]])
        P.dma("sp", lambda e: e.dma_start(out=g1, in_=ln1_g.partition_broadcast(128)), writes=[t_wl[5]])
        P.dma("sp", lambda e: e.dma_start(out=b1, in_=ln1_b.partition_broadcast(128)), writes=[t_wl[6]])
        P.dma("sp", lambda e: e.dma_start(out=rb, in_=rbias.partition_broadcast(128)), writes=[t_wl[7]])
        catb = [A.alloc([1024], BF16) for _ in range(2)]; t_catb = P.tiles_n(2, "catb")
        catT = [A.alloc([8, 128], BF16) for _ in range(2)]; t_catT = P.tiles_n(2, "catT")
        xrt = [A.alloc([1024], F32) for _ in range(2)]; t_xrt = P.tiles_n(2, "xrt")
        zt = [A.alloc([1024], F32) for _ in range(2)]; t_zt = P.tiles_n(2, "zt")
        x1t = [A.alloc([1024], F32) for _ in range(2)]; t_x1t = P.tiles_n(2, "x1t")
        x1T32 = [A.alloc([8, 128], F32) for _ in range(2)]; t_x1T32 = P.tiles_n(2, "x1T32")
        lst = [A.alloc([32], F32) for _ in range(2)]; t_lst = P.tiles_n(2, "lst")
        rt = [A.alloc([768], F32) for _ in range(2)]; t_rt = P.tiles_n(2, "rt")
        gTs = [A.alloc([128], F32) for _ in range(2)]; t_gTs = P.tiles_n(2, "gTs")
        sgp = [A.alloc([1024], F32) for _ in range(2)]; t_sgp = P.tiles_n(2, "sgp")
        bst = [A.alloc([1024], F32) for _ in range(2)]; t_bst = P.tiles_n(2, "bst")

        def layer_norm(src, t_src, dst, t_dst, st, t_st, gain, bias, t_gb):
            for hf_ in range(2):
                P.op("dve", lambda e, hf_=hf_: e.bn_stats(out=st[:, hf_ * 6:(hf_ + 1) * 6], in_=src[:, hf_ * 512:(hf_ + 1) * 512]),
                     reads=[t_src], writes=[t_st])
            P.op("dve", lambda e: e.bn_aggr(out=st[:, 12:14], in_=st[:, 0:12]), reads=[t_st], writes=[t_st])
            P.op("act", lambda e: e.activation(out=st[:, 14:15], in_=st[:, 13:14], func=AF.Ln, bias=cst[:, 4:5], scale=1.0),
                 reads=[t_st, t_cst], writes=[t_st])
            P.op("act", lambda e: e.activation(out=st[:, 14:15], in_=st[:, 14:15], func=AF.Exp, scale=-0.5),
                 reads=[t_st], writes=[t_st])
            P.op("dve", lambda e: e.tensor_scalar(out=dst, in0=src, scalar1=st[:, 12:13], scalar2=st[:, 14:15],
                                                  op0=ALU.subtract, op1=ALU.mult), reads=[t_src, t_st], writes=[t_dst])
            P.op("pool", lambda e: e.tensor_tensor(out=dst, in0=dst, in1=gain, op=ALU.mult), reads=[t_dst] + t_gb, writes=[t_dst])
            P.op("pool", lambda e: e.tensor_tensor(out=dst, in0=dst, in1=bias, op=ALU.add), reads=[t_dst] + t_gb, writes=[t_dst])

        m_pending = []
        m_pendA2 = []
        m_stB = []
        for t in range(NT):
            par = t % 2
            tsl = slice(t * 128, (t + 1) * 128)
            def stageA(t=t, par=par, tsl=tsl):
                P.dma("pool", lambda e, par=par, tsl=tsl: e.dma_start(out=catb[par], in_=cat[tsl, :]),
                      reads=[t_cat[0][t], t_cat[1][t]], writes=[t_catb[par]])
                P.dma("pool", lambda e, par=par, tsl=tsl: e.dma_start(out=xrt[par], in_=xr[tsl, :]), writes=[t_xrt[par]])

                def fCT(e, par=par):
                    ins = None
                    for c in range(8):
                        ins = e.transpose(out=PSB[0][:, c * 128:(c + 1) * 128], in_=catb[par][:, c * 128:(c + 1) * 128], identity=ident_b)
                    return ins
                P.op("pe", fCT, reads=[t_catb[par], t_ident], writes=[t_ps[0]])
                P.op("dve", lambda e, par=par: e.tensor_copy(out=catT[par].rearrange("p a b -> p (a b)"), in_=PSB[0]),
                     reads=[t_ps[0]], writes=[t_catT[par]])
                for half in range(2):
                    def fM(e, par=par, half=half):
                        ins = None
                        for kc in range(8):
                            ins = e.matmul(PS[1 + half], lhsT=catT[par][:, kc, :], rhs=w_o[:, kc, half * 512:(half + 1) * 512],
                                           start=(kc == 0), stop=(kc == 7))
                        return ins
                    P.op("pe", fM, reads=[t_catT[par], t_wl[0]], writes=[t_ps[1 + half]])
                    P.op("dve", lambda e, par=par, half=half: e.scalar_tensor_tensor(
                        out=zt[par][:, half * 512:(half + 1) * 512], in0=xrt[par][:, half * 512:(half + 1) * 512], scalar=ALPHA,
                        in1=PS[1 + half], op0=ALU.mult, op1=ALU.add), reads=[t_xrt[par], t_ps[1 + half]], writes=[t_zt[par]])

            def stageA2(t=t, par=par, tsl=tsl):
                layer_norm(zt[par], t_zt[par], x1t[par], t_x1t[par], lst[par], t_lst[par], g1, b1, [t_wl[5], t_wl[6]])
                if "x1" in dbg:
                    P.dma("sp", lambda e, par=par, tsl=tsl: e.dma_start(out=dbg["x1"][tsl, :], in_=x1t[par]),
                          reads=[t_x1t[par]], sem_tile=t_x1t[par])
                for half in range(2):
                    def fXT(e, par=par, half=half):
                        ins = None
                        for c in range(4):
                            cc_ = half * 4 + c
                            ins = e.transpose(out=PS[3 + half][:, c * 128:(c + 1) * 128], in_=x1t[par][:, cc_ * 128:(cc_ + 1) * 128],
                                              identity=ident_f)
                        return ins
                    P.op("pe", fXT, reads=[t_x1t[par], t_ident], writes=[t_ps[3 + half]])
                    P.op("dve", lambda e, half=half, tsl=tsl: e.tensor_copy(
                        out=x1T[:, half * 4:half * 4 + 4, tsl], in_=PS[3 + half].rearrange("p (a b) -> p a b", b=128)),
                        reads=[t_ps[3 + half]], writes=[t_x1T[t]])
                    P.op("act", lambda e, half=half, par=par: e.copy(
                        out=x1T32[par].rearrange("p a b -> p (a b)")[:, half * 512:(half + 1) * 512], in_=PS[3 + half]),
                        reads=[t_ps[3 + half]], writes=[t_x1T32[par]])


            def stageB(t=t, par=par, tsl=tsl):
                def fR(e, par=par):
                    ins = None
                    for kc in range(8):
                        ins = e.matmul(PS[5][:, 0:64], lhsT=x1T32[par][:, kc, :], rhs=w_rt[:, kc, :], start=(kc == 0), stop=(kc == 7))
                    return ins
                P.op("pe", fR, reads=[t_x1T32[par], t_wl[4]], writes=[t_ps[5]])
                R = rt[par]
                sc, bi, eq, bi2 = R[:, 0:64], R[:, 64:128], R[:, 128:192], R[:, 192:256]
                m1, m2, gs, t8a, gm, pen = R[:, 256:264], R[:, 264:272], R[:, 272:280], R[:, 280:288], R[:, 288:296], R[:, 296:304]
                msk, t8b, sel, wv, den, gate = R[:, 320:384], R[:, 304:312], R[:, 384:448], R[:, 448:512], R[:, 312:314], R[:, 512:576]
                g3 = lambda ap: ap.rearrange("p (a b) -> p a b", b=8)
                tr_ = [t_rt[par]]
                P.op("act", lambda e, sc=sc: e.activation(out=sc, in_=PS[5][:, 0:64], func=AF.Sigmoid), reads=[t_ps[5]], writes=tr_)
                P.op("dve", lambda e, sc=sc, bi=bi: e.tensor_tensor(out=bi, in0=sc, in1=rb, op=ALU.add), reads=tr_ + [t_wl[7]], writes=tr_)
                P.op("dve", lambda e, bi=bi, m1=m1: e.tensor_reduce(out=m1, in_=g3(bi), axis=AX.X, op=ALU.max), reads=tr_, writes=tr_)
                P.op("dve", lambda e, bi=bi, m1=m1, eq=eq: e.tensor_tensor(out=g3(eq), in0=g3(bi), in1=bc_last(m1, 8), op=ALU.is_equal),
                     reads=tr_, writes=tr_)
                P.op("dve", lambda e, bi=bi, eq=eq, bi2=bi2: e.scalar_tensor_tensor(out=bi2, in0=eq, scalar=-1e9, in1=bi,
                                                                                    op0=ALU.mult, op1=ALU.add), reads=tr_, writes=tr_)
                P.op("dve", lambda e, bi2=bi2, m2=m2: e.tensor_reduce(out=m2, in_=g3(bi2), axis=AX.X, op=ALU.max), reads=tr_, writes=tr_)
                P.op("dve", lambda e, m1=m1, m2=m2, gs=gs: e.tensor_tensor(out=gs, in0=m1, in1=m2, op=ALU.add), reads=tr_, writes=tr_)
                P.op("dve", lambda e, gs=gs, t8a=t8a: e.max(out=t8a, in_=gs), reads=tr_, writes=tr_)
                P.op("dve", lambda e, gs=gs, t8a=t8a, gm=gm: e.tensor_scalar(out=gm, in0=gs, scalar1=t8a[:, 3:4], scalar2=None, op0=ALU.is_ge),
                     reads=tr_, writes=tr_)
                P.op("dve", lambda e, gm=gm, pen=pen: e.tensor_scalar(out=pen, in0=gm, scalar1=-1.0, scalar2=1e9, op0=ALU.add, op1=ALU.mult),
                     reads=tr_, writes=tr_)
                P.op("dve", lambda e, bi=bi, pen=pen, msk=msk: e.tensor_tensor(out=g3(msk), in0=g3(bi), in1=bc_last(pen, 8), op=ALU.add),
                     reads=tr_, writes=tr_)
                P.op("dve", lambda e, msk=msk, t8b=t8b: e.max(out=t8b, in_=msk), reads=tr_, writes=tr_)
                P.op("dve", lambda e, msk=msk, t8b=t8b, sel=sel: e.tensor_scalar(out=sel, in0=msk, scalar1=t8b[:, 7:8], scalar2=None,
                                                                                 op0=ALU.is_ge), reads=tr_, writes=tr_)
                P.op("dve", lambda e, sc=sc, sel=sel, wv=wv: e.tensor_tensor(out=wv, in0=sc, in1=sel, op=ALU.mult), reads=tr_, writes=tr_)
                P.op("dve", lambda e, wv=wv, den=den: e.tensor_reduce(out=den[:, 0:1], in_=wv, axis=AX.X, op=ALU.add), reads=tr_, writes=tr_)
                P.op("dve", lambda e, den=den: e.reciprocal(out=den[:, 1:2], in_=den[:, 0:1]), reads=tr_, writes=tr_)
                P.op("dve", lambda e, wv=wv, den=den, gate=gate: e.tensor_scalar(out=gate, in0=wv, scalar1=den[:, 1:2], scalar2=2.5,
                                                                                 op0=ALU.mult, op1=ALU.mult), reads=tr_, writes=tr_)
                if "gate" in dbg:
                    P.dma("sp", lambda e, gate=gate, tsl=tsl: e.dma_start(out=dbg["gate"][tsl, :], in_=gate), reads=tr_, sem_tile=t_rt[par])
                P.op("pe", lambda e, gate=gate: e.transpose(out=PS[5][0:64, 128:256], in_=gate, identity=ident_f),
                     reads=tr_ + [t_ident], writes=[t_ps[5]])
                P.op("act", lambda e, par=par: e.activation(func=AF.Copy, out=gTs[par][0:64, :], in_=PS[5][0:64, 128:256]), reads=[t_ps[5]], writes=[t_gTs[par]])
                P.dma("sp", lambda e, par=par, tsl=tsl: e.dma_start(out=GT[:, tsl], in_=gTs[par][0:64, :]),
                      reads=[t_gTs[par]], writes=[t_GT[t]], sem_tile=t_gTs[par])
                for half in range(2):
                    hs = slice(half * 512, (half + 1) * 512)

                    def fPG(e, half=half, tsl=tsl, hs=hs):
                        ins = None
                        for kc in range(8):
                            ins = e.matmul(PS[6 + half], lhsT=x1T[:, kc, tsl], rhs=w_pgt[:, kc, hs], start=(kc == 0), stop=(kc == 7))
                        return ins
                    P.op("pe", fPG, reads=[t_x1T[t], t_wl[3]], writes=[t_ps[6 + half]])
                    P.op("act", lambda e, half=half, par=par, hs=hs: e.activation(out=sgp[par][:, hs], in_=PS[6 + half], func=AF.Sigmoid),
                         reads=[t_ps[6 + half]], writes=[t_sgp[par]])

                    def fPP(e, half=half, tsl=tsl, hs=hs):
                        ins = None
                        for kc in range(2):
                            ins = e.matmul(PS[6 + half], lhsT=pTb[:, kc, tsl], rhs=w_ppt[:, kc, hs], start=(kc == 0), stop=(kc == 1))
                        return ins
                    P.op("pe", fPP, reads=[t_wl[1], t_wl[2]], writes=[t_ps[6 + half]])
                    P.op("dve", lambda e, half=half, par=par, hs=hs: e.tensor_tensor(out=bst[par][:, hs], in0=PS[6 + half], in1=sgp[par][:, hs],
                                                                                     op=ALU.mult),
                         reads=[t_ps[6 + half], t_sgp[par]], writes=[t_bst[par]])
                P.op("dve", lambda e, par=par: e.scalar_tensor_tensor(out=bst[par], in0=x1t[par], scalar=ALPHA, in1=bst[par],
                                                                      op0=ALU.mult, op1=ALU.add),
                     reads=[t_x1t[par], t_bst[par]], writes=[t_bst[par]])
                P.dma("sp", lambda e, par=par, tsl=tsl: e.dma_start(out=base[tsl, :], in_=bst[par]),
                      reads=[t_bst[par]], writes=[t_base[t]], sem_tile=t_bst[par])

            stageA()
            m_pendA2.append(stageA2)
            if len(m_pendA2) > 1:
                m_pendA2.pop(0)()
                m_pending.append(m_stB.pop(0))
            m_stB.append(stageB)
            if len(m_pending) > 1:
                m_pending.pop(0)()
        while m_pendA2:
            m_pendA2.pop(0)()
            m_pending.append(m_stB.pop(0))
            if len(m_pending) > 1:
                m_pending.pop(0)()
        while m_pending:
            m_pending.pop(0)()
        P.barrier()
        A.release()
        if stage == "x1":
            P.final_wait("sp")
            P.emit_all()
            return nc

        y_acc = A.alloc([NT, 1024], F32); t_y = P.tiles_n(NT, "yacc")
        A.mark()
        m_Wgu = [A.alloc([8, 512], BF16) for _ in range(4)]; m_Wdn = [A.alloc([2, 1024], BF16) for _ in range(4)]
        t_mWgu = P.tiles_n(4, "mWgu"); t_mWdn = P.tiles_n(4, "mWdn")
        m_gbc = [A.alloc([TOK], F32) for _ in range(4)]; t_mgbc = P.tiles_n(4, "mgbc")
        m_act = A.alloc([4, TOK], BF16)
        t_mact = [P.tiles_n(4, "mact%d_" % k_) for k_ in range(4)]
        m_sg = [A.alloc([512], BF16) for _ in range(2)]; m_tt = [A.alloc([512], BF16) for _ in range(2)]
        t_msg = P.tiles_n(2, "msg"); t_mtt = P.tiles_n(2, "mtt")

        def m_load(ex):
            s_ = ex % 4
            src_gu = w_egu[ex] if ex < NEXP else w_sgu
            src_dn = w_edn[ex] if ex < NEXP else w_sdn
            P.dma("pool", lambda eng, s_=s_, src_gu=src_gu: eng.dma_start(
                out=m_Wgu[s_], in_=src_gu.rearrange("(kc k) n -> k kc n", k=128)), writes=[t_mWgu[s_]])
            P.dma("pool", lambda eng, s_=s_, src_dn=src_dn: eng.dma_start(
                out=m_Wdn[s_], in_=src_dn.rearrange("(kc k) n -> k kc n", k=128)), writes=[t_mWdn[s_]])
            if ex < NEXP:
                P.dma("sp", lambda eng, ex=ex: eng.dma_start(out=m_gbc[ex % 4], in_=GT[ex:ex + 1, :].partition_broadcast(128)),
                      reads=t_GT, writes=[t_mgbc[ex % 4]])

        m_groups = [(2 * g_, 2 * g_ + 1) for g_ in range(NEXP // 2)] + [(NEXP,)]
        for ex in m_groups[0]:
            m_load(ex)
        m_cnt = 0
        for gi, grp in enumerate(m_groups):
            if gi + 1 < len(m_groups):
                for ex in m_groups[gi + 1]:
                    m_load(ex)
            for eg, ex in enumerate(grp):
                s_ = ex % 4
                for tg in range(4):
                    tgs = slice(tg * 512, (tg + 1) * 512)
                    for j in range(2):
                        q_ = m_cnt % 2
                        m_cnt += 1
                        bg, bu = 2 * q_, 2 * q_ + 1

                        def fUp(eng, s_=s_, j=j, tgs=tgs, bg=bg, bu=bu):
                            ins = None
                            for kc in range(8):
                                eng.matmul(PS[bg], lhsT=m_Wgu[s_][:, kc, j * 128:(j + 1) * 128], rhs=x1T[:, kc, tgs],
                                           start=(kc == 0), stop=(kc == 7))
                            for kc in range(8):
                                ins = eng.matmul(PS[bu], lhsT=m_Wgu[s_][:, kc, 256 + j * 128:256 + (j + 1) * 128], rhs=x1T[:, kc, tgs],
                                                 start=(kc == 0), stop=(kc == 7))
                            return ins
                        P.op("pe", fUp, reads=[t_mWgu[s_]] + t_x1T[4 * tg:4 * tg + 4], writes=[t_ps[bg], t_ps[bu]])
                        P.op("act", lambda eng, q_=q_, bg=bg: eng.activation(out=m_sg[q_], in_=PS[bg], func=AF.Silu),
                             reads=[t_ps[bg]], writes=[t_msg[q_]])
                        if ex < NEXP:
                            P.op("dve", lambda eng, q_=q_, bu=bu, ex=ex, tgs=tgs: eng.tensor_tensor(
                                out=m_tt[q_], in0=PS[bu], in1=m_gbc[ex % 4][:, tgs], op=ALU.mult),
                                reads=[t_ps[bu], t_mgbc[ex % 4]], writes=[t_mtt[q_]])
                        else:
                            P.op("dve", lambda eng, q_=q_, bu=bu: eng.tensor_copy(out=m_tt[q_], in_=PS[bu]),
                                 reads=[t_ps[bu]], writes=[t_mtt[q_]])
                        P.op("pool", lambda eng, q_=q_, eg=eg, j=j, tgs=tgs: eng.tensor_tensor(
                            out=m_act[:, eg * 2 + j, tgs], in0=m_sg[q_], in1=m_tt[q_], op=ALU.mult),
                            reads=[t_msg[q_], t_mtt[q_]], writes=[t_mact[eg * 2 + j][tg]])
            for t in range(NT):
                yq = t % 2
                tsl = slice(t * 128, (t + 1) * 128)
                for half in range(2):
                    by = 4 + 2 * yq + half
                    hs = slice(half * 512, (half + 1) * 512)

                    def fDn(eng, grp=grp, tsl=tsl, by=by, hs=hs):
                        ins = None
                        n_ = len(grp) * 2
                        k_ = 0
                        for eg, ex in enumerate(grp):
                            for j in range(2):
                                ins = eng.matmul(PS[by], lhsT=m_act[:, eg * 2 + j, tsl], rhs=m_Wdn[ex % 4][:, j, hs],
                                                 start=(k_ == 0), stop=(k_ == n_ - 1))
                                k_ += 1
                        return ins
                    rd_ = [t_mact[eg * 2 + j][t // 4] for eg in range(len(grp)) for j in range(2)] + [t_mWdn[ex % 4] for ex in grp]
                    P.op("pe", fDn, reads=rd_, writes=[t_ps[by]])
                    if gi == 0:
                        P.op("dve", lambda eng, t=t, hs=hs, by=by: eng.tensor_copy(out=y_acc[:, t, hs], in_=PS[by]),
                             reads=[t_ps[by]], writes=[t_y[t]])
                    else:
                        P.op("dve", lambda eng, t=t, hs=hs, by=by: eng.tensor_tensor(out=y_acc[:, t, hs], in0=PS[by], in1=y_acc[:, t, hs],
                                                                                     op=ALU.add),
                             reads=[t_ps[by], t_y[t]], writes=[t_y[t]])
        P.barrier()
        A.release()
        g2 = A.alloc([1024], F32); b2 = A.alloc([1024], F32)
        t_f = P.tiles_n(2, "fgb")
        P.dma("sp", lambda eng: eng.dma_start(out=g2, in_=ln2_g.partition_broadcast(128)), writes=[t_f[0]])
        P.dma("sp", lambda eng: eng.dma_start(out=b2, in_=ln2_b.partition_broadcast(128)), writes=[t_f[1]])
        m_bt = [A.alloc([1024], F32) for _ in range(2)]; m_z2 = [A.alloc([1024], F32) for _ in range(2)]
        m_o = [A.alloc([1024], F32) for _ in range(2)]; m_st = [A.alloc([32], F32) for _ in range(2)]
        t_mbt = P.tiles_n(2, "mbt"); t_mz2 = P.tiles_n(2, "mz2"); t_mo = P.tiles_n(2, "mo"); t_mst = P.tiles_n(2, "mst")
        for t in range(NT):
            par = t % 2
            tsl = slice(t * 128, (t + 1) * 128)
            P.dma("pool", lambda eng, par=par, tsl=tsl: eng.dma_start(out=m_bt[par], in_=base[tsl, :]),
                  reads=[t_base[t]], writes=[t_mbt[par]])
            P.op("pool", lambda eng, par=par, t=t: eng.tensor_tensor(out=m_z2[par], in0=y_acc[:, t, :], in1=m_bt[par], op=ALU.add),
                 reads=[t_y[t], t_mbt[par]], writes=[t_mz2[par]])
            layer_norm(m_z2[par], t_mz2[par], m_o[par], t_mo[par], m_st[par], t_mst[par], g2, b2, t_f)
            P.dma("sp", lambda eng, par=par, tsl=tsl: eng.dma_start(out=out[tsl, :], in_=m_o[par]),
                  reads=[t_mo[par]], sem_tile=t_mo[par])
        P.final_wait("sp")
        P.emit_all()
        return nc


def _na_table(rpb, hf):
    base = 32 * hf
    tab = np.full((8, 128, NA_BLOCKS * 128), NEG, np.float32)
    kk = np.arange(128)
    qq = np.arange(128)
    for i in (0, 1, 2, 14, 15):
        kt0, nk, b0 = NA_TYPES[i]
        r = base + 2 * i + qq // 64
        c = qq % 64
        rs = np.clip(r - 4, 0, 56)
        cs = np.clip(c - 8, 0, 48)
        for idx in range(nk):
            kt = kt0 + idx
            kr = base - 4 + 2 * kt + kk // 64
            kc = kk % 64
            vr = (kr[:, None] >= rs[None, :]) & (kr[:, None] <= rs[None, :] + 7) & (kr[:, None] >= 0) & (kr[:, None] <= 63)
            vc = (kc[:, None] >= cs[None, :]) & (kc[:, None] <= cs[None, :] + 15)
            valid = vr & vc
            dr = np.clip(kr[:, None] - r[None, :] + 7, 0, 14)
            dc = np.clip(kc[:, None] - c[None, :] + 15, 0, 30)
            vals = rpb[:, dr, dc]
            blk = np.where(valid[None], vals, np.float32(NEG))
            tab[:, :, (b0 + idx) * 128:(b0 + idx + 1) * 128] = blk
    return tab


def _const_tables(hf):
    half = 32
    inv = (10000.0 ** (-np.arange(half, dtype=np.float32) / half)).astype(np.float32)
    j = np.arange(128)
    t = np.arange(NT)

    def rot(pos0):
        pos = (pos0 + t[None, :] * 128 + j[:, None]).astype(np.float32)
        ang = pos[:, :, None] * inv[None, None, :]
        cos = np.cos(ang).astype(np.float32)
        sin = np.sin(ang).astype(np.float32)
        cc = np.concatenate([cos, cos], -1).reshape(128, NT * 64)
        ss = np.concatenate([-sin, sin], -1).reshape(128, NT * 64)
        return np.ascontiguousarray(cc), np.ascontiguousarray(ss)
    cc_own, ss_own = rot(hf * TOK)
    cc_oth, ss_oth = rot((1 - hf) * TOK)
    m = t[None, :] * 128 + j[:, None]
    dist = (2047 - m if hf == 1 else m).astype(np.float32)
    ii = np.arange(128, dtype=np.float32)
    epos = np.maximum(ii[None, :] - ii[:, None], 0).astype(np.float32)
    eneg = np.maximum(ii[:, None] - ii[None, :], 0).astype(np.float32)
    iota1 = np.broadcast_to(ii[None, :] + 1, (128, 128)).astype(np.float32).copy()
    iota2 = np.broadcast_to(128 - ii[None, :], (128, 128)).astype(np.float32).copy()
    return dict(cc_own=cc_own, ss_own=ss_own, cc_oth=cc_oth, ss_oth=ss_oth, dist=np.ascontiguousarray(dist),
                epos=epos, eneg=eneg, iota1=iota1, iota2=iota2,
                c127=(127 - ii).reshape(128, 1).astype(np.float32), cj=ii.reshape(128, 1).copy(),
                flag=np.full((1, 1), float(hf), np.float32))


def make_in_maps(inputs, cores=range(NCORES)):
    f = lambda a: np.ascontiguousarray(np.asarray(a, dtype=np.float32))
    x = f(inputs["x"]); p = f(inputs["p"])[0]
    shared = dict(
        w_in=f(inputs["w_in"][0]), w_out=f(inputs["w_out"][0]), w_router=f(inputs["w_router"][0]),
        w_egu=f(inputs["w_expert_gu"][0]), w_edn=f(inputs["w_expert_down"][0]),
        w_sgu=f(inputs["w_shared_gu"][0]), w_sdn=f(inputs["w_shared_down"][0]),
        w_pp=f(inputs["w_ple_proj"][0]), w_pg=f(inputs["w_ple_gate"][0]),
        dec_f=f(inputs["ret_decay_fwd"]).reshape(1, 8), dec_b=f(inputs["ret_decay_bwd"]).reshape(1, 8),
        decfT=f(f(inputs["ret_decay_fwd"]).reshape(4, 2).T), decbT=f(f(inputs["ret_decay_bwd"]).reshape(4, 2).T),
        gn_gain=f(inputs["ret_gn_gain"]).reshape(1, 512),
        ln1_g=f(inputs["ln1_gain"]).reshape(1, D), ln1_b=f(inputs["ln1_bias"]).reshape(1, D),
        ln2_g=f(inputs["ln2_gain"]).reshape(1, D), ln2_b=f(inputs["ln2_bias"]).reshape(1, D),
        rbias=f(inputs["router_bias"]).reshape(1, NEXP))
    rpb = f(inputs["na_rpb"][0])
    per_hf = {}
    for hf in (0, 1):
        d = _const_tables(hf)
        d["natab"] = _na_table(rpb, hf)
        per_hf[hf] = d
    maps = []
    for c in cores:
        b, hf = divmod(c, 2)
        own = x[b, hf * TOK:(hf + 1) * TOK]
        oth = x[b, (1 - hf) * TOK:(2 - hf) * TOK]
        xh = np.zeros((512, D), np.float32)
        if hf == 0:
            xh[256:512] = oth[0:256]
        else:
            xh[0:256] = oth[TOK - 256:TOK]
        m = dict(shared)
        m.update(per_hf[hf])
        m.update(xT=f(own.T), xo=f(oth.T), xh=f(xh.T), xr=f(own), pT=f(p[b, hf * TOK:(hf + 1) * TOK].T))
        maps.append(m)
    return maps


_NC_CACHE = {}


def kernel(**inputs):
    if "full" not in _NC_CACHE:
        _NC_CACHE["full"] = build_program("full")
    nc = _NC_CACHE["full"]
    maps = make_in_maps(inputs)
    res = run_bass_kernel_spmd(nc, maps, core_ids=list(range(NCORES)))
    outp = np.empty((4, S, D), np.float32)
    for c in range(NCORES):
        b, hf = divmod(c, 2)
        outp[b, hf * TOK:(hf + 1) * TOK] = res.results[c]["out"]
    return outp
```

```python
import math
from contextlib import ExitStack

import numpy as np
import concourse.bass as bass
import concourse.mybir as mybir
from concourse.bass_utils import run_bass_kernel_spmd

F32 = mybir.dt.float32
BF16 = mybir.dt.bfloat16
U8 = mybir.dt.uint8
AF = mybir.ActivationFunctionType
ALU = mybir.AluOpType
AX = mybir.AxisListType
DT_SIZE = {F32: 4, BF16: 2, U8: 1}

NCORES = 8
D = 1024
S = 4096
TOK = 2048
NT = 16
NEXP = 64
ALPHA = 2.0 ** 0.25
LN_EPS = 1e-5
GN_EPS = 1e-6
NEG = -30000.0
NA_TYPES = {0: (0, 6, 0), 1: (1, 5, 6), 14: (14, 5, 16), 15: (14, 6, 21)}
for _i in range(2, 14):
    NA_TYPES[_i] = (_i, 5, 11)
NA_BLOCKS = 27


class Tile:
    __slots__ = ("name", "lw", "rd", "dsem", "excl")

    def __init__(self, name):
        self.name = name
        self.excl = False
        self.lw = None
        self.rd = []
        self.dsem = None


class Prog:
    ENGS = ("pe", "act", "dve", "pool", "sp")

    def __init__(self, nc, stack):
        self.nc = nc
        self.stack = stack
        self.streams = {e: [] for e in self.ENGS}
        self.sems = {}
        self.cnt = {}
        for e in self.ENGS:
            self.sems[e] = stack.enter_context(nc.semaphore("s_" + e))
            self.cnt[e] = 0
        self.seen = {e: {} for e in self.ENGS}
        self.ndsem = 0
        self.tiles = []

    def tile(self, name="t"):
        t = Tile(name)
        self.tiles.append(t)
        return t

    def tiles_n(self, n, name="t"):
        return [self.tile("%s%d" % (name, i)) for i in range(n)]

    def _dma_sem(self, t):
        if t.dsem is None:
            key = "d%d" % self.ndsem
            self.ndsem += 1
            self.sems[key] = self.stack.enter_context(self.nc.semaphore("s_" + key))
            self.cnt[key] = 0
            t.dsem = key
        return t.dsem

    def _waits(self, eng, reads, writes):
        need = {}

        def add(ev):
            if ev is None:
                return
            k, v = ev
            if need.get(k, 0) < v:
                need[k] = v
        for t in reads:
            add(t.lw)
            if t.excl:
                for ev in t.rd:
                    if ev[0] != eng:
                        add(ev)
        for t in writes:
            add(t.lw)
            for ev in t.rd:
                add(ev)
        out = []
        for k, v in need.items():
            if k == "pe" and eng == "pe":
                continue
            if self.seen[eng].get(k, 0) >= v:
                continue
            self.seen[eng][k] = v
            out.append((k, v))
        return out

    def op(self, eng, fn, reads=(), writes=()):
        waits = self._waits(eng, reads, writes)
        self.cnt[eng] += 1
        ev = (eng, self.cnt[eng])
        sems = self.sems

        def emit(e, waits=waits, fn=fn, semk=eng):
            for k, v in waits:
                e.wait_ge(sems[k], v)
            ins = fn(e)
            ins.then_inc(sems[semk], 1)
        self.streams[eng].append(emit)
        for t in reads:
            t.rd.append(ev)
        for t in writes:
            t.lw = ev
            t.rd = []
        return ev

    def dma(self, q, fn, reads=(), writes=(), sem_tile=None):
        st = sem_tile or (writes[0] if writes else reads[0])
        key = self._dma_sem(st)
        waits = self._waits(q, reads, writes)
        if self.cnt[key] > 0 and self.seen[q].get(key, 0) < self.cnt[key]:
            self.seen[q][key] = self.cnt[key]
            waits.append((key, self.cnt[key]))
        self.cnt[key] += 16
        ev = (key, self.cnt[key])
        sems = self.sems

        def emit(e, waits=waits, fn=fn, key=key):
            for k, v in waits:
                e.wait_ge(sems[k], v)
            ins = fn(e)
            ins.then_inc(sems[key], 16)
        self.streams[q].append(emit)
        for t in reads:
            t.rd.append(ev)
        for t in writes:
            t.lw = ev
            t.rd = []
        return ev

    def barrier(self):
        snap = {k: v for k, v in self.cnt.items() if v > 0}
        sems = self.sems
        for eng in self.ENGS:
            waits = []
            for k, v in snap.items():
                if k == eng:
                    continue
                if self.seen[eng].get(k, 0) >= v:
                    continue
                self.seen[eng][k] = v
                waits.append((k, v))

            def emit(e, waits=waits):
                for k, v in waits:
                    e.wait_ge(sems[k], v)
            self.streams[eng].append(emit)
        for t in self.tiles:
            t.rd = []

    def final_wait(self, eng="sp"):
        snap = {k: v for k, v in self.cnt.items() if v > 0}
        sems = self.sems

        def emit(e):
            for k, v in snap.items():
                if k == eng:
                    continue
                e.wait_ge(sems[k], v)
        self.streams[eng].append(emit)

    def emit_all(self):
        nc = self.nc
        streams = self.streams
        with nc.Block() as block:
            @block.tensor
            def _(e):
                for f in streams["pe"]:
                    f(e)

            @block.scalar
            def _(e):
                for f in streams["act"]:
                    f(e)

            @block.vector
            def _(e):
                for f in streams["dve"]:
                    f(e)

            @block.gpsimd
            def _(e):
                for f in streams["pool"]:
                    f(e)

            @block.sync
            def _(e):
                for f in streams["sp"]:
                    f(e)


class Arena:
    def __init__(self, nc, stack, nbytes):
        self.t = stack.enter_context(nc.sbuf_tensor("arena", [128, nbytes], U8))
        self.n = nbytes
        self.off = 0
        self.marks = []
        self.peak = 0

    def mark(self):
        self.marks.append(self.off)

    def release(self):
        self.off = self.marks.pop()

    def alloc(self, free_shape, dtype):
        n = int(np.prod(free_shape)) * DT_SIZE[dtype]
        n_al = (n + 63) // 64 * 64
        assert self.off + n_al <= self.n, ("SBUF arena overflow", self.off, n_al, self.n)
        ap = self.t[:, self.off:self.off + n].bitcast(dtype)
        self.off += n_al
        self.peak = max(self.peak, self.off)
        if len(free_shape) == 2:
            ap = ap.rearrange("p (a b) -> p a b", b=free_shape[1])
        elif len(free_shape) == 3:
            ap = ap.rearrange("p (a b c) -> p a b c", b=free_shape[1], c=free_shape[2])
        return ap


def bc_mid(ap2, n):
    p, f = ap2.shape
    return ap2.unsqueeze(1).to_broadcast([p, n, f])


def bc_last(ap2, n):
    p, a = ap2.shape
    return ap2.unsqueeze(2).to_broadcast([p, a, n])


def build_program(stage="full"):
    nc = bass.Bass("TRN2", target_bir_lowering=False)

    def din(name, shape):
        return nc.dram_tensor(name, list(shape), F32, kind="ExternalInput").ap()

    xT = din("xT", [D, TOK]); xo = din("xo", [D, TOK]); xh = din("xh", [D, 512])
    xr = din("xr", [TOK, D]); pT = din("pT", [256, TOK])
    w_in = din("w_in", [D, 3584]); w_out = din("w_out", [D, D]); w_router = din("w_router", [D, NEXP])
    w_egu = din("w_egu", [NEXP, D, 512]); w_edn = din("w_edn", [NEXP, 256, D])
    w_sgu = din("w_sgu", [D, 512]); w_sdn = din("w_sdn", [256, D])
    w_pp = din("w_pp", [256, D]); w_pg = din("w_pg", [D, D])
    cc_own = din("cc_own", [128, NT * 64]); ss_own = din("ss_own", [128, NT * 64])
    cc_oth = din("cc_oth", [128, NT * 64]); ss_oth = din("ss_oth", [128, NT * 64])
    dist = din("dist", [128, NT])
    dec_f = din("dec_f", [1, 8]); dec_b = din("dec_b", [1, 8])
    decfT = din("decfT", [2, 4]); decbT = din("decbT", [2, 4]); flag = din("flag", [1, 1])
    epos = din("epos", [128, 128]); eneg = din("eneg", [128, 128])
    iota1 = din("iota1", [128, 128]); iota2 = din("iota2", [128, 128])
    c127 = din("c127", [128, 1]); cj = din("cj", [128, 1])
    gn_gain = din("gn_gain", [1, 512]); natab = din("natab", [8, 128, NA_BLOCKS * 128])
    ln1_g = din("ln1_g", [1, D]); ln1_b = din("ln1_b", [1, D])
    ln2_g = din("ln2_g", [1, D]); ln2_b = din("ln2_b", [1, D]); rbias = din("rbias", [1, NEXP])
    out = nc.dram_tensor("out", [TOK, D], F32, kind="ExternalOutput").ap()
    GT = nc.dram_tensor("GT", [NEXP, TOK], F32, kind="Internal").ap()
    base = nc.dram_tensor("base", [TOK, D], F32, kind="Internal").ap()
    cat = nc.dram_tensor("cat", [TOK, D], BF16, kind="Internal").ap()
    dbg = {}
    if stage != "full":
        dbg["ret"] = nc.dram_tensor("dbg_ret", [TOK, 512], F32, kind="ExternalOutput").ap()
        dbg["na"] = nc.dram_tensor("dbg_na", [TOK, 512], F32, kind="ExternalOutput").ap()
        dbg["x1"] = nc.dram_tensor("dbg_x1", [TOK, D], F32, kind="ExternalOutput").ap()
        dbg["gate"] = nc.dram_tensor("dbg_gate", [TOK, NEXP], F32, kind="ExternalOutput").ap()
        dbg["misc"] = nc.dram_tensor("dbg_misc", [128, 64], F32, kind="ExternalOutput").ap()

    with ExitStack() as st:
        P = Prog(nc, st)
        A = Arena(nc, st, 206 * 1024)
        PS = [st.enter_context(nc.psum_tensor("ps%d" % i, [128, 512], F32))[:, :] for i in range(8)]
        PSB = [p.bitcast(BF16) for p in PS]
        t_ps = P.tiles_n(8, "ps")
        for t_ in t_ps:
            t_.excl = True

        ident_f = A.alloc([128], F32); ident_b = A.alloc([128], BF16)
        t_ident = P.tile("ident")

        P.op("pool", lambda e: e.memset(ident_f, 0.0), writes=[t_ident])
        P.op("pool", lambda e: e.affine_select(out=ident_f, in_=ident_f, pattern=[[-1, 128]], compare_op=ALU.not_equal,
                                               fill=1.0, base=0, channel_multiplier=1), reads=[t_ident], writes=[t_ident])
        P.op("dve", lambda e: e.tensor_copy(out=ident_b, in_=ident_f), reads=[t_ident], writes=[t_ident])

        cst = A.alloc([8], F32)
        t_cst = P.tile("cst")

        for (p0, p1, c0, c1, val) in ((0, 128, 0, 1, math.log(0.125)), (0, 128, 1, 2, 1.0), (0, 128, 2, 3, 0.0),
                                      (0, 128, 3, 4, GN_EPS), (0, 128, 4, 5, LN_EPS), (0, 64, 5, 6, 1.0),
                                      (64, 128, 5, 6, 0.0), (0, 64, 6, 7, 0.0), (64, 128, 6, 7, 1.0)):
            P.op("pool", lambda e, p0=p0, p1=p1, c0=c0, c1=c1, val=val: e.memset(cst[p0:p1, c0:c1], val), writes=[t_cst])

        t_cat = [P.tiles_n(NT, "catR"), P.tiles_n(NT, "catN")]

        A.mark()
        w_ret = A.alloc([8, 2048], BF16)
        t_wret = P.tiles_n(4, "wret")
        for g in range(4):
            P.dma("pool", lambda e, g=g: e.dma_start(
                out=w_ret[:, :, g * 512:(g + 1) * 512],
                in_=w_in[:, g * 512:(g + 1) * 512].rearrange("(kc k) n -> k kc n", k=128)), writes=[t_wret[g]])
        ccX = A.alloc([NT, 64], F32); ssX = A.alloc([NT, 64], F32)
        ccO, ssO = ccX, ssX
        t_rotl = P.tiles_n(2, "rot")

        def load_rot(c_src, s_src):
            for i_, (dst, src) in enumerate(((ccX, c_src), (ssX, s_src))):
                P.dma("sp", lambda e, dst=dst, src=src: e.dma_start(out=dst.rearrange("p a b -> p (a b)"), in_=src),
                      writes=[t_rotl[i_]])
        load_rot(cc_oth, ss_oth)
        small = A.alloc([256], F32)
        t_small = P.tile("small")
        sm = lambda a, b: small[:, a:b]
        lds = [(sm(0, 8), dec_f.partition_broadcast(128)), (sm(8, 16), dec_b.partition_broadcast(128)),
               (small[0:64, 16:20], decfT[0:1, :].partition_broadcast(64)),
               (small[64:128, 16:20], decfT[1:2, :].partition_broadcast(64)),
               (small[0:64, 20:24], decbT[0:1, :].partition_broadcast(64)),
               (small[64:128, 20:24], decbT[1:2, :].partition_broadcast(64)),
               (sm(24, 25), flag.partition_broadcast(128)), (sm(96, 97), c127), (sm(97, 98), cj),
               (sm(100, 116), dist)]
        t_smld = P.tiles_n(len(lds), "smld")
        for i_, (dst, src) in enumerate(lds):
            P.dma("sp", lambda e, dst=dst, src=src: e.dma_start(out=dst, in_=src), writes=[t_smld[i_]])
        eposT = A.alloc([128], F32); enegT = A.alloc([128], F32)
        io1 = A.alloc([128], F32); io2 = A.alloc([128], F32)
        gnb = A.alloc([512], F32)
        t_tabl = P.tiles_n(5, "tabl")
        t_gc = P.tile("gc")
        for i_, (dst, src) in enumerate(((eposT, epos), (enegT, eneg), (io1, iota1), (io2, iota2),
                                         (gnb, gn_gain.partition_broadcast(128)))):
            P.dma("sp", lambda e, dst=dst, src=src: e.dma_start(out=dst, in_=src), writes=[t_tabl[i_]])

        P.op("act", lambda e: e.activation(out=sm(32, 56), in_=sm(0, 24), func=AF.Exp, scale=-1.0),
             reads=t_smld, writes=[t_small])
        P.op("act", lambda e: e.activation(out=sm(32, 56), in_=sm(32, 56), func=AF.Ln, bias=cst[:, 1:2], scale=1.0),
             reads=[t_small, t_cst], writes=[t_small])
        P.op("dve", lambda e: e.tensor_scalar(out=sm(32, 56), in0=sm(32, 56), scalar1=-1.0, scalar2=None, op0=ALU.mult),
             reads=[t_small], writes=[t_small])
        P.op("dve", lambda e: e.tensor_scalar(out=sm(25, 26), in0=sm(24, 25), scalar1=-1.0, scalar2=1.0,
                                              op0=ALU.mult, op1=ALU.add), reads=[t_small], writes=[t_small])
        P.op("dve", lambda e: e.tensor_scalar(out=sm(56, 64), in0=sm(32, 40), scalar1=sm(24, 25), scalar2=None,
                                              op0=ALU.mult), reads=[t_small], writes=[t_small])
        P.op("dve", lambda e: e.scalar_tensor_tensor(out=sm(56, 64), in0=sm(40, 48), scalar=sm(25, 26), in1=sm(56, 64),
                                                     op0=ALU.mult, op1=ALU.add), reads=[t_small], writes=[t_small])
        P.op("act", lambda e: e.activation(out=sm(64, 72), in_=sm(32, 40), func=AF.Exp, scale=sm(96, 97)),
             reads=[t_small], writes=[t_small])
        P.op("act", lambda e: e.activation(out=sm(72, 80), in_=sm(40, 48), func=AF.Exp, scale=sm(97, 98)),
             reads=[t_small], writes=[t_small])
        P.op("act", lambda e: e.activation(out=sm(80, 88), in_=sm(48, 56), func=AF.Exp, scale=128.0),
             reads=[t_small], writes=[t_small])
        gcfB = A.alloc([4, 64], F32); gcbB = A.alloc([4, 64], F32)
        P.op("dve", lambda e: e.tensor_copy(out=gcfB, in_=bc_last(sm(80, 84), 64)), reads=[t_small], writes=[t_gc])
        P.op("dve", lambda e: e.tensor_copy(out=gcbB, in_=bc_last(sm(84, 88), 64)), reads=[t_small], writes=[t_gc])
        Mmask = A.alloc([8, 128], BF16)
        mtmp = A.alloc([128], F32)
        t_M = P.tile("M"); t_mtmp = P.tile("mtmp")
        for h in range(8):
            P.op("dve", lambda e, h=h: e.tensor_scalar(out=mtmp, in0=eposT, scalar1=sm(32 + h, 33 + h), scalar2=None,
                                                       op0=ALU.mult), reads=[t_small] + t_tabl, writes=[t_mtmp])
            P.op("dve", lambda e, h=h: e.scalar_tensor_tensor(out=mtmp, in0=enegT, scalar=sm(40 + h, 41 + h), in1=mtmp,
                                                              op0=ALU.mult, op1=ALU.add),
                 reads=[t_small, t_mtmp] + t_tabl, writes=[t_mtmp])
            P.op("act", lambda e, h=h: e.activation(out=Mmask[:, h, :], in_=mtmp, func=AF.Exp, bias=cst[:, 0:1], scale=1.0),
                 reads=[t_mtmp, t_cst], writes=[t_M])
        Df = A.alloc([4, 128], BF16); Db = A.alloc([4, 128], BF16)
        t_D = P.tile("D")
        for pr in range(4):
            P.op("act", lambda e, pr=pr: e.activation(out=Df[:, pr, :], in_=io1, func=AF.Exp, bias=cst[:, 0:1],
                                                      scale=sm(48 + pr, 49 + pr)), reads=[t_small, t_cst] + t_tabl, writes=[t_D])
            P.op("act", lambda e, pr=pr: e.activation(out=Db[:, pr, :], in_=io2, func=AF.Exp, bias=cst[:, 0:1],
                                                      scale=sm(52 + pr, 53 + pr)), reads=[t_small, t_cst] + t_tabl, writes=[t_D])
        if "misc" in dbg:
            P.dma("sp", lambda e: e.dma_start(out=dbg["misc"][:, 0:56], in_=sm(32, 88)), reads=[t_small], sem_tile=P.tile("dm"))

        if stage == "ret0":
            P.final_wait("sp")
            P.emit_all()
            return nc
        qT = A.alloc([4, TOK], BF16); kT = A.alloc([4, TOK], BF16)
        vS = A.alloc([NT, 512], BF16); GS = A.alloc([NT, 512], BF16)
        SfB = A.alloc([NT, 256], BF16); SbB = A.alloc([NT, 256], BF16)
        UbS = A.alloc([NT, 256], BF16)
        Sst = A.alloc([2, 256], F32)
        t_qT = P.tiles_n(NT, "qT"); t_kT = P.tiles_n(NT, "kT"); t_v = P.tiles_n(NT, "v"); t_G = P.tiles_n(NT, "G")
        t_SfB = P.tiles_n(NT, "SfB"); t_SbB = P.tiles_n(NT, "SbB"); t_Ub = P.tiles_n(NT, "Ub")
        t_Sf = P.tile("Sf"); t_Sb = P.tile("Sb")
        A.mark()
        xs = [A.alloc([8, 256], BF16) for _ in range(2)]; t_xs = P.tiles_n(2, "xs")
        q_tm = [A.alloc([512], BF16) for _ in range(2)]; k_tm = [A.alloc([512], BF16) for _ in range(2)]
        t_qtm = P.tiles_n(2, "qtm"); t_ktm = P.tiles_n(2, "ktm")
        rA = [A.alloc([512], F32) for _ in range(2)]; rB = [A.alloc([512], F32) for _ in range(2)]
        t_rA = P.tiles_n(2, "rA"); t_rB = P.tiles_n(2, "rB")
        vfb = [A.alloc([4, 256], BF16) for _ in range(2)]; t_vfb = P.tiles_n(2, "vfb")
        wo = [A.alloc([8], F32) for _ in range(2)]; t_wo = P.tiles_n(2, "wo")
        Ud = [A.alloc([4, 2, 64], F32) for _ in range(2)]; t_Ud = P.tiles_n(2, "Ud")
        Uf = [A.alloc([2, 512], F32) for _ in range(2)]; t_Uf = P.tiles_n(2, "Uf")

        def v3(ap):
            return ap.rearrange("p (h d) -> p h d", d=64)

        def inproj(ps_i, xs_ap, wcols, reads):
            def f(e):
                ins = None
                for kc in range(8):
                    ins = e.matmul(PS[ps_i], lhsT=xs_ap(kc), rhs=w_ret[:, kc, wcols * 512:(wcols + 1) * 512],
                                   start=(kc == 0), stop=(kc == 7))
                return ins
            P.op("pe", f, reads=reads, writes=[t_ps[ps_i]])

        def rotary(ps_i, cc, ss, t, dst, t_dst, par):
            src = v3(PS[ps_i])
            a3 = v3(rA[par]); b3 = v3(rB[par])
            P.op("dve", lambda e: e.tensor_tensor(out=a3, in0=src, in1=bc_mid(cc[:, t, :], 8), op=ALU.mult),
                 reads=[t_ps[ps_i]] + t_rotl, writes=[t_rA[par]])
            P.op("dve", lambda e: e.tensor_tensor(out=b3[:, :, 0:32], in0=src[:, :, 32:64],
                                                  in1=bc_mid(ss[:, t, 0:32], 8), op=ALU.mult),
                 reads=[t_ps[ps_i]] + t_rotl, writes=[t_rB[par]])
            P.op("dve", lambda e: e.tensor_tensor(out=b3[:, :, 32:64], in0=src[:, :, 0:32],
                                                  in1=bc_mid(ss[:, t, 32:64], 8), op=ALU.mult),
                 reads=[t_ps[ps_i]] + t_rotl, writes=[t_rB[par]])
            P.op("pool", lambda e: e.tensor_tensor(out=dst, in0=rA[par], in1=rB[par], op=ALU.add),
                 reads=[t_rA[par], t_rB[par]], writes=[t_dst])

        sinB = [PS[4 + pr][:, 0:128] for pr in range(4)]
        for t in range(NT):
            ch, sub = divmod(t, 2)
            par = t % 2
            if sub == 0:
                P.dma("pool", lambda e, ch=ch: e.dma_start(
                    out=xs[ch % 2], in_=xo[:, ch * 256:(ch + 1) * 256].rearrange("(kc k) n -> k kc n", k=128)),
                    writes=[t_xs[ch % 2]])
            xs_ap = lambda kc, ch=ch, sub=sub: xs[ch % 2][:, kc, sub * 128:(sub + 1) * 128]
            inproj(0, xs_ap, 1, [t_xs[ch % 2], t_wret[1]])
            inproj(1, xs_ap, 2, [t_xs[ch % 2], t_wret[2]])
            rotary(0, ccX, ssX, t, k_tm[par], t_ktm[par], par)
            P.op("act", lambda e, t=t, par=par: e.activation(out=wo[par], in_=sm(56, 64), func=AF.Exp,
                                                             scale=sm(100 + t, 101 + t)),
                 reads=[t_small], writes=[t_wo[par]])
            P.op("dve", lambda e, par=par: e.tensor_tensor(
                out=vfb[par].rearrange("p a b -> p (a b)")[:, 0:512].rearrange("p (h d) -> p h d", d=64),
                in0=v3(PS[1]), in1=bc_last(wo[par], 64), op=ALU.mult),
                reads=[t_ps[1], t_wo[par]], writes=[t_vfb[par]])

            def fU(e, t=t, par=par):
                ins = None
                vw = vfb[par].rearrange("p a b -> p (a b)")
                for pr in range(4):
                    ins = e.matmul(sinB[pr], lhsT=k_tm[par][:, pr * 128:(pr + 1) * 128],
                                   rhs=vw[:, pr * 128:(pr + 1) * 128], start=(t == 0), stop=(t == NT - 1))
                return ins
            P.op("pe", fU, reads=[t_ktm[par], t_vfb[par]], writes=t_ps[4:8])
        S3 = Sst.rearrange("p a (b c) -> p a b c", c=64)
        for hp in range(2):
            pl, ph = hp * 64, hp * 64 + 64
            for pr in range(4):
                P.op("dve", lambda e, pl=pl, ph=ph, pr=pr: e.tensor_scalar(
                    out=S3[pl:ph, 0, pr, :], in0=sinB[pr][pl:ph, pl:ph], scalar1=small[pl:ph, 24:25], scalar2=None, op0=ALU.mult),
                    reads=[t_ps[4 + pr], t_small], writes=[t_Sf])
                P.op("dve", lambda e, pl=pl, ph=ph, pr=pr: e.tensor_scalar(
                    out=S3[pl:ph, 1, pr, :], in0=sinB[pr][pl:ph, pl:ph], scalar1=small[pl:ph, 25:26], scalar2=None, op0=ALU.mult),
                    reads=[t_ps[4 + pr], t_small], writes=[t_Sb])

        if stage == "ret1":
            P.final_wait("sp")
            P.emit_all()
            return nc
        load_rot(cc_own, ss_own)
        for t in range(NT):
            ch, sub = divmod(t, 2)
            par = t % 2
            if sub == 0:
                P.dma("pool", lambda e, ch=ch: e.dma_start(
                    out=xs[ch % 2], in_=xT[:, ch * 256:(ch + 1) * 256].rearrange("(kc k) n -> k kc n", k=128)),
                    writes=[t_xs[ch % 2]])
            xs_ap = lambda kc, ch=ch, sub=sub: xs[ch % 2][:, kc, sub * 128:(sub + 1) * 128]
            for g in range(4):
                inproj(g, xs_ap, g, [t_xs[ch % 2], t_wret[g]])
            rotary(0, ccO, ssO, t, q_tm[par], t_qtm[par], par)
            rotary(1, ccO, ssO, t, k_tm[par], t_ktm[par], par)
            P.op("act", lambda e, t=t: e.activation(func=AF.Copy, out=vS[:, t, :], in_=PS[2]), reads=[t_ps[2]], writes=[t_v[t]])
            vfl = vfb[par].rearrange("p a b -> p (a b)")
            for dr, col in ((0, 64), (1, 72)):
                P.op("dve", lambda e, dr=dr, col=col, vfl=vfl: e.tensor_tensor(
                    out=v3(vfl[:, dr * 512:(dr + 1) * 512]), in0=v3(PS[2]), in1=bc_last(small[:, col:col + 8], 64),
                    op=ALU.mult), reads=[t_ps[2], t_small], writes=[t_vfb[par]])
            P.op("act", lambda e, t=t: e.activation(out=GS[:, t, :], in_=PS[3], func=AF.Silu),
                 reads=[t_ps[3]], writes=[t_G[t]])
            P.op("pool", lambda e, t=t: e.tensor_tensor(out=GS[:, t, :], in0=GS[:, t, :], in1=gnb, op=ALU.mult),
                 reads=[t_G[t]] + t_tabl, writes=[t_G[t]])

            def fT(e, par=par):
                ins = None
                for pr in range(4):
                    ins = e.transpose(out=PSB[4][:, pr * 128:(pr + 1) * 128], in_=q_tm[par][:, pr * 128:(pr + 1) * 128],
                                      identity=ident_b)
                for pr in range(4):
                    ins = e.transpose(out=PSB[7][:, pr * 128:(pr + 1) * 128],
                                      in_=k_tm[par][:, pr * 128:(pr + 1) * 128], identity=ident_b)
                return ins
            P.op("pe", fT, reads=[t_qtm[par], t_ktm[par], t_ident], writes=[t_ps[4], t_ps[7]])
            P.op("dve", lambda e, t=t: e.tensor_copy(out=qT[:, :, t * 128:(t + 1) * 128],
                                                     in_=PSB[4][:, 0:512].rearrange("p (a b) -> p a b", b=128)),
                 reads=[t_ps[4]], writes=[t_qT[t]])
            P.op("dve", lambda e, t=t: e.tensor_copy(out=kT[:, :, t * 128:(t + 1) * 128],
                                                     in_=PSB[7][:, 0:512].rearrange("p (a b) -> p a b", b=128)),
                 reads=[t_ps[7]], writes=[t_kT[t]])

            def fU2(e, par=par):
                ins = None
                vfl = vfb[par].rearrange("p a b -> p (a b)")
                for pr in range(4):
                    bank = PS[5 + pr // 2]
                    for dr in range(2):
                        c0 = (pr % 2) * 256 + dr * 128
                        ins = e.matmul(bank[:, c0:c0 + 128], lhsT=k_tm[par][:, pr * 128:(pr + 1) * 128],
                                       rhs=vfl[:, dr * 512 + pr * 128:dr * 512 + (pr + 1) * 128], start=True, stop=True)
                return ins
            P.op("pe", fU2, reads=[t_ktm[par], t_vfb[par]], writes=[t_ps[5], t_ps[6]])
            P.op("act", lambda e, t=t: e.activation(func=AF.Copy, out=SfB[:, t, :], in_=Sst[:, 0, :]), reads=[t_Sf], writes=[t_SfB[t]])
            P.op("dve", lambda e: e.tensor_tensor(out=Sst[:, 0, :], in0=Sst[:, 0, :],
                                                  in1=gcfB.rearrange("p a b -> p (a b)"), op=ALU.mult),
                 reads=[t_Sf, t_gc], writes=[t_Sf])
            for bk in range(2):
                P.op("dve", lambda e, bk=bk, par=par: e.tensor_copy(out=Uf[par][:, bk, :], in_=PS[5 + bk]),
                     reads=[t_ps[5 + bk]], writes=[t_Uf[par]])
                Ub4 = Uf[par][:, bk, :].rearrange("p (a s c) -> p a s c", s=2, c=128)
                ud = Ud[par][:, 2 * bk:2 * bk + 2, :, :]
                for a_ in range(2):
                    P.op("dve", lambda e, Ub4=Ub4, ud=ud, a_=a_: e.tensor_scalar(
                        out=ud[:, a_], in0=Ub4[:, a_, :, 64:128], scalar1=cst[:, 6:7], scalar2=None, op0=ALU.mult),
                        reads=[t_Uf[par], t_cst], writes=[t_Ud[par]])
                    P.op("dve", lambda e, Ub4=Ub4, ud=ud, a_=a_: e.scalar_tensor_tensor(
                        out=ud[:, a_], in0=Ub4[:, a_, :, 0:64], scalar=cst[:, 5:6], in1=ud[:, a_], op0=ALU.mult, op1=ALU.add),
                        reads=[t_Uf[par], t_cst, t_Ud[par]], writes=[t_Ud[par]])
            P.op("dve", lambda e, par=par: e.tensor_tensor(out=S3[:, 0], in0=S3[:, 0], in1=Ud[par][:, :, 0, :], op=ALU.add),
                 reads=[t_Sf, t_Ud[par]], writes=[t_Sf])
            P.op("pool", lambda e, par=par, t=t: e.tensor_copy(out=UbS[:, t, :].rearrange("p (a c) -> p a c", c=64),
                                                               in_=Ud[par][:, :, 1, :]),
                 reads=[t_Ud[par]], writes=[t_Ub[t]])
        for t in range(NT - 1, -1, -1):
            P.op("act", lambda e, t=t: e.activation(func=AF.Copy, out=SbB[:, t, :], in_=Sst[:, 1, :]), reads=[t_Sb], writes=[t_SbB[t]])
            if t > 0:
                P.op("dve", lambda e: e.tensor_tensor(out=Sst[:, 1, :], in0=Sst[:, 1, :],
                                                      in1=gcbB.rearrange("p a b -> p (a b)"), op=ALU.mult),
                     reads=[t_Sb, t_gc], writes=[t_Sb])
                P.op("dve", lambda e, t=t: e.tensor_tensor(out=Sst[:, 1, :], in0=Sst[:, 1, :], in1=UbS[:, t, :], op=ALU.add),
                     reads=[t_Sb, t_Ub[t]], writes=[t_Sb])

        P.barrier()
        A.release()
        if stage == "ret2":
            P.final_wait("sp")
            P.emit_all()
            return nc
        sT_sb = [A.alloc([8, 128], BF16) for _ in range(2)]; t_sT = P.tiles_n(2, "sT")
        qfT = [A.alloc([4, 128], BF16) for _ in range(2)]; qbT = [A.alloc([4, 128], BF16) for _ in range(2)]
        t_qf = P.tiles_n(2, "qf"); t_qb = P.tiles_n(2, "qb")
        sq = [A.alloc([512], F32) for _ in range(2)]; t_sq = P.tiles_n(2, "sq")
        gst = [A.alloc([64], F32) for _ in range(2)]; t_gst = P.tiles_n(2, "gst")
        yc = [A.alloc([512], F32) for _ in range(2)]; t_yc = P.tiles_n(2, "yc")
        retb = [A.alloc([512], BF16) for _ in range(2)]; t_retb = P.tiles_n(2, "retb")
        retf = [A.alloc([512], F32) for _ in range(2)] if "ret" in dbg else None
        ret_pending = []
        for t in range(NT):
            par = t % 2
            b0 = 4 * par
            tsl = slice(t * 128, (t + 1) * 128)

            def fS(e, b0=b0, tsl=tsl):
                ins = None
                for h in range(8):
                    pr, hp = divmod(h, 2)
                    pl, ph = hp * 64, hp * 64 + 64
                    ins = e.matmul(PS[b0 + hp][:, pr * 128:pr * 128 + 128], lhsT=kT[pl:ph, pr, tsl],
                                   rhs=qT[pl:ph, pr, tsl], start=True, stop=True)
                return ins
            P.op("pe", fS, reads=[t_kT[t], t_qT[t]], writes=[t_ps[b0], t_ps[b0 + 1]])
            for bk in range(2):
                P.op("dve", lambda e, bk=bk, b0=b0, par=par: e.tensor_tensor(
                    out=sT_sb[par].rearrange("p (a s) b -> p a s b", s=2)[:, :, bk, :],
                    in0=PS[b0 + bk].rearrange("p (a b) -> p a b", b=128),
                    in1=Mmask.rearrange("p (a s) b -> p a s b", s=2)[:, :, bk, :], op=ALU.mult),
                    reads=[t_ps[b0 + bk], t_M], writes=[t_sT[par]])
            P.op("pool", lambda e, par=par, tsl=tsl: e.tensor_tensor(out=qfT[par], in0=qT[:, :, tsl], in1=Df, op=ALU.mult),
                 reads=[t_qT[t], t_D], writes=[t_qf[par]])
            P.op("pool", lambda e, par=par, tsl=tsl: e.tensor_tensor(out=qbT[par], in0=qT[:, :, tsl], in1=Db, op=ALU.mult),
                 reads=[t_qT[t], t_D], writes=[t_qb[par]])

            def ret_tail(b0=b0, par=par, t=t, tsl=tsl):
                def fY(e, b0=b0, par=par, t=t):
                    ins = None
                    for h in range(8):
                        pr, hp = divmod(h, 2)
                        pl, ph = hp * 64, hp * 64 + 64
                        o = PS[b0 + 2][:, h * 64:(h + 1) * 64]
                        e.matmul(o, lhsT=sT_sb[par][:, h, :], rhs=vS[:, t, h * 64:(h + 1) * 64], start=True, stop=False)
                        e.matmul(o, lhsT=qfT[par][pl:ph, pr, :], rhs=SfB[pl:ph, t, pr * 64:(pr + 1) * 64], start=False, stop=False)
                        ins = e.matmul(o, lhsT=qbT[par][pl:ph, pr, :], rhs=SbB[pl:ph, t, pr * 64:(pr + 1) * 64], start=False, stop=True)
                    return ins
                P.op("pe", fY, reads=[t_sT[par], t_v[t], t_qf[par], t_qb[par], t_SfB[t], t_SbB[t]], writes=[t_ps[b0 + 2]])
                y3 = v3(PS[b0 + 2])
                g = gst[par]
                P.op("dve", lambda e, y3=y3, g=g: e.tensor_reduce(out=g[:, 0:8], in_=y3, axis=AX.X, op=ALU.add),
                     reads=[t_ps[b0 + 2]], writes=[t_gst[par]])
                P.op("act", lambda e, b0=b0, par=par: e.activation(out=sq[par], in_=PS[b0 + 2], func=AF.Square),
                     reads=[t_ps[b0 + 2]], writes=[t_sq[par]])
                P.op("dve", lambda e, g=g, par=par: e.tensor_reduce(out=g[:, 8:16], in_=v3(sq[par]), axis=AX.X, op=ALU.add),
                     reads=[t_sq[par]], writes=[t_gst[par]])
                P.op("dve", lambda e, g=g: e.tensor_scalar(out=g[:, 16:24], in0=g[:, 0:8], scalar1=1.0 / 64, scalar2=None,
                                                           op0=ALU.mult), reads=[t_gst[par]], writes=[t_gst[par]])
                P.op("dve", lambda e, g=g: e.tensor_tensor(out=g[:, 24:32], in0=g[:, 16:24], in1=g[:, 16:24], op=ALU.mult),
                     reads=[t_gst[par]], writes=[t_gst[par]])
                P.op("dve", lambda e, g=g: e.scalar_tensor_tensor(out=g[:, 32:40], in0=g[:, 8:16], scalar=1.0 / 64,
                                                                  in1=g[:, 24:32], op0=ALU.mult, op1=ALU.subtract),
                     reads=[t_gst[par]], writes=[t_gst[par]])
                P.op("act", lambda e, g=g: e.activation(out=g[:, 40:48], in_=g[:, 32:40], func=AF.Ln, bias=cst[:, 3:4], scale=1.0),
                     reads=[t_gst[par], t_cst], writes=[t_gst[par]])
                P.op("act", lambda e, g=g: e.activation(out=g[:, 40:48], in_=g[:, 40:48], func=AF.Exp, scale=-0.5),
                     reads=[t_gst[par]], writes=[t_gst[par]])
                P.op("dve", lambda e, g=g, y3=y3, par=par: e.tensor_tensor(out=v3(yc[par]), in0=y3, in1=bc_last(g[:, 16:24], 64),
                                                                           op=ALU.subtract),
                     reads=[t_ps[b0 + 2], t_gst[par]], writes=[t_yc[par]])
                P.op("pool", lambda e, g=g, par=par: e.tensor_tensor(out=v3(yc[par]), in0=v3(yc[par]), in1=bc_last(g[:, 40:48], 64),
                                                                     op=ALU.mult), reads=[t_yc[par], t_gst[par]], writes=[t_yc[par]])
                P.op("pool", lambda e, par=par, t=t: e.tensor_tensor(out=retb[par], in0=yc[par], in1=GS[:, t, :], op=ALU.mult),
                     reads=[t_yc[par], t_G[t]], writes=[t_retb[par]])
                if "ret" in dbg:
                    P.op("dve", lambda e, par=par: e.tensor_copy(out=retf[par], in_=retb[par]), reads=[t_retb[par]], writes=[t_yc[par]])
                    P.dma("sp", lambda e, par=par, tsl=tsl: e.dma_start(out=dbg["ret"][tsl, :], in_=retf[par]),
                          reads=[t_yc[par]], sem_tile=t_yc[par])

                P.dma("sp", lambda e, par=par, tsl=tsl: e.dma_start(out=cat[tsl, 0:512], in_=retb[par]),
                      reads=[t_retb[par]], writes=[t_cat[0][t]], sem_tile=t_retb[par])

            ret_pending.append(ret_tail)
            if len(ret_pending) > 1:
                ret_pending.pop(0)()
        while ret_pending:
            ret_pending.pop(0)()
        P.barrier()
        A.release()
        if stage == "ret":
            P.final_wait("sp")
            P.emit_all()
            return nc

        A.mark()
        NKB = 20
        w_na = A.alloc([8, 1536], BF16); t_wna = P.tiles_n(3, "wna")
        for g in range(3):
            P.dma("pool", lambda e, g=g: e.dma_start(
                out=w_na[:, :, g * 512:(g + 1) * 512],
                in_=w_in[:, 2048 + g * 512:2048 + (g + 1) * 512].rearrange("(kc k) n -> k kc n", k=128)), writes=[t_wna[g]])
        nqT = A.alloc([4, TOK], BF16); nkT = A.alloc([4, NKB * 128], BF16)
        Vaug = A.alloc([NKB, 8 * 65], BF16)
        na_sb = A.alloc([NT, 512], BF16)
        t_nq = P.tiles_n(NT, "nq"); t_nk = P.tiles_n(NKB, "nk"); t_V = P.tiles_n(NKB, "V"); t_na = P.tiles_n(NT, "na")
        V4 = Vaug.rearrange("p a (h c) -> p a h c", c=65)
        for kb in range(NKB):
            P.op("pool", lambda e, kb=kb: e.memset(V4[:, kb, :, 64:65], 1.0), writes=[t_V[kb]])
        nxs = [A.alloc([8, 256], BF16) for _ in range(2)]; t_nxs = P.tiles_n(2, "xsn")
        nq_tm = [A.alloc([512], BF16) for _ in range(2)]; nk_tm = [A.alloc([512], BF16) for _ in range(2)]
        t_nqtm = P.tiles_n(2, "nqtm"); t_nktm = P.tiles_n(2, "nktm")

        def na_inproj(ps_i, xs_ap, g, reads):
            def f(e):
                ins = None
                for kc in range(8):
                    ins = e.matmul(PS[ps_i], lhsT=xs_ap(kc), rhs=w_na[:, kc, g * 512:(g + 1) * 512],
                                   start=(kc == 0), stop=(kc == 7))
                return ins
            P.op("pe", f, reads=reads, writes=[t_ps[ps_i]])

        chunks = [xh[:, 0:256]] + [xT[:, c * 256:(c + 1) * 256] for c in range(8)] + [xh[:, 256:512]]
        for kb in range(NKB):
            ch, sub = divmod(kb, 2)
            par = kb % 2
            own = 2 <= kb < 18
            if sub == 0:
                P.dma("pool", lambda e, ch=ch: e.dma_start(
                    out=nxs[ch % 2], in_=chunks[ch].rearrange("(kc k) n -> k kc n", k=128)), writes=[t_nxs[ch % 2]])
            xs_ap = lambda kc, ch=ch, sub=sub: nxs[ch % 2][:, kc, sub * 128:(sub + 1) * 128]
            if own:
                t = kb - 2
                na_inproj(0, xs_ap, 0, [t_nxs[ch % 2], t_wna[0]])
                P.op("act", lambda e, par=par: e.activation(func=AF.Copy, out=nq_tm[par], in_=PS[0]), reads=[t_ps[0]], writes=[t_nqtm[par]])

                def fTq(e, par=par):
                    ins = None
                    for pr in range(4):
                        ins = e.transpose(out=PSB[3][:, pr * 128:(pr + 1) * 128], in_=nq_tm[par][:, pr * 128:(pr + 1) * 128],
                                          identity=ident_b)
                    return ins
                P.op("pe", fTq, reads=[t_nqtm[par], t_ident], writes=[t_ps[3]])
                P.op("dve", lambda e, t=t: e.tensor_copy(out=nqT[:, :, t * 128:(t + 1) * 128],
                                                         in_=PSB[3][:, 0:512].rearrange("p (a b) -> p a b", b=128)),
                     reads=[t_ps[3]], writes=[t_nq[t]])
            na_inproj(1, xs_ap, 1, [t_nxs[ch % 2], t_wna[1]])
            na_inproj(2, xs_ap, 2, [t_nxs[ch % 2], t_wna[2]])
            P.op("act", lambda e, par=par: e.activation(func=AF.Copy, out=nk_tm[par], in_=PS[1]), reads=[t_ps[1]], writes=[t_nktm[par]])

            def fTk(e, par=par):
                ins = None
                for pr in range(4):
                    ins = e.transpose(out=PSB[4][:, pr * 128:(pr + 1) * 128], in_=nk_tm[par][:, pr * 128:(pr + 1) * 128],
                                      identity=ident_b)
                return ins
            P.op("pe", fTk, reads=[t_nktm[par], t_ident], writes=[t_ps[4]])
            P.op("dve", lambda e, kb=kb: e.tensor_copy(out=nkT[:, :, kb * 128:(kb + 1) * 128],
                                                       in_=PSB[4][:, 0:512].rearrange("p (a b) -> p a b", b=128)),
                 reads=[t_ps[4]], writes=[t_nk[kb]])
            P.op("dve", lambda e, kb=kb: e.tensor_copy(out=V4[:, kb, :, 0:64], in_=v3(PS[2])),
                 reads=[t_ps[2]], writes=[t_V[kb]])
        Est = [A.alloc([NA_BLOCKS * 128], F32) for _ in range(2)]; t_Est = P.tiles_n(2, "Est")
        Ebf = [A.alloc([NA_BLOCKS, 128], BF16) for _ in range(2)]; t_Ebf = P.tiles_n(2, "Ebf")
        nes = [A.alloc([768], BF16) for _ in range(2)]; t_nes = P.tiles_n(2, "nes")
        pTt = [A.alloc([768], BF16) for _ in range(2)]; t_pT = P.tiles_n(2, "pT")
        rcp = [A.alloc([2], F32) for _ in range(4)]; t_rcp = P.tiles_n(4, "rcp")
        na_pending = []
        for h in range(8):
            hp2 = h % 2
            pr, hp = divmod(h, 2)
            pl, ph = hp * 64, hp * 64 + 64
            P.dma("sp", lambda e, h=h, hp2=hp2: e.dma_start(out=Est[hp2], in_=natab[h]), writes=[t_Est[hp2]])
            P.op("act", lambda e, hp2=hp2: e.activation(out=Ebf[hp2].rearrange("p a b -> p (a b)"), in_=Est[hp2], func=AF.Exp),
                 reads=[t_Est[hp2]], writes=[t_Ebf[hp2]])
            for i in range(NT):
                kt0, nk, b0 = NA_TYPES[i]
                n = h * NT + i
                par = n % 2
                bX, bY = 2 * par, 2 * par + 1
                bO = 4 + n % 4
                qsl = slice(i * 128, (i + 1) * 128)

                def fS(e, kt0=kt0, nk=nk, bX=bX, bY=bY, pl=pl, ph=ph, pr=pr, qsl=qsl):
                    ins = None
                    for idx in range(nk):
                        kt = kt0 + idx
                        bank = PS[bX] if idx < 4 else PS[bY]
                        ins = e.matmul(bank[:, (idx % 4) * 128:(idx % 4) * 128 + 128], lhsT=nkT[pl:ph, pr, kt * 128:(kt + 1) * 128],
                                       rhs=nqT[pl:ph, pr, qsl], start=True, stop=True)
                    return ins
                P.op("pe", fS, reads=[t_nq[i]] + t_nk[kt0:kt0 + nk], writes=[t_ps[bX], t_ps[bY]])
                P.op("act", lambda e, par=par, bX=bX: e.activation(out=nes[par][:, 0:512], in_=PS[bX], func=AF.Exp, scale=0.125),
                     reads=[t_ps[bX]], writes=[t_nes[par]])
                w2 = (nk - 4) * 128
                P.op("act", lambda e, par=par, bY=bY, w2=w2: e.activation(out=nes[par][:, 512:512 + w2], in_=PS[bY][:, 0:w2],
                                                                        func=AF.Exp, scale=0.125),
                     reads=[t_ps[bY]], writes=[t_nes[par]])
                P.op("pool", lambda e, par=par, nk=nk, b0=b0, hp2=hp2: e.tensor_tensor(
                    out=pTt[par][:, 0:nk * 128], in0=nes[par][:, 0:nk * 128],
                    in1=Ebf[hp2][:, b0:b0 + nk, :].rearrange("p a b -> p (a b)"), op=ALU.mult),
                    reads=[t_nes[par], t_Ebf[hp2]], writes=[t_pT[par]])

                def na_tail(kt0=kt0, nk=nk, par=par, bO=bO, h=h, n=n, i=i):
                    def fO(e):
                        ins = None
                        for idx in range(nk):
                            ins = e.matmul(PS[bO][:, 0:65], lhsT=pTt[par][:, idx * 128:(idx + 1) * 128],
                                           rhs=Vaug[:, kt0 + idx, h * 65:(h + 1) * 65], start=(idx == 0), stop=(idx == nk - 1))
                        return ins
                    P.op("pe", fO, reads=[t_pT[par]] + t_V[kt0:kt0 + nk], writes=[t_ps[bO]])
                    r4 = n % 4
                    P.op("dve", lambda e: e.reciprocal(out=rcp[r4][:, 0:1], in_=PS[bO][:, 64:65]),
                         reads=[t_ps[bO]], writes=[t_rcp[r4]])
                    P.op("dve", lambda e: e.tensor_scalar(
                        out=na_sb[:, i, h * 64:(h + 1) * 64], in0=PS[bO][:, 0:64], scalar1=rcp[r4][:, 0:1], scalar2=None, op0=ALU.mult),
                        reads=[t_ps[bO], t_rcp[r4]], writes=[t_na[i]])
                na_pending.append(na_tail)
                if len(na_pending) > 1:
                    na_pending.pop(0)()
        while na_pending:
            na_pending.pop(0)()
        naf = [A.alloc([512], F32) for _ in range(2)] if "na" in dbg else None
        t_naf = P.tiles_n(2, "naf")
        for i in range(NT):
            tsl = slice(i * 128, (i + 1) * 128)
            P.dma("sp", lambda e, i=i, tsl=tsl: e.dma_start(out=cat[tsl, 512:1024], in_=na_sb[:, i, :]),
                  reads=[t_na[i]], writes=[t_cat[1][i]], sem_tile=P.tile("nast%d" % (i % 4)) if i >= 4 else P.tile("nast%d" % i))
            if "na" in dbg:
                P.op("dve", lambda e, i=i: e.tensor_copy(out=naf[i % 2], in_=na_sb[:, i, :]), reads=[t_na[i]], writes=[t_naf[i % 2]])
                P.dma("sp", lambda e, i=i, tsl=tsl: e.dma_start(out=dbg["na"][tsl, :], in_=naf[i % 2]),
                      reads=[t_naf[i % 2]], sem_tile=t_naf[i % 2])
        P.barrier()
        A.release()
        if stage == "na":
            P.final_wait("sp")
            P.emit_all()
            return nc

        x1T = A.alloc([8, TOK], BF16)
        t_x1T = P.tiles_n(NT, "x1T")
        t_GT = P.tiles_n(NT, "GT"); t_base = P.tiles_n(NT, "base")
        A.mark()
        w_o = A.alloc([8, 1024], BF16); w_pgt = A.alloc([8, 1024], BF16); w_ppt = A.alloc([2, 1024], BF16)
        pTb = A.alloc([2, TOK], BF16)
        w_rt = A.alloc([8, 64], F32)
        g1 = A.alloc([1024], F32); b1 = A.alloc([1024], F32); rb = A.alloc([64], F32)
        t_wl = P.tiles_n(9, "mw")
        P.dma("pool", lambda e: e.dma_start(out=w_o, in_=w_out.rearrange("(kc k) n -> k kc n", k=128)), writes=[t_wl[0]])
        P.dma("pool", lambda e: e.dma_start(out=pTb, in_=pT.rearrange("(kc k) n -> k kc n", k=128)), writes=[t_wl[1]])
        P.dma("pool", lambda e: e.dma_start(out=w_ppt, in_=w_pp.rearrange("(kc k) n -> k kc n", k=128)), writes=[t_wl[2]])
        P.dma("pool", lambda e: e.dma_start(out=w_pgt, in_=w_pg.rearrange("(kc k) n -> k kc n", k=128)), writes=[t_wl[3]])
        P.dma("sp", lambda e: e.dma_start(out=w_rt, in_=w_router.rearrange("(kc k) n -> k kc n", k=128)), writes=[t_wl[4]])
        P.dma("sp", lambda e: e.dma_start(out=g1, in_=ln1_g.partition_broadcast(128)), writes=[t_wl[5]])
        P.dma("sp", lambda e: e.dma_start(out=b1, in_=ln1_b.partition_broadcast(128)), writes=[t_wl[6]])
        P.dma("sp", lambda e: e.dma_start(out=rb, in_=rbias.partition_broadcast(128)), writes=[t_wl[7]])
        catb = [A.alloc([1024], BF16) for _ in range(2)]; t_catb = P.tiles_n(2, "catb")
        catT = [A.alloc([8, 128], BF16) for _ in range(2)]; t_catT = P.tiles_n(2, "catT")
        xrt = [A.alloc([1024], F32) for _ in range(2)]; t_xrt = P.tiles_n(2, "xrt")
        zt = [A.alloc([1024], F32) for _ in range(2)]; t_zt = P.tiles_n(2, "zt")
        x1t = [A.alloc([1024], F32) for _ in range(2)]; t_x1t = P.tiles_n(2, "x1t")
        x1T32 = [A.alloc([8, 128], F32) for _ in range(2)]; t_x1T32 = P.tiles_n(2, "x1T32")
        lst = [A.alloc([32], F32) for _ in range(2)]; t_lst = P.tiles_n(2, "lst")
        rt = [A.alloc([768], F32) for _ in range(2)]; t_rt = P.tiles_n(2, "rt")
        gTs = [A.alloc([128], F32) for _ in range(2)]; t_gTs = P.tiles_n(2, "gTs")
        sgp = [A.alloc([1024], F32) for _ in range(2)]; t_sgp = P.tiles_n(2, "sgp")
        bst = [A.alloc([1024], F32) for _ in range(2)]; t_bst = P.tiles_n(2, "bst")

        def layer_norm(src, t_src, dst, t_dst, st, t_st, gain, bias, t_gb):
            for hf_ in range(2):
                P.op("dve", lambda e, hf_=hf_: e.bn_stats(out=st[:, hf_ * 6:(hf_ + 1) * 6], in_=src[:, hf_ * 512:(hf_ + 1) * 512]),
                     reads=[t_src], writes=[t_st])
            P.op("dve", lambda e: e.bn_aggr(out=st[:, 12:14], in_=st[:, 0:12]), reads=[t_st], writes=[t_st])
            P.op("act", lambda e: e.activation(out=st[:, 14:15], in_=st[:, 13:14], func=AF.Ln, bias=cst[:, 4:5], scale=1.0),
                 reads=[t_st, t_cst], writes=[t_st])
            P.op("act", lambda e: e.activation(out=st[:, 14:15], in_=st[:, 14:15], func=AF.Exp, scale=-0.5),
                 reads=[t_st], writes=[t_st])
            P.op("dve", lambda e: e.tensor_scalar(out=dst, in0=src, scalar1=st[:, 12:13], scalar2=st[:, 14:15],
                                                  op0=ALU.subtract, op1=ALU.mult), reads=[t_src, t_st], writes=[t_dst])
            P.op("pool", lambda e: e.tensor_tensor(out=dst, in0=dst, in1=gain, op=ALU.mult), reads=[t_dst] + t_gb, writes=[t_dst])
            P.op("pool", lambda e: e.tensor_tensor(out=dst, in0=dst, in1=bias, op=ALU.add), reads=[t_dst] + t_gb, writes=[t_dst])

        m_pending = []
        m_pendA2 = []
        m_stB = []
        for t in range(NT):
            par = t % 2
            tsl = slice(t * 128, (t + 1) * 128)
            def stageA(t=t, par=par, tsl=tsl):
                P.dma("pool", lambda e, par=par, tsl=tsl: e.dma_start(out=catb[par], in_=cat[tsl, :]),
                      reads=[t_cat[0][t], t_cat[1][t]], writes=[t_catb[par]])
                P.dma("pool", lambda e, par=par, tsl=tsl: e.dma_start(out=xrt[par], in_=xr[tsl, :]), writes=[t_xrt[par]])

                def fCT(e, par=par):
                    ins = None
                    for c in range(8):
                        ins = e.transpose(out=PSB[0][:, c * 128:(c + 1) * 128], in_=catb[par][:, c * 128:(c + 1) * 128], identity=ident_b)
                    return ins
                P.op("pe", fCT, reads=[t_catb[par], t_ident], writes=[t_ps[0]])
                P.op("dve", lambda e, par=par: e.tensor_copy(out=catT[par].rearrange("p a b -> p (a b)"), in_=PSB[0]),
                     reads=[t_ps[0]], writes=[t_catT[par]])
                for half in range(2):
                    def fM(e, par=par, half=half):
                        ins = None
                        for kc in range(8):
                            ins = e.matmul(PS[1 + half], lhsT=catT[par][:, kc, :], rhs=w_o[:, kc, half * 512:(half + 1) * 512],
                                           start=(kc == 0), stop=(kc == 7))
                        return ins
                    P.op("pe", fM, reads=[t_catT[par], t_wl[0]], writes=[t_ps[1 + half]])
                    P.op("dve", lambda e, par=par, half=half: e.scalar_tensor_tensor(
                        out=zt[par][:, half * 512:(half + 1) * 512], in0=xrt[par][:, half * 512:(half + 1) * 512], scalar=ALPHA,
                        in1=PS[1 + half], op0=ALU.mult, op1=ALU.add), reads=[t_xrt[par], t_ps[1 + half]], writes=[t_zt[par]])

            def stageA2(t=t, par=par, tsl=tsl):
                layer_norm(zt[par], t_zt[par], x1t[par], t_x1t[par], lst[par], t_lst[par], g1, b1, [t_wl[5], t_wl[6]])
                if "x1" in dbg:
                    P.dma("sp", lambda e, par=par, tsl=tsl: e.dma_start(out=dbg["x1"][tsl, :], in_=x1t[par]),
                          reads=[t_x1t[par]], sem_tile=t_x1t[par])
                for half in range(2):
                    def fXT(e, par=par, half=half):
                        ins = None
                        for c in range(4):
                            cc_ = half * 4 + c
                            ins = e.transpose(out=PS[3 + half][:, c * 128:(c + 1) * 128], in_=x1t[par][:, cc_ * 128:(cc_ + 1) * 128],
                                              identity=ident_f)
                        return ins
                    P.op("pe", fXT, reads=[t_x1t[par], t_ident], writes=[t_ps[3 + half]])
                    P.op("dve", lambda e, half=half, tsl=tsl: e.tensor_copy(
                        out=x1T[:, half * 4:half * 4 + 4, tsl], in_=PS[3 + half].rearrange("p (a b) -> p a b", b=128)),
                        reads=[t_ps[3 + half]], writes=[t_x1T[t]])
                    P.op("act", lambda e, half=half, par=par: e.copy(
                        out=x1T32[par].rearrange("p a b -> p (a b)")[:, half * 512:(half + 1) * 512], in_=PS[3 + half]),
                        reads=[t_ps[3 + half]], writes=[t_x1T32[par]])


            def stageB(t=t, par=par, tsl=tsl):
                def fR(e, par=par):
                    ins = None
                    for kc in range(8):
                        ins = e.matmul(PS[5][:, 0:64], lhsT=x1T32[par][:, kc, :], rhs=w_rt[:, kc, :], start=(kc == 0), stop=(kc == 7))
                    return ins
                P.op("pe", fR, reads=[t_x1T32[par], t_wl[4]], writes=[t_ps[5]])
                R = rt[par]
                sc, bi, eq, bi2 = R[:, 0:64], R[:, 64:128], R[:, 128:192], R[:, 192:256]
                m1, m2, gs, t8a, gm, pen = R[:, 256:264], R[:, 264:272], R[:, 272:280], R[:, 280:288], R[:, 288:296], R[:, 296:304]
                msk, t8b, sel, wv, den, gate = R[:, 320:384], R[:, 304:312], R[:, 384:448], R[:, 448:512], R[:, 312:314], R[:, 512:576]
                g3 = lambda ap: ap.rearrange("p (a b) -> p a b", b=8)
                tr_ = [t_rt[par]]
                P.op("act", lambda e, sc=sc: e.activation(out=sc, in_=PS[5][:, 0:64], func=AF.Sigmoid), reads=[t_ps[5]], writes=tr_)
                P.op("dve", lambda e, sc=sc, bi=bi: e.tensor_tensor(out=bi, in0=sc, in1=rb, op=ALU.add), reads=tr_ + [t_wl[7]], writes=tr_)
                P.op("dve", lambda e, bi=bi, m1=m1: e.tensor_reduce(out=m1, in_=g3(bi), axis=AX.X, op=ALU.max), reads=tr_, writes=tr_)
                P.op("dve", lambda e, bi=bi, m1=m1, eq=eq: e.tensor_tensor(out=g3(eq), in0=g3(bi), in1=bc_last(m1, 8), op=ALU.is_equal),
                     reads=tr_, writes=tr_)
                P.op("dve", lambda e, bi=bi, eq=eq, bi2=bi2: e.scalar_tensor_tensor(out=bi2, in0=eq, scalar=-1e9, in1=bi,
                                                                                    op0=ALU.mult, op1=ALU.add), reads=tr_, writes=tr_)
                P.op("dve", lambda e, bi2=bi2, m2=m2: e.tensor_reduce(out=m2, in_=g3(bi2), axis=AX.X, op=ALU.max), reads=tr_, writes=tr_)
                P.op("dve", lambda e, m1=m1, m2=m2, gs=gs: e.tensor_tensor(out=gs, in0=m1, in1=m2, op=ALU.add), reads=tr_, writes=tr_)
                P.op("dve", lambda e, gs=gs, t8a=t8a: e.max(out=t8a, in_=gs), reads=tr_, writes=tr_)
                P.op("dve", lambda e, gs=gs, t8a=t8a, gm=gm: e.tensor_scalar(out=gm, in0=gs, scalar1=t8a[:, 3:4], scalar2=None, op0=ALU.is_ge),
                     reads=tr_, writes=tr_)
                P.op("dve", lambda e, gm=gm, pen=pen: e.tensor_scalar(out=pen, in0=gm, scalar1=-1.0, scalar2=1e9, op0=ALU.add, op1=ALU.mult),
                     reads=tr_, writes=tr_)
                P.op("dve", lambda e, bi=bi, pen=pen, msk=msk: e.tensor_tensor(out=g3(msk), in0=g3(bi), in1=bc_last(pen, 8), op=ALU.add),
                     reads=tr_, writes=tr_)
                P.op("dve", lambda e, msk=msk, t8b=t8b: e.max(out=t8b, in_=msk), reads=tr_, writes=tr_)
                P.op("dve", lambda e, msk=msk, t8b=t8b, sel=sel: e.tensor_scalar(out=sel, in0=msk, scalar1=t8b[:, 7:8], scalar2=None,
                                                                                 op0=ALU.is_ge), reads=tr_, writes=tr_)
                P.op("dve", lambda e, sc=sc, sel=sel, wv=wv: e.tensor_tensor(out=wv, in0=sc, in1=sel, op=ALU.mult), reads=tr_, writes=tr_)
                P.op("dve", lambda e, wv=wv, den=den: e.tensor_reduce(out=den[:, 0:1], in_=wv, axis=AX.X, op=ALU.add), reads=tr_, writes=tr_)
                P.op("dve", lambda e, den=den: e.reciprocal(out=den[:, 1:2], in_=den[:, 0:1]), reads=tr_, writes=tr_)
                P.op("dve", lambda e, wv=wv, den=den, gate=gate: e.tensor_scalar(out=gate, in0=wv, scalar1=den[:, 1:2], scalar2=2.5,
                                                                                 op0=ALU.mult, op1=ALU.mult), reads=tr_, writes=tr_)
                if "gate" in dbg:
                    P.dma("sp", lambda e, gate=gate, tsl=tsl: e.dma_start(out=dbg["gate"][tsl, :], in_=gate), reads=tr_, sem_tile=t_rt[par])
                P.op("pe", lambda e, gate=gate: e.transpose(out=PS[5][0:64, 128:256], in_=gate, identity=ident_f),
                     reads=tr_ + [t_ident], writes=[t_ps[5]])
                P.op("act", lambda e, par=par: e.activation(func=AF.Copy, out=gTs[par][0:64, :], in_=PS[5][0:64, 128:256]), reads=[t_ps[5]], writes=[t_gTs[par]])
                P.dma("sp", lambda e, par=par, tsl=tsl: e.dma_start(out=GT[:, tsl], in_=gTs[par][0:64, :]),
                      reads=[t_gTs[par]], writes=[t_GT[t]], sem_tile=t_gTs[par])
                for half in range(2):
                    hs = slice(half * 512, (half + 1) * 512)

                    def fPG(e, half=half, tsl=tsl, hs=hs):
                        ins = None
                        for kc in range(8):
                            ins = e.matmul(PS[6 + half], lhsT=x1T[:, kc, tsl], rhs=w_pgt[:, kc, hs], start=(kc == 0), stop=(kc == 7))
                        return ins
                    P.op("pe", fPG, reads=[t_x1T[t], t_wl[3]], writes=[t_ps[6 + half]])
                    P.op("act", lambda e, half=half, par=par, hs=hs: e.activation(out=sgp[par][:, hs], in_=PS[6 + half], func=AF.Sigmoid),
                         reads=[t_ps[6 + half]], writes=[t_sgp[par]])

                    def fPP(e, half=half, tsl=tsl, hs=hs):
                        ins = None
                        for kc in range(2):
                            ins = e.matmul(PS[6 + half], lhsT=pTb[:, kc, tsl], rhs=w_ppt[:, kc, hs], start=(kc == 0), stop=(kc == 1))
                        return ins
                    P.op("pe", fPP, reads=[t_wl[1], t_wl[2]], writes=[t_ps[6 + half]])
                    P.op("dve", lambda e, half=half, par=par, hs=hs: e.tensor_tensor(out=bst[par][:, hs], in0=PS[6 + half], in1=sgp[par][:, hs],
                                                                                     op=ALU.mult),
                         reads=[t_ps[6 + half], t_sgp[par]], writes=[t_bst[par]])
                P.op("dve", lambda e, par=par: e.scalar_tensor_tensor(out=bst[par], in0=x1t[par], scalar=ALPHA, in1=bst[par],
                                                                      op0=ALU.mult, op1=ALU.add),
                     reads=[t_x1t[par], t_bst[par]], writes=[t_bst[par]])
                P.dma("sp", lambda e, par=par, tsl=tsl: e.dma_start(out=base[tsl, :], in_=bst[par]),
                      reads=[t_bst[par]], writes=[t_base[t]], sem_tile=t_bst[par])

            stageA()
            m_pendA2.append(stageA2)
            if len(m_pendA2) > 1:
                m_pendA2.pop(0)()
                m_pending.append(m_stB.pop(0))
            m_stB.append(stageB)
            if len(m_pending) > 1:
                m_pending.pop(0)()
        while m_pendA2:
            m_pendA2.pop(0)()
            m_pending.append(m_stB.pop(0))
            if len(m_pending) > 1:
                m_pending.pop(0)()
        while m_pending:
            m_pending.pop(0)()
        P.barrier()
        A.release()
        if stage == "x1":
            P.final_wait("sp")
            P.emit_all()
            return nc

        y_acc = A.alloc([NT, 1024], F32); t_y = P.tiles_n(NT, "yacc")
        A.mark()
        m_Wgu = [A.alloc([8, 512], BF16) for _ in range(4)]; m_Wdn = [A.alloc([2, 1024], BF16) for _ in range(4)]
        t_mWgu = P.tiles_n(4, "mWgu"); t_mWdn = P.tiles_n(4, "mWdn")
        m_gbc = [A.alloc([TOK], F32) for _ in range(4)]; t_mgbc = P.tiles_n(4, "mgbc")
        m_act = A.alloc([4, TOK], BF16)
        t_mact = [P.tiles_n(4, "mact%d_" % k_) for k_ in range(4)]
        m_sg = [A.alloc([512], BF16) for _ in range(2)]; m_tt = [A.alloc([512], BF16) for _ in range(2)]
        t_msg = P.tiles_n(2, "msg"); t_mtt = P.tiles_n(2, "mtt")

        def m_load(ex):
            s_ = ex % 4
            src_gu = w_egu[ex] if ex < NEXP else w_sgu
            src_dn = w_edn[ex] if ex < NEXP else w_sdn
            P.dma("pool", lambda eng, s_=s_, src_gu=src_gu: eng.dma_start(
                out=m_Wgu[s_], in_=src_gu.rearrange("(kc k) n -> k kc n", k=128)), writes=[t_mWgu[s_]])
            P.dma("pool", lambda eng, s_=s_, src_dn=src_dn: eng.dma_start(
                out=m_Wdn[s_], in_=src_dn.rearrange("(kc k) n -> k kc n", k=128)), writes=[t_mWdn[s_]])
            if ex < NEXP:
                P.dma("sp", lambda eng, ex=ex: eng.dma_start(out=m_gbc[ex % 4], in_=GT[ex:ex + 1, :].partition_broadcast(128)),
                      reads=t_GT, writes=[t_mgbc[ex % 4]])

        m_groups = [(2 * g_, 2 * g_ + 1) for g_ in range(NEXP // 2)] + [(NEXP,)]
        for ex in m_groups[0]:
            m_load(ex)
        m_cnt = 0
        for gi, grp in enumerate(m_groups):
            if gi + 1 < len(m_groups):
                for ex in m_groups[gi + 1]:
                    m_load(ex)
            for eg, ex in enumerate(grp):
                s_ = ex % 4
                for tg in range(4):
                    tgs = slice(tg * 512, (tg + 1) * 512)
                    for j in range(2):
                        q_ = m_cnt % 2
                        m_cnt += 1
                        bg, bu = 2 * q_, 2 * q_ + 1

                        def fUp(eng, s_=s_, j=j, tgs=tgs, bg=bg, bu=bu):
                            ins = None
                            for kc in range(8):
                                eng.matmul(PS[bg], lhsT=m_Wgu[s_][:, kc, j * 128:(j + 1) * 128], rhs=x1T[:, kc, tgs],
                                           start=(kc == 0), stop=(kc == 7))
                            for kc in range(8):
                                ins = eng.matmul(PS[bu], lhsT=m_Wgu[s_][:, kc, 256 + j * 128:256 + (j + 1) * 128], rhs=x1T[:, kc, tgs],
                                                 start=(kc == 0), stop=(kc == 7))
                            return ins
                        P.op("pe", fUp, reads=[t_mWgu[s_]] + t_x1T[4 * tg:4 * tg + 4], writes=[t_ps[bg], t_ps[bu]])
                        P.op("act", lambda eng, q_=q_, bg=bg: eng.activation(out=m_sg[q_], in_=PS[bg], func=AF.Silu),
                             reads=[t_ps[bg]], writes=[t_msg[q_]])
                        if ex < NEXP:
                            P.op("dve", lambda eng, q_=q_, bu=bu, ex=ex, tgs=tgs: eng.tensor_tensor(
                                out=m_tt[q_], in0=PS[bu], in1=m_gbc[ex % 4][:, tgs], op=ALU.mult),
                                reads=[t_ps[bu], t_mgbc[ex % 4]], writes=[t_mtt[q_]])
                        else:
                            P.op("dve", lambda eng, q_=q_, bu=bu: eng.tensor_copy(out=m_tt[q_], in_=PS[bu]),
                                 reads=[t_ps[bu]], writes=[t_mtt[q_]])
                        P.op("pool", lambda eng, q_=q_, eg=eg, j=j, tgs=tgs: eng.tensor_tensor(
                            out=m_act[:, eg * 2 + j, tgs], in0=m_sg[q_], in1=m_tt[q_], op=ALU.mult),
                            reads=[t_msg[q_], t_mtt[q_]], writes=[t_mact[eg * 2 + j][tg]])
            for t in range(NT):
                yq = t % 2
                tsl = slice(t * 128, (t + 1) * 128)
                for half in range(2):
                    by = 4 + 2 * yq + half
                    hs = slice(half * 512, (half + 1) * 512)

                    def fDn(eng, grp=grp, tsl=tsl, by=by, hs=hs):
                        ins = None
                        n_ = len(grp) * 2
                        k_ = 0
                        for eg, ex in enumerate(grp):
                            for j in range(2):
                                ins = eng.matmul(PS[by], lhsT=m_act[:, eg * 2 + j, tsl], rhs=m_Wdn[ex % 4][:, j, hs],
                                                 start=(k_ == 0), stop=(k_ == n_ - 1))
                                k_ += 1
                        return ins
                    rd_ = [t_mact[eg * 2 + j][t // 4] for eg in range(len(grp)) for j in range(2)] + [t_mWdn[ex % 4] for ex in grp]
                    P.op("pe", fDn, reads=rd_, writes=[t_ps[by]])
                    if gi == 0:
                        P.op("dve", lambda eng, t=t, hs=hs, by=by: eng.tensor_copy(out=y_acc[:, t, hs], in_=PS[by]),
                             reads=[t_ps[by]], writes=[t_y[t]])
                    else:
                        P.op("dve", lambda eng, t=t, hs=hs, by=by: eng.tensor_tensor(out=y_acc[:, t, hs], in0=PS[by], in1=y_acc[:, t, hs],
                                                                                     op=ALU.add),
                             reads=[t_ps[by], t_y[t]], writes=[t_y[t]])
        P.barrier()
        A.release()
        g2 = A.alloc([1024], F32); b2 = A.alloc([1024], F32)
        t_f = P.tiles_n(2, "fgb")
        P.dma("sp", lambda eng: eng.dma_start(out=g2, in_=ln2_g.partition_broadcast(128)), writes=[t_f[0]])
        P.dma("sp", lambda eng: eng.dma_start(out=b2, in_=ln2_b.partition_broadcast(128)), writes=[t_f[1]])
        m_bt = [A.alloc([1024], F32) for _ in range(4)]; m_z2 = [A.alloc([1024], F32) for _ in range(4)]
        m_o = [A.alloc([1024], F32) for _ in range(4)]; m_st = [A.alloc([32], F32) for _ in range(4)]
        t_mbt = P.tiles_n(4, "mbt"); t_mz2 = P.tiles_n(4, "mz2"); t_mo = P.tiles_n(4, "mo"); t_mst = P.tiles_n(4, "mst")
        fin_pending = []
        for t in range(NT):
            par = t % 4
            tsl = slice(t * 128, (t + 1) * 128)
            P.dma("pool", lambda eng, par=par, tsl=tsl: eng.dma_start(out=m_bt[par], in_=base[tsl, :]),
                  reads=[t_base[t]], writes=[t_mbt[par]])
            P.op("pool", lambda eng, par=par, t=t: eng.tensor_tensor(out=m_z2[par], in0=y_acc[:, t, :], in1=m_bt[par], op=ALU.add),
                 reads=[t_y[t], t_mbt[par]], writes=[t_mz2[par]])
            def fin_tail(par=par, tsl=tsl):
                layer_norm(m_z2[par], t_mz2[par], m_o[par], t_mo[par], m_st[par], t_mst[par], g2, b2, t_f)
                P.dma("sp", lambda eng: eng.dma_start(out=out[tsl, :], in_=m_o[par]),
                      reads=[t_mo[par]], sem_tile=t_mo[par])
            fin_pending.append(fin_tail)
            if len(fin_pending) > 2:
                fin_pending.pop(0)()
        while fin_pending:
            fin_pending.pop(0)()
        P.final_wait("sp")
        P.emit_all()
        return nc


def _na_table(rpb, hf):
    base = 32 * hf
    tab = np.full((8, 128, NA_BLOCKS * 128), NEG, np.float32)
    kk = np.arange(128)
    qq = np.arange(128)
    for i in (0, 1, 2, 14, 15):
        kt0, nk, b0 = NA_TYPES[i]
        r = base + 2 * i + qq // 64
        c = qq % 64
        rs = np.clip(r - 4, 0, 56)
        cs = np.clip(c - 8, 0, 48)
        for idx in range(nk):
            kt = kt0 + idx
            kr = base - 4 + 2 * kt + kk // 64
            kc = kk % 64
            vr = (kr[:, None] >= rs[None, :]) & (kr[:, None] <= rs[None, :] + 7) & (kr[:, None] >= 0) & (kr[:, None] <= 63)
            vc = (kc[:, None] >= cs[None, :]) & (kc[:, None] <= cs[None, :] + 15)
            valid = vr & vc
            dr = np.clip(kr[:, None] - r[None, :] + 7, 0, 14)
            dc = np.clip(kc[:, None] - c[None, :] + 15, 0, 30)
            vals = rpb[:, dr, dc]
            blk = np.where(valid[None], vals, np.float32(NEG))
            tab[:, :, (b0 + idx) * 128:(b0 + idx + 1) * 128] = blk
    return tab


def _const_tables(hf):
    half = 32
    inv = (10000.0 ** (-np.arange(half, dtype=np.float32) / half)).astype(np.float32)
    j = np.arange(128)
    t = np.arange(NT)

    def rot(pos0):
        pos = (pos0 + t[None, :] * 128 + j[:, None]).astype(np.float32)
        ang = pos[:, :, None] * inv[None, None, :]
        cos = np.cos(ang).astype(np.float32)
        sin = np.sin(ang).astype(np.float32)
        cc = np.concatenate([cos, cos], -1).reshape(128, NT * 64)
        ss = np.concatenate([-sin, sin], -1).reshape(128, NT * 64)
        return np.ascontiguousarray(cc), np.ascontiguousarray(ss)
    cc_own, ss_own = rot(hf * TOK)
    cc_oth, ss_oth = rot((1 - hf) * TOK)
    m = t[None, :] * 128 + j[:, None]
    dist = (2047 - m if hf == 1 else m).astype(np.float32)
    ii = np.arange(128, dtype=np.float32)
    epos = np.maximum(ii[None, :] - ii[:, None], 0).astype(np.float32)
    eneg = np.maximum(ii[:, None] - ii[None, :], 0).astype(np.float32)
    iota1 = np.broadcast_to(ii[None, :] + 1, (128, 128)).astype(np.float32).copy()
    iota2 = np.broadcast_to(128 - ii[None, :], (128, 128)).astype(np.float32).copy()
    return dict(cc_own=cc_own, ss_own=ss_own, cc_oth=cc_oth, ss_oth=ss_oth, dist=np.ascontiguousarray(dist),
                epos=epos, eneg=eneg, iota1=iota1, iota2=iota2,
                c127=(127 - ii).reshape(128, 1).astype(np.float32), cj=ii.reshape(128, 1).copy(),
                flag=np.full((1, 1), float(hf), np.float32))


def make_in_maps(inputs, cores=range(NCORES)):
    f = lambda a: np.ascontiguousarray(np.asarray(a, dtype=np.float32))
    x = f(inputs["x"]); p = f(inputs["p"])[0]
    shared = dict(
        w_in=f(inputs["w_in"][0]), w_out=f(inputs["w_out"][0]), w_router=f(inputs["w_router"][0]),
        w_egu=f(inputs["w_expert_gu"][0]), w_edn=f(inputs["w_expert_down"][0]),
        w_sgu=f(inputs["w_shared_gu"][0]), w_sdn=f(inputs["w_shared_down"][0]),
        w_pp=f(inputs["w_ple_proj"][0]), w_pg=f(inputs["w_ple_gate"][0]),
        dec_f=f(inputs["ret_decay_fwd"]).reshape(1, 8), dec_b=f(inputs["ret_decay_bwd"]).reshape(1, 8),
        decfT=f(f(inputs["ret_decay_fwd"]).reshape(4, 2).T), decbT=f(f(inputs["ret_decay_bwd"]).reshape(4, 2).T),
        gn_gain=f(inputs["ret_gn_gain"]).reshape(1, 512),
        ln1_g=f(inputs["ln1_gain"]).reshape(1, D), ln1_b=f(inputs["ln1_bias"]).reshape(1, D),
        ln2_g=f(inputs["ln2_gain"]).reshape(1, D), ln2_b=f(inputs["ln2_bias"]).reshape(1, D),
        rbias=f(inputs["router_bias"]).reshape(1, NEXP))
    rpb = f(inputs["na_rpb"][0])
    per_hf = {}
    for hf in (0, 1):
        d = _const_tables(hf)
        d["natab"] = _na_table(rpb, hf)
        per_hf[hf] = d
    maps = []
    for c in cores:
        b, hf = divmod(c, 2)
        own = x[b, hf * TOK:(hf + 1) * TOK]
        oth = x[b, (1 - hf) * TOK:(2 - hf) * TOK]
        xh = np.zeros((512, D), np.float32)
        if hf == 0:
            xh[256:512] = oth[0:256]
        else:
            xh[0:256] = oth[TOK - 256:TOK]
        m = dict(shared)
        m.update(per_hf[hf])
        m.update(xT=f(own.T), xo=f(oth.T), xh=f(xh.T), xr=f(own), pT=f(p[b, hf * TOK:(hf + 1) * TOK].T))
        maps.append(m)
    return maps


_NC_CACHE = {}


def kernel(**inputs):
    if "full" not in _NC_CACHE:
        _NC_CACHE["full"] = build_program("full")
    nc = _NC_CACHE["full"]
    maps = make_in_maps(inputs)
    res = run_bass_kernel_spmd(nc, maps, core_ids=list(range(NCORES)))
    outp = np.empty((4, S, D), np.float32)
    for c in range(NCORES):
        b, hf = divmod(c, 2)
        outp[b, hf * TOK:(hf + 1) * TOK] = res.results[c]["out"]
    return outp
```

```python
import math
from contextlib import ExitStack

import numpy as np
import concourse.bass as bass
import concourse.mybir as mybir
from concourse.bass_utils import run_bass_kernel_spmd

F32 = mybir.dt.float32
BF16 = mybir.dt.bfloat16
U8 = mybir.dt.uint8
AF = mybir.ActivationFunctionType
ALU = mybir.AluOpType
AX = mybir.AxisListType
DT_SIZE = {F32: 4, BF16: 2, U8: 1}

NCORES = 8
D = 1024
S = 4096
TOK = 2048
NT = 16
NEXP = 64
ALPHA = 2.0 ** 0.25
LN_EPS = 1e-5
GN_EPS = 1e-6
NEG = -30000.0
NA_TYPES = {0: (0, 6, 0), 1: (1, 5, 6), 14: (14, 5, 16), 15: (14, 6, 21)}
for _i in range(2, 14):
    NA_TYPES[_i] = (_i, 5, 11)
NA_BLOCKS = 27


class Tile:
    __slots__ = ("name", "lw", "rd", "dsem", "excl")

    def __init__(self, name):
        self.name = name
        self.excl = False
        self.lw = None
        self.rd = []
        self.dsem = None


class Prog:
    ENGS = ("pe", "act", "dve", "pool", "sp")

    def __init__(self, nc, stack):
        self.nc = nc
        self.stack = stack
        self.streams = {e: [] for e in self.ENGS}
        self.sems = {}
        self.cnt = {}
        for e in self.ENGS:
            self.sems[e] = stack.enter_context(nc.semaphore("s_" + e))
            self.cnt[e] = 0
        self.seen = {e: {} for e in self.ENGS}
        self.ndsem = 0
        self.tiles = []

    def tile(self, name="t"):
        t = Tile(name)
        self.tiles.append(t)
        return t

    def tiles_n(self, n, name="t"):
        return [self.tile("%s%d" % (name, i)) for i in range(n)]

    def _dma_sem(self, t):
        if t.dsem is None:
            key = "d%d" % self.ndsem
            self.ndsem += 1
            self.sems[key] = self.stack.enter_context(self.nc.semaphore("s_" + key))
            self.cnt[key] = 0
            t.dsem = key
        return t.dsem

    def _waits(self, eng, reads, writes):
        need = {}

        def add(ev):
            if ev is None:
                return
            k, v = ev
            if need.get(k, 0) < v:
                need[k] = v
        for t in reads:
            add(t.lw)
            if t.excl:
                for ev in t.rd:
                    if ev[0] != eng:
                        add(ev)
        for t in writes:
            add(t.lw)
            for ev in t.rd:
                add(ev)
        out = []
        for k, v in need.items():
            if k == "pe" and eng == "pe":
                continue
            if self.seen[eng].get(k, 0) >= v:
                continue
            self.seen[eng][k] = v
            out.append((k, v))
        return out

    def op(self, eng, fn, reads=(), writes=()):
        waits = self._waits(eng, reads, writes)
        self.cnt[eng] += 1
        ev = (eng, self.cnt[eng])
        sems = self.sems

        def emit(e, waits=waits, fn=fn, semk=eng):
            for k, v in waits:
                e.wait_ge(sems[k], v)
            ins = fn(e)
            ins.then_inc(sems[semk], 1)
        self.streams[eng].append(emit)
        for t in reads:
            t.rd.append(ev)
        for t in writes:
            t.lw = ev
            t.rd = []
        return ev

    def dma(self, q, fn, reads=(), writes=(), sem_tile=None):
        st = sem_tile or (writes[0] if writes else reads[0])
        key = self._dma_sem(st)
        waits = self._waits(q, reads, writes)
        if self.cnt[key] > 0 and self.seen[q].get(key, 0) < self.cnt[key]:
            self.seen[q][key] = self.cnt[key]
            waits.append((key, self.cnt[key]))
        self.cnt[key] += 16
        ev = (key, self.cnt[key])
        sems = self.sems

        def emit(e, waits=waits, fn=fn, key=key):
            for k, v in waits:
                e.wait_ge(sems[k], v)
            ins = fn(e)
            ins.then_inc(sems[key], 16)
        self.streams[q].append(emit)
        for t in reads:
            t.rd.append(ev)
        for t in writes:
            t.lw = ev
            t.rd = []
        return ev

    def barrier(self):
        snap = {k: v for k, v in self.cnt.items() if v > 0}
        sems = self.sems
        for eng in self.ENGS:
            waits = []
            for k, v in snap.items():
                if k == eng:
                    continue
                if self.seen[eng].get(k, 0) >= v:
                    continue
                self.seen[eng][k] = v
                waits.append((k, v))

            def emit(e, waits=waits):
                for k, v in waits:
                    e.wait_ge(sems[k], v)
            self.streams[eng].append(emit)
        for t in self.tiles:
            t.rd = []

    def final_wait(self, eng="sp"):
        snap = {k: v for k, v in self.cnt.items() if v > 0}
        sems = self.sems

        def emit(e):
            for k, v in snap.items():
                if k == eng:
                    continue
                e.wait_ge(sems[k], v)
        self.streams[eng].append(emit)

    def emit_all(self):
        nc = self.nc
        streams = self.streams
        with nc.Block() as block:
            @block.tensor
            def _(e):
                for f in streams["pe"]:
                    f(e)

            @block.scalar
            def _(e):
                for f in streams["act"]:
                    f(e)

            @block.vector
            def _(e):
                for f in streams["dve"]:
                    f(e)

            @block.gpsimd
            def _(e):
                for f in streams["pool"]:
                    f(e)

            @block.sync
            def _(e):
                for f in streams["sp"]:
                    f(e)


class Arena:
    def __init__(self, nc, stack, nbytes):
        self.t = stack.enter_context(nc.sbuf_tensor("arena", [128, nbytes], U8))
        self.n = nbytes
        self.off = 0
        self.marks = []
        self.peak = 0

    def mark(self):
        self.marks.append(self.off)

    def release(self):
        self.off = self.marks.pop()

    def alloc(self, free_shape, dtype):
        n = int(np.prod(free_shape)) * DT_SIZE[dtype]
        n_al = (n + 63) // 64 * 64
        assert self.off + n_al <= self.n, ("SBUF arena overflow", self.off, n_al, self.n)
        ap = self.t[:, self.off:self.off + n].bitcast(dtype)
        self.off += n_al
        self.peak = max(self.peak, self.off)
        if len(free_shape) == 2:
            ap = ap.rearrange("p (a b) -> p a b", b=free_shape[1])
        elif len(free_shape) == 3:
            ap = ap.rearrange("p (a b c) -> p a b c", b=free_shape[1], c=free_shape[2])
        return ap


def bc_mid(ap2, n):
    p, f = ap2.shape
    return ap2.unsqueeze(1).to_broadcast([p, n, f])


def bc_last(ap2, n):
    p, a = ap2.shape
    return ap2.unsqueeze(2).to_broadcast([p, a, n])


def build_program(stage="full"):
    nc = bass.Bass("TRN2", target_bir_lowering=False)

    def din(name, shape):
        return nc.dram_tensor(name, list(shape), F32, kind="ExternalInput").ap()

    xT = din("xT", [D, TOK]); xo = din("xo", [D, TOK]); xh = din("xh", [D, 512])
    xr = din("xr", [TOK, D]); pT = din("pT", [256, TOK])
    w_in = din("w_in", [D, 3584]); w_out = din("w_out", [D, D]); w_router = din("w_router", [D, NEXP])
    w_egu = din("w_egu", [NEXP, D, 512]); w_edn = din("w_edn", [NEXP, 256, D])
    w_sgu = din("w_sgu", [D, 512]); w_sdn = din("w_sdn", [256, D])
    w_pp = din("w_pp", [256, D]); w_pg = din("w_pg", [D, D])
    cc_own = din("cc_own", [128, NT * 64]); ss_own = din("ss_own", [128, NT * 64])
    cc_oth = din("cc_oth", [128, NT * 64]); ss_oth = din("ss_oth", [128, NT * 64])
    dist = din("dist", [128, NT])
    dec_f = din("dec_f", [1, 8]); dec_b = din("dec_b", [1, 8])
    decfT = din("decfT", [2, 4]); decbT = din("decbT", [2, 4]); flag = din("flag", [1, 1])
    epos = din("epos", [128, 128]); eneg = din("eneg", [128, 128])
    iota1 = din("iota1", [128, 128]); iota2 = din("iota2", [128, 128])
    c127 = din("c127", [128, 1]); cj = din("cj", [128, 1])
    gn_gain = din("gn_gain", [1, 512]); natab = din("natab", [8, 128, NA_BLOCKS * 128])
    ln1_g = din("ln1_g", [1, D]); ln1_b = din("ln1_b", [1, D])
    ln2_g = din("ln2_g", [1, D]); ln2_b = din("ln2_b", [1, D]); rbias = din("rbias", [1, NEXP])
    out = nc.dram_tensor("out", [TOK, D], F32, kind="ExternalOutput").ap()
    GT = nc.dram_tensor("GT", [NEXP, TOK], F32, kind="Internal").ap()
    base = nc.dram_tensor("base", [TOK, D], F32, kind="Internal").ap()
    cat = nc.dram_tensor("cat", [TOK, D], BF16, kind="Internal").ap()
    dbg = {}
    if stage != "full":
        dbg["ret"] = nc.dram_tensor("dbg_ret", [TOK, 512], F32, kind="ExternalOutput").ap()
        dbg["na"] = nc.dram_tensor("dbg_na", [TOK, 512], F32, kind="ExternalOutput").ap()
        dbg["x1"] = nc.dram_tensor("dbg_x1", [TOK, D], F32, kind="ExternalOutput").ap()
        dbg["gate"] = nc.dram_tensor("dbg_gate", [TOK, NEXP], F32, kind="ExternalOutput").ap()
        dbg["misc"] = nc.dram_tensor("dbg_misc", [128, 64], F32, kind="ExternalOutput").ap()

    with ExitStack() as st:
        P = Prog(nc, st)
        A = Arena(nc, st, 206 * 1024)
        PS = [st.enter_context(nc.psum_tensor("ps%d" % i, [128, 512], F32))[:, :] for i in range(8)]
        PSB = [p.bitcast(BF16) for p in PS]
        t_ps = P.tiles_n(8, "ps")
        for t_ in t_ps:
            t_.excl = True

        ident_f = A.alloc([128], F32); ident_b = A.alloc([128], BF16)
        t_ident = P.tile("ident")

        P.op("pool", lambda e: e.memset(ident_f, 0.0), writes=[t_ident])
        P.op("pool", lambda e: e.affine_select(out=ident_f, in_=ident_f, pattern=[[-1, 128]], compare_op=ALU.not_equal,
                                               fill=1.0, base=0, channel_multiplier=1), reads=[t_ident], writes=[t_ident])
        P.op("dve", lambda e: e.tensor_copy(out=ident_b, in_=ident_f), reads=[t_ident], writes=[t_ident])

        cst = A.alloc([8], F32)
        t_cst = P.tile("cst")

        for (p0, p1, c0, c1, val) in ((0, 128, 0, 1, math.log(0.125)), (0, 128, 1, 2, 1.0), (0, 128, 2, 3, 0.0),
                                      (0, 128, 3, 4, GN_EPS), (0, 128, 4, 5, LN_EPS), (0, 64, 5, 6, 1.0),
                                      (64, 128, 5, 6, 0.0), (0, 64, 6, 7, 0.0), (64, 128, 6, 7, 1.0)):
            P.op("pool", lambda e, p0=p0, p1=p1, c0=c0, c1=c1, val=val: e.memset(cst[p0:p1, c0:c1], val), writes=[t_cst])

        t_cat = [P.tiles_n(NT, "catR"), P.tiles_n(NT, "catN")]

        A.mark()
        w_ret = A.alloc([8, 2048], BF16)
        t_wret = P.tiles_n(4, "wret")
        for g in range(4):
            P.dma("pool", lambda e, g=g: e.dma_start(
                out=w_ret[:, :, g * 512:(g + 1) * 512],
                in_=w_in[:, g * 512:(g + 1) * 512].rearrange("(kc k) n -> k kc n", k=128)), writes=[t_wret[g]])
        ccX = A.alloc([NT, 64], F32); ssX = A.alloc([NT, 64], F32)
        ccO, ssO = ccX, ssX
        t_rotl = P.tiles_n(2, "rot")

        def load_rot(c_src, s_src):
            for i_, (dst, src) in enumerate(((ccX, c_src), (ssX, s_src))):
                P.dma("sp", lambda e, dst=dst, src=src: e.dma_start(out=dst.rearrange("p a b -> p (a b)"), in_=src),
                      writes=[t_rotl[i_]])
        load_rot(cc_oth, ss_oth)
        small = A.alloc([256], F32)
        t_small = P.tile("small")
        sm = lambda a, b: small[:, a:b]
        lds = [(sm(0, 8), dec_f.partition_broadcast(128)), (sm(8, 16), dec_b.partition_broadcast(128)),
               (small[0:64, 16:20], decfT[0:1, :].partition_broadcast(64)),
               (small[64:128, 16:20], decfT[1:2, :].partition_broadcast(64)),
               (small[0:64, 20:24], decbT[0:1, :].partition_broadcast(64)),
               (small[64:128, 20:24], decbT[1:2, :].partition_broadcast(64)),
               (sm(24, 25), flag.partition_broadcast(128)), (sm(96, 97), c127), (sm(97, 98), cj),
               (sm(100, 116), dist)]
        t_smld = P.tiles_n(len(lds), "smld")
        for i_, (dst, src) in enumerate(lds):
            P.dma("sp", lambda e, dst=dst, src=src: e.dma_start(out=dst, in_=src), writes=[t_smld[i_]])
        eposT = A.alloc([128], F32); enegT = A.alloc([128], F32)
        io1 = A.alloc([128], F32); io2 = A.alloc([128], F32)
        gnb = A.alloc([512], F32)
        t_tabl = P.tiles_n(5, "tabl")
        t_gc = P.tile("gc")
        for i_, (dst, src) in enumerate(((eposT, epos), (enegT, eneg), (io1, iota1), (io2, iota2),
                                         (gnb, gn_gain.partition_broadcast(128)))):
            P.dma("sp", lambda e, dst=dst, src=src: e.dma_start(out=dst, in_=src), writes=[t_tabl[i_]])

        P.op("act", lambda e: e.activation(out=sm(32, 56), in_=sm(0, 24), func=AF.Exp, scale=-1.0),
             reads=t_smld, writes=[t_small])
        P.op("act", lambda e: e.activation(out=sm(32, 56), in_=sm(32, 56), func=AF.Ln, bias=cst[:, 1:2], scale=1.0),
             reads=[t_small, t_cst], writes=[t_small])
        P.op("dve", lambda e: e.tensor_scalar(out=sm(32, 56), in0=sm(32, 56), scalar1=-1.0, scalar2=None, op0=ALU.mult),
             reads=[t_small], writes=[t_small])
        P.op("dve", lambda e: e.tensor_scalar(out=sm(25, 26), in0=sm(24, 25), scalar1=-1.0, scalar2=1.0,
                                              op0=ALU.mult, op1=ALU.add), reads=[t_small], writes=[t_small])
        P.op("dve", lambda e: e.tensor_scalar(out=sm(56, 64), in0=sm(32, 40), scalar1=sm(24, 25), scalar2=None,
                                              op0=ALU.mult), reads=[t_small], writes=[t_small])
        P.op("dve", lambda e: e.scalar_tensor_tensor(out=sm(56, 64), in0=sm(40, 48), scalar=sm(25, 26), in1=sm(56, 64),
                                                     op0=ALU.mult, op1=ALU.add), reads=[t_small], writes=[t_small])
        P.op("act", lambda e: e.activation(out=sm(64, 72), in_=sm(32, 40), func=AF.Exp, scale=sm(96, 97)),
             reads=[t_small], writes=[t_small])
        P.op("act", lambda e: e.activation(out=sm(72, 80), in_=sm(40, 48), func=AF.Exp, scale=sm(97, 98)),
             reads=[t_small], writes=[t_small])
        P.op("act", lambda e: e.activation(out=sm(80, 88), in_=sm(48, 56), func=AF.Exp, scale=128.0),
             reads=[t_small], writes=[t_small])
        gcfB = A.alloc([4, 64], F32); gcbB = A.alloc([4, 64], F32)
        P.op("dve", lambda e: e.tensor_copy(out=gcfB, in_=bc_last(sm(80, 84), 64)), reads=[t_small], writes=[t_gc])
        P.op("dve", lambda e: e.tensor_copy(out=gcbB, in_=bc_last(sm(84, 88), 64)), reads=[t_small], writes=[t_gc])
        Mmask = A.alloc([8, 128], BF16)
        mtmp = A.alloc([128], F32)
        t_M = P.tile("M"); t_mtmp = P.tile("mtmp")
        for h in range(8):
            P.op("dve", lambda e, h=h: e.tensor_scalar(out=mtmp, in0=eposT, scalar1=sm(32 + h, 33 + h), scalar2=None,
                                                       op0=ALU.mult), reads=[t_small] + t_tabl, writes=[t_mtmp])
            P.op("dve", lambda e, h=h: e.scalar_tensor_tensor(out=mtmp, in0=enegT, scalar=sm(40 + h, 41 + h), in1=mtmp,
                                                              op0=ALU.mult, op1=ALU.add),
                 reads=[t_small, t_mtmp] + t_tabl, writes=[t_mtmp])
            P.op("act", lambda e, h=h: e.activation(out=Mmask[:, h, :], in_=mtmp, func=AF.Exp, bias=cst[:, 0:1], scale=1.0),
                 reads=[t_mtmp, t_cst], writes=[t_M])
        Df = A.alloc([4, 128], BF16); Db = A.alloc([4, 128], BF16)
        t_D = P.tile("D")
        for pr in range(4):
            P.op("act", lambda e, pr=pr: e.activation(out=Df[:, pr, :], in_=io1, func=AF.Exp, bias=cst[:, 0:1],
                                                      scale=sm(48 + pr, 49 + pr)), reads=[t_small, t_cst] + t_tabl, writes=[t_D])
            P.op("act", lambda e, pr=pr: e.activation(out=Db[:, pr, :], in_=io2, func=AF.Exp, bias=cst[:, 0:1],
                                                      scale=sm(52 + pr, 53 + pr)), reads=[t_small, t_cst] + t_tabl, writes=[t_D])
        if "misc" in dbg:
            P.dma("sp", lambda e: e.dma_start(out=dbg["misc"][:, 0:56], in_=sm(32, 88)), reads=[t_small], sem_tile=P.tile("dm"))

        if stage == "ret0":
            P.final_wait("sp")
            P.emit_all()
            return nc
        qT = A.alloc([4, TOK], BF16); kT = A.alloc([4, TOK], BF16)
        vS = A.alloc([NT, 512], BF16); GS = A.alloc([NT, 512], BF16)
        SfB = A.alloc([NT, 256], BF16); SbB = A.alloc([NT, 256], BF16)
        UbS = A.alloc([NT, 256], BF16)
        Sst = A.alloc([2, 256], F32)
        t_qT = P.tiles_n(NT, "qT"); t_kT = P.tiles_n(NT, "kT"); t_v = P.tiles_n(NT, "v"); t_G = P.tiles_n(NT, "G")
        t_SfB = P.tiles_n(NT, "SfB"); t_SbB = P.tiles_n(NT, "SbB"); t_Ub = P.tiles_n(NT, "Ub")
        t_Sf = P.tile("Sf"); t_Sb = P.tile("Sb")
        A.mark()
        xs = [A.alloc([8, 256], BF16) for _ in range(2)]; t_xs = P.tiles_n(2, "xs")
        q_tm = [A.alloc([512], BF16) for _ in range(2)]; k_tm = [A.alloc([512], BF16) for _ in range(2)]
        t_qtm = P.tiles_n(2, "qtm"); t_ktm = P.tiles_n(2, "ktm")
        rA = [A.alloc([512], F32) for _ in range(2)]; rB = [A.alloc([512], F32) for _ in range(2)]
        t_rA = P.tiles_n(2, "rA"); t_rB = P.tiles_n(2, "rB")
        vfb = [A.alloc([4, 256], BF16) for _ in range(2)]; t_vfb = P.tiles_n(2, "vfb")
        wo = [A.alloc([8], F32) for _ in range(2)]; t_wo = P.tiles_n(2, "wo")
        Ud = [A.alloc([4, 2, 64], F32) for _ in range(2)]; t_Ud = P.tiles_n(2, "Ud")
        Uf = [A.alloc([2, 512], F32) for _ in range(2)]; t_Uf = P.tiles_n(2, "Uf")

        def v3(ap):
            return ap.rearrange("p (h d) -> p h d", d=64)

        def inproj(ps_i, xs_ap, wcols, reads):
            def f(e):
                ins = None
                for kc in range(8):
                    ins = e.matmul(PS[ps_i], lhsT=xs_ap(kc), rhs=w_ret[:, kc, wcols * 512:(wcols + 1) * 512],
                                   start=(kc == 0), stop=(kc == 7))
                return ins
            P.op("pe", f, reads=reads, writes=[t_ps[ps_i]])

        def rotary(ps_i, cc, ss, t, dst, t_dst, par):
            src = v3(PS[ps_i])
            a3 = v3(rA[par]); b3 = v3(rB[par])
            P.op("dve", lambda e: e.tensor_tensor(out=a3, in0=src, in1=bc_mid(cc[:, t, :], 8), op=ALU.mult),
                 reads=[t_ps[ps_i]] + t_rotl, writes=[t_rA[par]])
            P.op("dve", lambda e: e.tensor_tensor(out=b3[:, :, 0:32], in0=src[:, :, 32:64],
                                                  in1=bc_mid(ss[:, t, 0:32], 8), op=ALU.mult),
                 reads=[t_ps[ps_i]] + t_rotl, writes=[t_rB[par]])
            P.op("dve", lambda e: e.tensor_tensor(out=b3[:, :, 32:64], in0=src[:, :, 0:32],
                                                  in1=bc_mid(ss[:, t, 32:64], 8), op=ALU.mult),
                 reads=[t_ps[ps_i]] + t_rotl, writes=[t_rB[par]])
            P.op("pool", lambda e: e.tensor_tensor(out=dst, in0=rA[par], in1=rB[par], op=ALU.add),
                 reads=[t_rA[par], t_rB[par]], writes=[t_dst])

        sinB = [PS[4 + pr][:, 0:128] for pr in range(4)]
        oth_pending = []
        for t in range(NT):
            ch, sub = divmod(t, 2)
            par = t % 2
            if sub == 0:
                P.dma("pool", lambda e, ch=ch: e.dma_start(
                    out=xs[ch % 2], in_=xo[:, ch * 256:(ch + 1) * 256].rearrange("(kc k) n -> k kc n", k=128)),
                    writes=[t_xs[ch % 2]])
            xs_ap = lambda kc, ch=ch, sub=sub: xs[ch % 2][:, kc, sub * 128:(sub + 1) * 128]
            inproj(0, xs_ap, 1, [t_xs[ch % 2], t_wret[1]])
            inproj(1, xs_ap, 2, [t_xs[ch % 2], t_wret[2]])
            while oth_pending:
                oth_pending.pop(0)()
            rotary(0, ccX, ssX, t, k_tm[par], t_ktm[par], par)
            P.op("act", lambda e, t=t, par=par: e.activation(out=wo[par], in_=sm(56, 64), func=AF.Exp,
                                                             scale=sm(100 + t, 101 + t)),
                 reads=[t_small], writes=[t_wo[par]])
            P.op("dve", lambda e, par=par: e.tensor_tensor(
                out=vfb[par].rearrange("p a b -> p (a b)")[:, 0:512].rearrange("p (h d) -> p h d", d=64),
                in0=v3(PS[1]), in1=bc_last(wo[par], 64), op=ALU.mult),
                reads=[t_ps[1], t_wo[par]], writes=[t_vfb[par]])

            def fU(e, t=t, par=par):
                ins = None
                vw = vfb[par].rearrange("p a b -> p (a b)")
                for pr in range(4):
                    ins = e.matmul(sinB[pr], lhsT=k_tm[par][:, pr * 128:(pr + 1) * 128],
                                   rhs=vw[:, pr * 128:(pr + 1) * 128], start=(t == 0), stop=(t == NT - 1))
                return ins
            oth_pending.append(lambda fU=fU, par=par: P.op("pe", fU, reads=[t_ktm[par], t_vfb[par]], writes=t_ps[4:8]))
        while oth_pending:
            oth_pending.pop(0)()
        S3 = Sst.rearrange("p a (b c) -> p a b c", c=64)
        for hp in range(2):
            pl, ph = hp * 64, hp * 64 + 64
            for pr in range(4):
                P.op("dve", lambda e, pl=pl, ph=ph, pr=pr: e.tensor_scalar(
                    out=S3[pl:ph, 0, pr, :], in0=sinB[pr][pl:ph, pl:ph], scalar1=small[pl:ph, 24:25], scalar2=None, op0=ALU.mult),
                    reads=[t_ps[4 + pr], t_small], writes=[t_Sf])
                P.op("dve", lambda e, pl=pl, ph=ph, pr=pr: e.tensor_scalar(
                    out=S3[pl:ph, 1, pr, :], in0=sinB[pr][pl:ph, pl:ph], scalar1=small[pl:ph, 25:26], scalar2=None, op0=ALU.mult),
                    reads=[t_ps[4 + pr], t_small], writes=[t_Sb])

        if stage == "ret1":
            P.final_wait("sp")
            P.emit_all()
            return nc
        load_rot(cc_own, ss_own)
        own_pending = []
        for t in range(NT):
            ch, sub = divmod(t, 2)
            par = t % 2
            if sub == 0:
                P.dma("pool", lambda e, ch=ch: e.dma_start(
                    out=xs[ch % 2], in_=xT[:, ch * 256:(ch + 1) * 256].rearrange("(kc k) n -> k kc n", k=128)),
                    writes=[t_xs[ch % 2]])
            xs_ap = lambda kc, ch=ch, sub=sub: xs[ch % 2][:, kc, sub * 128:(sub + 1) * 128]
            for g in range(4):
                inproj(g, xs_ap, g, [t_xs[ch % 2], t_wret[g]])
            rotary(0, ccO, ssO, t, q_tm[par], t_qtm[par], par)
            rotary(1, ccO, ssO, t, k_tm[par], t_ktm[par], par)
            P.op("act", lambda e, t=t: e.activation(func=AF.Copy, out=vS[:, t, :], in_=PS[2]), reads=[t_ps[2]], writes=[t_v[t]])
            vfl = vfb[par].rearrange("p a b -> p (a b)")
            for dr, col in ((0, 64), (1, 72)):
                P.op("dve", lambda e, dr=dr, col=col, vfl=vfl: e.tensor_tensor(
                    out=v3(vfl[:, dr * 512:(dr + 1) * 512]), in0=v3(PS[2]), in1=bc_last(small[:, col:col + 8], 64),
                    op=ALU.mult), reads=[t_ps[2], t_small], writes=[t_vfb[par]])
            P.op("act", lambda e, t=t: e.activation(out=GS[:, t, :], in_=PS[3], func=AF.Silu),
                 reads=[t_ps[3]], writes=[t_G[t]])
            P.op("pool", lambda e, t=t: e.tensor_tensor(out=GS[:, t, :], in0=GS[:, t, :], in1=gnb, op=ALU.mult),
                 reads=[t_G[t]] + t_tabl, writes=[t_G[t]])

            while own_pending:
                own_pending.pop(0)()

            def own_tail(t=t, par=par):
                def fT(e, par=par):
                    ins = None
                    for pr in range(4):
                        ins = e.transpose(out=PSB[4][:, pr * 128:(pr + 1) * 128], in_=q_tm[par][:, pr * 128:(pr + 1) * 128],
                                          identity=ident_b)
                    for pr in range(4):
                        ins = e.transpose(out=PSB[7][:, pr * 128:(pr + 1) * 128],
                                          in_=k_tm[par][:, pr * 128:(pr + 1) * 128], identity=ident_b)
                    return ins
                P.op("pe", fT, reads=[t_qtm[par], t_ktm[par], t_ident], writes=[t_ps[4], t_ps[7]])
                P.op("dve", lambda e, t=t: e.tensor_copy(out=qT[:, :, t * 128:(t + 1) * 128],
                                                         in_=PSB[4][:, 0:512].rearrange("p (a b) -> p a b", b=128)),
                     reads=[t_ps[4]], writes=[t_qT[t]])
                P.op("dve", lambda e, t=t: e.tensor_copy(out=kT[:, :, t * 128:(t + 1) * 128],
                                                         in_=PSB[7][:, 0:512].rearrange("p (a b) -> p a b", b=128)),
                     reads=[t_ps[7]], writes=[t_kT[t]])

                def fU2(e, par=par):
                    ins = None
                    vfl = vfb[par].rearrange("p a b -> p (a b)")
                    for pr in range(4):
                        bank = PS[5 + pr // 2]
                        for dr in range(2):
                            c0 = (pr % 2) * 256 + dr * 128
                            ins = e.matmul(bank[:, c0:c0 + 128], lhsT=k_tm[par][:, pr * 128:(pr + 1) * 128],
                                           rhs=vfl[:, dr * 512 + pr * 128:dr * 512 + (pr + 1) * 128], start=True, stop=True)
                    return ins
                P.op("pe", fU2, reads=[t_ktm[par], t_vfb[par]], writes=[t_ps[5], t_ps[6]])
                P.op("act", lambda e, t=t: e.activation(func=AF.Copy, out=SfB[:, t, :], in_=Sst[:, 0, :]), reads=[t_Sf], writes=[t_SfB[t]])
                P.op("dve", lambda e: e.tensor_tensor(out=Sst[:, 0, :], in0=Sst[:, 0, :],
                                                      in1=gcfB.rearrange("p a b -> p (a b)"), op=ALU.mult),
                     reads=[t_Sf, t_gc], writes=[t_Sf])
                for bk in range(2):
                    P.op("dve", lambda e, bk=bk, par=par: e.tensor_copy(out=Uf[par][:, bk, :], in_=PS[5 + bk]),
                         reads=[t_ps[5 + bk]], writes=[t_Uf[par]])
                    Ub4 = Uf[par][:, bk, :].rearrange("p (a s c) -> p a s c", s=2, c=128)
                    ud = Ud[par][:, 2 * bk:2 * bk + 2, :, :]
                    for a_ in range(2):
                        P.op("dve", lambda e, Ub4=Ub4, ud=ud, a_=a_: e.tensor_scalar(
                            out=ud[:, a_], in0=Ub4[:, a_, :, 64:128], scalar1=cst[:, 6:7], scalar2=None, op0=ALU.mult),
                            reads=[t_Uf[par], t_cst], writes=[t_Ud[par]])
                        P.op("dve", lambda e, Ub4=Ub4, ud=ud, a_=a_: e.scalar_tensor_tensor(
                            out=ud[:, a_], in0=Ub4[:, a_, :, 0:64], scalar=cst[:, 5:6], in1=ud[:, a_], op0=ALU.mult, op1=ALU.add),
                            reads=[t_Uf[par], t_cst, t_Ud[par]], writes=[t_Ud[par]])
                P.op("dve", lambda e, par=par: e.tensor_tensor(out=S3[:, 0], in0=S3[:, 0], in1=Ud[par][:, :, 0, :], op=ALU.add),
                     reads=[t_Sf, t_Ud[par]], writes=[t_Sf])
                P.op("pool", lambda e, par=par, t=t: e.tensor_copy(out=UbS[:, t, :].rearrange("p (a c) -> p a c", c=64),
                                                                   in_=Ud[par][:, :, 1, :]),
                     reads=[t_Ud[par]], writes=[t_Ub[t]])

            own_pending.append(own_tail)
        while own_pending:
            own_pending.pop(0)()
        for t in range(NT - 1, -1, -1):
            P.op("act", lambda e, t=t: e.activation(func=AF.Copy, out=SbB[:, t, :], in_=Sst[:, 1, :]), reads=[t_Sb], writes=[t_SbB[t]])
            if t > 0:
                P.op("dve", lambda e: e.tensor_tensor(out=Sst[:, 1, :], in0=Sst[:, 1, :],
                                                      in1=gcbB.rearrange("p a b -> p (a b)"), op=ALU.mult),
                     reads=[t_Sb, t_gc], writes=[t_Sb])
                P.op("dve", lambda e, t=t: e.tensor_tensor(out=Sst[:, 1, :], in0=Sst[:, 1, :], in1=UbS[:, t, :], op=ALU.add),
                     reads=[t_Sb, t_Ub[t]], writes=[t_Sb])

        P.barrier()
        A.release()
        if stage == "ret2":
            P.final_wait("sp")
            P.emit_all()
            return nc
        sT_sb = [A.alloc([8, 128], BF16) for _ in range(2)]; t_sT = P.tiles_n(2, "sT")
        qfT = [A.alloc([4, 128], BF16) for _ in range(2)]; qbT = [A.alloc([4, 128], BF16) for _ in range(2)]
        t_qf = P.tiles_n(2, "qf"); t_qb = P.tiles_n(2, "qb")
        sq = [A.alloc([512], F32) for _ in range(2)]; t_sq = P.tiles_n(2, "sq")
        gst = [A.alloc([64], F32) for _ in range(2)]; t_gst = P.tiles_n(2, "gst")
        yc = [A.alloc([512], F32) for _ in range(2)]; t_yc = P.tiles_n(2, "yc")
        retb = [A.alloc([512], BF16) for _ in range(2)]; t_retb = P.tiles_n(2, "retb")
        retf = [A.alloc([512], F32) for _ in range(2)] if "ret" in dbg else None
        ret_pending = []
        for t in range(NT):
            par = t % 2
            b0 = 4 * par
            tsl = slice(t * 128, (t + 1) * 128)

            def fS(e, b0=b0, tsl=tsl):
                ins = None
                for h in range(8):
                    pr, hp = divmod(h, 2)
                    pl, ph = hp * 64, hp * 64 + 64
                    ins = e.matmul(PS[b0 + hp][:, pr * 128:pr * 128 + 128], lhsT=kT[pl:ph, pr, tsl],
                                   rhs=qT[pl:ph, pr, tsl], start=True, stop=True)
                return ins
            P.op("pe", fS, reads=[t_kT[t], t_qT[t]], writes=[t_ps[b0], t_ps[b0 + 1]])
            for bk in range(2):
                P.op("dve", lambda e, bk=bk, b0=b0, par=par: e.tensor_tensor(
                    out=sT_sb[par].rearrange("p (a s) b -> p a s b", s=2)[:, :, bk, :],
                    in0=PS[b0 + bk].rearrange("p (a b) -> p a b", b=128),
                    in1=Mmask.rearrange("p (a s) b -> p a s b", s=2)[:, :, bk, :], op=ALU.mult),
                    reads=[t_ps[b0 + bk], t_M], writes=[t_sT[par]])
            P.op("pool", lambda e, par=par, tsl=tsl: e.tensor_tensor(out=qfT[par], in0=qT[:, :, tsl], in1=Df, op=ALU.mult),
                 reads=[t_qT[t], t_D], writes=[t_qf[par]])
            P.op("pool", lambda e, par=par, tsl=tsl: e.tensor_tensor(out=qbT[par], in0=qT[:, :, tsl], in1=Db, op=ALU.mult),
                 reads=[t_qT[t], t_D], writes=[t_qb[par]])

            def ret_tail(b0=b0, par=par, t=t, tsl=tsl):
                def fY(e, b0=b0, par=par, t=t):
                    ins = None
                    for h in range(8):
                        pr, hp = divmod(h, 2)
                        pl, ph = hp * 64, hp * 64 + 64
                        o = PS[b0 + 2][:, h * 64:(h + 1) * 64]
                        e.matmul(o, lhsT=sT_sb[par][:, h, :], rhs=vS[:, t, h * 64:(h + 1) * 64], start=True, stop=False)
                        e.matmul(o, lhsT=qfT[par][pl:ph, pr, :], rhs=SfB[pl:ph, t, pr * 64:(pr + 1) * 64], start=False, stop=False)
                        ins = e.matmul(o, lhsT=qbT[par][pl:ph, pr, :], rhs=SbB[pl:ph, t, pr * 64:(pr + 1) * 64], start=False, stop=True)
                    return ins
                P.op("pe", fY, reads=[t_sT[par], t_v[t], t_qf[par], t_qb[par], t_SfB[t], t_SbB[t]], writes=[t_ps[b0 + 2]])
                y3 = v3(PS[b0 + 2])
                g = gst[par]
                P.op("dve", lambda e, y3=y3, g=g: e.tensor_reduce(out=g[:, 0:8], in_=y3, axis=AX.X, op=ALU.add),
                     reads=[t_ps[b0 + 2]], writes=[t_gst[par]])
                P.op("act", lambda e, b0=b0, par=par: e.activation(out=sq[par], in_=PS[b0 + 2], func=AF.Square),
                     reads=[t_ps[b0 + 2]], writes=[t_sq[par]])
                P.op("dve", lambda e, g=g, par=par: e.tensor_reduce(out=g[:, 8:16], in_=v3(sq[par]), axis=AX.X, op=ALU.add),
                     reads=[t_sq[par]], writes=[t_gst[par]])
                P.op("dve", lambda e, g=g: e.tensor_scalar(out=g[:, 16:24], in0=g[:, 0:8], scalar1=1.0 / 64, scalar2=None,
                                                           op0=ALU.mult), reads=[t_gst[par]], writes=[t_gst[par]])
                P.op("dve", lambda e, g=g: e.tensor_tensor(out=g[:, 24:32], in0=g[:, 16:24], in1=g[:, 16:24], op=ALU.mult),
                     reads=[t_gst[par]], writes=[t_gst[par]])
                P.op("dve", lambda e, g=g: e.scalar_tensor_tensor(out=g[:, 32:40], in0=g[:, 8:16], scalar=1.0 / 64,
                                                                  in1=g[:, 24:32], op0=ALU.mult, op1=ALU.subtract),
                     reads=[t_gst[par]], writes=[t_gst[par]])
                P.op("act", lambda e, g=g: e.activation(out=g[:, 40:48], in_=g[:, 32:40], func=AF.Ln, bias=cst[:, 3:4], scale=1.0),
                     reads=[t_gst[par], t_cst], writes=[t_gst[par]])
                P.op("act", lambda e, g=g: e.activation(out=g[:, 40:48], in_=g[:, 40:48], func=AF.Exp, scale=-0.5),
                     reads=[t_gst[par]], writes=[t_gst[par]])
                P.op("dve", lambda e, g=g, y3=y3, par=par: e.tensor_tensor(out=v3(yc[par]), in0=y3, in1=bc_last(g[:, 16:24], 64),
                                                                           op=ALU.subtract),
                     reads=[t_ps[b0 + 2], t_gst[par]], writes=[t_yc[par]])
                P.op("pool", lambda e, g=g, par=par: e.tensor_tensor(out=v3(yc[par]), in0=v3(yc[par]), in1=bc_last(g[:, 40:48], 64),
                                                                     op=ALU.mult), reads=[t_yc[par], t_gst[par]], writes=[t_yc[par]])
                P.op("pool", lambda e, par=par, t=t: e.tensor_tensor(out=retb[par], in0=yc[par], in1=GS[:, t, :], op=ALU.mult),
                     reads=[t_yc[par], t_G[t]], writes=[t_retb[par]])
                if "ret" in dbg:
                    P.op("dve", lambda e, par=par: e.tensor_copy(out=retf[par], in_=retb[par]), reads=[t_retb[par]], writes=[t_yc[par]])
                    P.dma("sp", lambda e, par=par, tsl=tsl: e.dma_start(out=dbg["ret"][tsl, :], in_=retf[par]),
                          reads=[t_yc[par]], sem_tile=t_yc[par])

                P.dma("sp", lambda e, par=par, tsl=tsl: e.dma_start(out=cat[tsl, 0:512], in_=retb[par]),
                      reads=[t_retb[par]], writes=[t_cat[0][t]], sem_tile=t_retb[par])

            ret_pending.append(ret_tail)
            if len(ret_pending) > 1:
                ret_pending.pop(0)()
        while ret_pending:
            ret_pending.pop(0)()
        P.barrier()
        A.release()
        if stage == "ret":
            P.final_wait("sp")
            P.emit_all()
            return nc

        A.mark()
        NKB = 20
        w_na = A.alloc([8, 1536], BF16); t_wna = P.tiles_n(3, "wna")
        for g in range(3):
            P.dma("pool", lambda e, g=g: e.dma_start(
                out=w_na[:, :, g * 512:(g + 1) * 512],
                in_=w_in[:, 2048 + g * 512:2048 + (g + 1) * 512].rearrange("(kc k) n -> k kc n", k=128)), writes=[t_wna[g]])
        nqT = A.alloc([4, TOK], BF16); nkT = A.alloc([4, NKB * 128], BF16)
        Vaug = A.alloc([NKB, 8 * 65], BF16)
        na_sb = A.alloc([NT, 512], BF16)
        t_nq = P.tiles_n(NT, "nq"); t_nk = P.tiles_n(NKB, "nk"); t_V = P.tiles_n(NKB, "V"); t_na = P.tiles_n(NT, "na")
        V4 = Vaug.rearrange("p a (h c) -> p a h c", c=65)
        for kb in range(NKB):
            P.op("pool", lambda e, kb=kb: e.memset(V4[:, kb, :, 64:65], 1.0), writes=[t_V[kb]])
        nxs = [A.alloc([8, 256], BF16) for _ in range(2)]; t_nxs = P.tiles_n(2, "xsn")
        nq_tm = [A.alloc([512], BF16) for _ in range(2)]; nk_tm = [A.alloc([512], BF16) for _ in range(2)]
        t_nqtm = P.tiles_n(2, "nqtm"); t_nktm = P.tiles_n(2, "nktm")

        def na_inproj(ps_i, xs_ap, g, reads):
            def f(e):
                ins = None
                for kc in range(8):
                    ins = e.matmul(PS[ps_i], lhsT=xs_ap(kc), rhs=w_na[:, kc, g * 512:(g + 1) * 512],
                                   start=(kc == 0), stop=(kc == 7))
                return ins
            P.op("pe", f, reads=reads, writes=[t_ps[ps_i]])

        chunks = [xh[:, 0:256]] + [xT[:, c * 256:(c + 1) * 256] for c in range(8)] + [xh[:, 256:512]]
        for kb in range(NKB):
            ch, sub = divmod(kb, 2)
            par = kb % 2
            own = 2 <= kb < 18
            if sub == 0:
                P.dma("pool", lambda e, ch=ch: e.dma_start(
                    out=nxs[ch % 2], in_=chunks[ch].rearrange("(kc k) n -> k kc n", k=128)), writes=[t_nxs[ch % 2]])
            xs_ap = lambda kc, ch=ch, sub=sub: nxs[ch % 2][:, kc, sub * 128:(sub + 1) * 128]
            if own:
                t = kb - 2
                na_inproj(0, xs_ap, 0, [t_nxs[ch % 2], t_wna[0]])
                P.op("act", lambda e, par=par: e.activation(func=AF.Copy, out=nq_tm[par], in_=PS[0]), reads=[t_ps[0]], writes=[t_nqtm[par]])

                def fTq(e, par=par):
                    ins = None
                    for pr in range(4):
                        ins = e.transpose(out=PSB[3][:, pr * 128:(pr + 1) * 128], in_=nq_tm[par][:, pr * 128:(pr + 1) * 128],
                                          identity=ident_b)
                    return ins
                P.op("pe", fTq, reads=[t_nqtm[par], t_ident], writes=[t_ps[3]])
                P.op("dve", lambda e, t=t: e.tensor_copy(out=nqT[:, :, t * 128:(t + 1) * 128],
                                                         in_=PSB[3][:, 0:512].rearrange("p (a b) -> p a b", b=128)),
                     reads=[t_ps[3]], writes=[t_nq[t]])
            na_inproj(1, xs_ap, 1, [t_nxs[ch % 2], t_wna[1]])
            na_inproj(2, xs_ap, 2, [t_nxs[ch % 2], t_wna[2]])
            P.op("act", lambda e, par=par: e.activation(func=AF.Copy, out=nk_tm[par], in_=PS[1]), reads=[t_ps[1]], writes=[t_nktm[par]])

            def fTk(e, par=par):
                ins = None
                for pr in range(4):
                    ins = e.transpose(out=PSB[4][:, pr * 128:(pr + 1) * 128], in_=nk_tm[par][:, pr * 128:(pr + 1) * 128],
                                      identity=ident_b)
                return ins
            P.op("pe", fTk, reads=[t_nktm[par], t_ident], writes=[t_ps[4]])
            P.op("dve", lambda e, kb=kb: e.tensor_copy(out=nkT[:, :, kb * 128:(kb + 1) * 128],
                                                       in_=PSB[4][:, 0:512].rearrange("p (a b) -> p a b", b=128)),
                 reads=[t_ps[4]], writes=[t_nk[kb]])
            P.op("dve", lambda e, kb=kb: e.tensor_copy(out=V4[:, kb, :, 0:64], in_=v3(PS[2])),
                 reads=[t_ps[2]], writes=[t_V[kb]])
        Est = [A.alloc([NA_BLOCKS * 128], F32) for _ in range(2)]; t_Est = P.tiles_n(2, "Est")
        Ebf = [A.alloc([NA_BLOCKS, 128], BF16) for _ in range(2)]; t_Ebf = P.tiles_n(2, "Ebf")
        nes = [A.alloc([768], BF16) for _ in range(2)]; t_nes = P.tiles_n(2, "nes")
        pTt = [A.alloc([768], BF16) for _ in range(2)]; t_pT = P.tiles_n(2, "pT")
        rcp = [A.alloc([2], F32) for _ in range(4)]; t_rcp = P.tiles_n(4, "rcp")
        na_pending = []
        for h in range(8):
            hp2 = h % 2
            pr, hp = divmod(h, 2)
            pl, ph = hp * 64, hp * 64 + 64
            P.dma("sp", lambda e, h=h, hp2=hp2: e.dma_start(out=Est[hp2], in_=natab[h]), writes=[t_Est[hp2]])
            P.op("act", lambda e, hp2=hp2: e.activation(out=Ebf[hp2].rearrange("p a b -> p (a b)"), in_=Est[hp2], func=AF.Exp),
                 reads=[t_Est[hp2]], writes=[t_Ebf[hp2]])
            for i in range(NT):
                kt0, nk, b0 = NA_TYPES[i]
                n = h * NT + i
                par = n % 2
                bX, bY = 2 * par, 2 * par + 1
                bO = 4 + n % 4
                qsl = slice(i * 128, (i + 1) * 128)

                def fS(e, kt0=kt0, nk=nk, bX=bX, bY=bY, pl=pl, ph=ph, pr=pr, qsl=qsl):
                    ins = None
                    for idx in range(nk):
                        kt = kt0 + idx
                        bank = PS[bX] if idx < 4 else PS[bY]
                        ins = e.matmul(bank[:, (idx % 4) * 128:(idx % 4) * 128 + 128], lhsT=nkT[pl:ph, pr, kt * 128:(kt + 1) * 128],
                                       rhs=nqT[pl:ph, pr, qsl], start=True, stop=True)
                    return ins
                P.op("pe", fS, reads=[t_nq[i]] + t_nk[kt0:kt0 + nk], writes=[t_ps[bX], t_ps[bY]])
                P.op("act", lambda e, par=par, bX=bX: e.activation(out=nes[par][:, 0:512], in_=PS[bX], func=AF.Exp, scale=0.125),
                     reads=[t_ps[bX]], writes=[t_nes[par]])
                w2 = (nk - 4) * 128
                P.op("act", lambda e, par=par, bY=bY, w2=w2: e.activation(out=nes[par][:, 512:512 + w2], in_=PS[bY][:, 0:w2],
                                                                        func=AF.Exp, scale=0.125),
                     reads=[t_ps[bY]], writes=[t_nes[par]])
                P.op("pool", lambda e, par=par, nk=nk, b0=b0, hp2=hp2: e.tensor_tensor(
                    out=pTt[par][:, 0:nk * 128], in0=nes[par][:, 0:nk * 128],
                    in1=Ebf[hp2][:, b0:b0 + nk, :].rearrange("p a b -> p (a b)"), op=ALU.mult),
                    reads=[t_nes[par], t_Ebf[hp2]], writes=[t_pT[par]])

                def na_tail(kt0=kt0, nk=nk, par=par, bO=bO, h=h, n=n, i=i):
                    def fO(e):
                        ins = None
                        for idx in range(nk):
                            ins = e.matmul(PS[bO][:, 0:65], lhsT=pTt[par][:, idx * 128:(idx + 1) * 128],
                                           rhs=Vaug[:, kt0 + idx, h * 65:(h + 1) * 65], start=(idx == 0), stop=(idx == nk - 1))
                        return ins
                    P.op("pe", fO, reads=[t_pT[par]] + t_V[kt0:kt0 + nk], writes=[t_ps[bO]])
                    r4 = n % 4
                    P.op("dve", lambda e: e.reciprocal(out=rcp[r4][:, 0:1], in_=PS[bO][:, 64:65]),
                         reads=[t_ps[bO]], writes=[t_rcp[r4]])
                    P.op("dve", lambda e: e.tensor_scalar(
                        out=na_sb[:, i, h * 64:(h + 1) * 64], in0=PS[bO][:, 0:64], scalar1=rcp[r4][:, 0:1], scalar2=None, op0=ALU.mult),
                        reads=[t_ps[bO], t_rcp[r4]], writes=[t_na[i]])
                na_pending.append(na_tail)
                if len(na_pending) > 1:
                    na_pending.pop(0)()
        while na_pending:
            na_pending.pop(0)()
        naf = [A.alloc([512], F32) for _ in range(2)] if "na" in dbg else None
        t_naf = P.tiles_n(2, "naf")
        for i in range(NT):
            tsl = slice(i * 128, (i + 1) * 128)
            P.dma("sp", lambda e, i=i, tsl=tsl: e.dma_start(out=cat[tsl, 512:1024], in_=na_sb[:, i, :]),
                  reads=[t_na[i]], writes=[t_cat[1][i]], sem_tile=P.tile("nast%d" % (i % 4)) if i >= 4 else P.tile("nast%d" % i))
            if "na" in dbg:
                P.op("dve", lambda e, i=i: e.tensor_copy(out=naf[i % 2], in_=na_sb[:, i, :]), reads=[t_na[i]], writes=[t_naf[i % 2]])
                P.dma("sp", lambda e, i=i, tsl=tsl: e.dma_start(out=dbg["na"][tsl, :], in_=naf[i % 2]),
                      reads=[t_naf[i % 2]], sem_tile=t_naf[i % 2])
        P.barrier()
        A.release()
        if stage == "na":
            P.final_wait("sp")
            P.emit_all()
            return nc

        x1T = A.alloc([8, TOK], BF16)
        t_x1T = P.tiles_n(NT, "x1T")
        t_GT = P.tiles_n(NT, "GT"); t_base = P.tiles_n(NT, "base")
        A.mark()
        w_o = A.alloc([8, 1024], BF16); w_pgt = A.alloc([8, 1024], BF16); w_ppt = A.alloc([2, 1024], BF16)
        pTb = A.alloc([2, TOK], BF16)
        w_rt = A.alloc([8, 64], F32)
        g1 = A.alloc([1024], F32); b1 = A.alloc([1024], F32); rb = A.alloc([64], F32)
        t_wl = P.tiles_n(9, "mw")
        P.dma("pool", lambda e: e.dma_start(out=w_o, in_=w_out.rearrange("(kc k) n -> k kc n", k=128)), writes=[t_wl[0]])
        P.dma("pool", lambda e: e.dma_start(out=pTb, in_=pT.rearrange("(kc k) n -> k kc n", k=128)), writes=[t_wl[1]])
        P.dma("pool", lambda e: e.dma_start(out=w_ppt, in_=w_pp.rearrange("(kc k) n -> k kc n", k=128)), writes=[t_wl[2]])
        P.dma("pool", lambda e: e.dma_start(out=w_pgt, in_=w_pg.rearrange("(kc k) n -> k kc n", k=128)), writes=[t_wl[3]])
        P.dma("sp", lambda e: e.dma_start(out=w_rt, in_=w_router.rearrange("(kc k) n -> k kc n", k=128)), writes=[t_wl[4]])
        P.dma("sp", lambda e: e.dma_start(out=g1, in_=ln1_g.partition_broadcast(128)), writes=[t_wl[5]])
        P.dma("sp", lambda e: e.dma_start(out=b1, in_=ln1_b.partition_broadcast(128)), writes=[t_wl[6]])
        P.dma("sp", lambda e: e.dma_start(out=rb, in_=rbias.partition_broadcast(128)), writes=[t_wl[7]])
        catb = [A.alloc([1024], BF16) for _ in range(2)]; t_catb = P.tiles_n(2, "catb")
        catT = [A.alloc([8, 128], BF16) for _ in range(2)]; t_catT = P.tiles_n(2, "catT")
        xrt = [A.alloc([1024], F32) for _ in range(2)]; t_xrt = P.tiles_n(2, "xrt")
        zt = [A.alloc([1024], F32) for _ in range(2)]; t_zt = P.tiles_n(2, "zt")
        x1t = [A.alloc([1024], F32) for _ in range(2)]; t_x1t = P.tiles_n(2, "x1t")
        x1T32 = [A.alloc([8, 128], F32) for _ in range(2)]; t_x1T32 = P.tiles_n(2, "x1T32")
        lst = [A.alloc([32], F32) for _ in range(2)]; t_lst = P.tiles_n(2, "lst")
        rt = [A.alloc([768], F32) for _ in range(2)]; t_rt = P.tiles_n(2, "rt")
        gTs = [A.alloc([128], F32) for _ in range(2)]; t_gTs = P.tiles_n(2, "gTs")
        sgp = [A.alloc([1024], F32) for _ in range(2)]; t_sgp = P.tiles_n(2, "sgp")
        bst = [A.alloc([1024], F32) for _ in range(2)]; t_bst = P.tiles_n(2, "bst")

        def layer_norm(src, t_src, dst, t_dst, st, t_st, gain, bias, t_gb):
            for hf_ in range(2):
                P.op("dve", lambda e, hf_=hf_: e.bn_stats(out=st[:, hf_ * 6:(hf_ + 1) * 6], in_=src[:, hf_ * 512:(hf_ + 1) * 512]),
                     reads=[t_src], writes=[t_st])
            P.op("dve", lambda e: e.bn_aggr(out=st[:, 12:14], in_=st[:, 0:12]), reads=[t_st], writes=[t_st])
            P.op("act", lambda e: e.activation(out=st[:, 14:15], in_=st[:, 13:14], func=AF.Ln, bias=cst[:, 4:5], scale=1.0),
                 reads=[t_st, t_cst], writes=[t_st])
            P.op("act", lambda e: e.activation(out=st[:, 14:15], in_=st[:, 14:15], func=AF.Exp, scale=-0.5),
                 reads=[t_st], writes=[t_st])
            P.op("dve", lambda e: e.tensor_scalar(out=dst, in0=src, scalar1=st[:, 12:13], scalar2=st[:, 14:15],
                                                  op0=ALU.subtract, op1=ALU.mult), reads=[t_src, t_st], writes=[t_dst])
            P.op("pool", lambda e: e.tensor_tensor(out=dst, in0=dst, in1=gain, op=ALU.mult), reads=[t_dst] + t_gb, writes=[t_dst])
            P.op("pool", lambda e: e.tensor_tensor(out=dst, in0=dst, in1=bias, op=ALU.add), reads=[t_dst] + t_gb, writes=[t_dst])

        m_pending = []
        m_pendA2 = []
        m_stB = []
        for t in range(NT):
            par = t % 2
            tsl = slice(t * 128, (t + 1) * 128)
            def stageA(t=t, par=par, tsl=tsl):
                P.dma("pool", lambda e, par=par, tsl=tsl: e.dma_start(out=catb[par], in_=cat[tsl, :]),
                      reads=[t_cat[0][t], t_cat[1][t]], writes=[t_catb[par]])
                P.dma("pool", lambda e, par=par, tsl=tsl: e.dma_start(out=xrt[par], in_=xr[tsl, :]), writes=[t_xrt[par]])

                def fCT(e, par=par):
                    ins = None
                    for c in range(8):
                        ins = e.transpose(out=PSB[0][:, c * 128:(c + 1) * 128], in_=catb[par][:, c * 128:(c + 1) * 128], identity=ident_b)
                    return ins
                P.op("pe", fCT, reads=[t_catb[par], t_ident], writes=[t_ps[0]])
                P.op("dve", lambda e, par=par: e.tensor_copy(out=catT[par].rearrange("p a b -> p (a b)"), in_=PSB[0]),
                     reads=[t_ps[0]], writes=[t_catT[par]])
                for half in range(2):
                    def fM(e, par=par, half=half):
                        ins = None
                        for kc in range(8):
                            ins = e.matmul(PS[1 + half], lhsT=catT[par][:, kc, :], rhs=w_o[:, kc, half * 512:(half + 1) * 512],
                                           start=(kc == 0), stop=(kc == 7))
                        return ins
                    P.op("pe", fM, reads=[t_catT[par], t_wl[0]], writes=[t_ps[1 + half]])
                    P.op("dve", lambda e, par=par, half=half: e.scalar_tensor_tensor(
                        out=zt[par][:, half * 512:(half + 1) * 512], in0=xrt[par][:, half * 512:(half + 1) * 512], scalar=ALPHA,
                        in1=PS[1 + half], op0=ALU.mult, op1=ALU.add), reads=[t_xrt[par], t_ps[1 + half]], writes=[t_zt[par]])

            def stageA2(t=t, par=par, tsl=tsl):
                layer_norm(zt[par], t_zt[par], x1t[par], t_x1t[par], lst[par], t_lst[par], g1, b1, [t_wl[5], t_wl[6]])
                if "x1" in dbg:
                    P.dma("sp", lambda e, par=par, tsl=tsl: e.dma_start(out=dbg["x1"][tsl, :], in_=x1t[par]),
                          reads=[t_x1t[par]], sem_tile=t_x1t[par])
                for half in range(2):
                    def fXT(e, par=par, half=half):
                        ins = None
                        for c in range(4):
                            cc_ = half * 4 + c
                            ins = e.transpose(out=PS[3 + half][:, c * 128:(c + 1) * 128], in_=x1t[par][:, cc_ * 128:(cc_ + 1) * 128],
                                              identity=ident_f)
                        return ins
                    P.op("pe", fXT, reads=[t_x1t[par], t_ident], writes=[t_ps[3 + half]])
                    P.op("dve", lambda e, half=half, tsl=tsl: e.tensor_copy(
                        out=x1T[:, half * 4:half * 4 + 4, tsl], in_=PS[3 + half].rearrange("p (a b) -> p a b", b=128)),
                        reads=[t_ps[3 + half]], writes=[t_x1T[t]])
                    P.op("act", lambda e, half=half, par=par: e.copy(
                        out=x1T32[par].rearrange("p a b -> p (a b)")[:, half * 512:(half + 1) * 512], in_=PS[3 + half]),
                        reads=[t_ps[3 + half]], writes=[t_x1T32[par]])


            def stageB(t=t, par=par, tsl=tsl):
                def fR(e, par=par):
                    ins = None
                    for kc in range(8):
                        ins = e.matmul(PS[5][:, 0:64], lhsT=x1T32[par][:, kc, :], rhs=w_rt[:, kc, :], start=(kc == 0), stop=(kc == 7))
                    return ins
                P.op("pe", fR, reads=[t_x1T32[par], t_wl[4]], writes=[t_ps[5]])
                R = rt[par]
                sc, bi, eq, bi2 = R[:, 0:64], R[:, 64:128], R[:, 128:192], R[:, 192:256]
                m1, m2, gs, t8a, gm, pen = R[:, 256:264], R[:, 264:272], R[:, 272:280], R[:, 280:288], R[:, 288:296], R[:, 296:304]
                msk, t8b, sel, wv, den, gate = R[:, 320:384], R[:, 304:312], R[:, 384:448], R[:, 448:512], R[:, 312:314], R[:, 512:576]
                g3 = lambda ap: ap.rearrange("p (a b) -> p a b", b=8)
                tr_ = [t_rt[par]]
                P.op("act", lambda e, sc=sc: e.activation(out=sc, in_=PS[5][:, 0:64], func=AF.Sigmoid), reads=[t_ps[5]], writes=tr_)
                P.op("dve", lambda e, sc=sc, bi=bi: e.tensor_tensor(out=bi, in0=sc, in1=rb, op=ALU.add), reads=tr_ + [t_wl[7]], writes=tr_)
                P.op("dve", lambda e, bi=bi, m1=m1: e.tensor_reduce(out=m1, in_=g3(bi), axis=AX.X, op=ALU.max), reads=tr_, writes=tr_)
                P.op("dve", lambda e, bi=bi, m1=m1, eq=eq: e.tensor_tensor(out=g3(eq), in0=g3(bi), in1=bc_last(m1, 8), op=ALU.is_equal),
                     reads=tr_, writes=tr_)
                P.op("dve", lambda e, bi=bi, eq=eq, bi2=bi2: e.scalar_tensor_tensor(out=bi2, in0=eq, scalar=-1e9, in1=bi,
                                                                                    op0=ALU.mult, op1=ALU.add), reads=tr_, writes=tr_)
                P.op("dve", lambda e, bi2=bi2, m2=m2: e.tensor_reduce(out=m2, in_=g3(bi2), axis=AX.X, op=ALU.max), reads=tr_, writes=tr_)
                P.op("dve", lambda e, m1=m1, m2=m2, gs=gs: e.tensor_tensor(out=gs, in0=m1, in1=m2, op=ALU.add), reads=tr_, writes=tr_)
                P.op("dve", lambda e, gs=gs, t8a=t8a: e.max(out=t8a, in_=gs), reads=tr_, writes=tr_)
                P.op("dve", lambda e, gs=gs, t8a=t8a, gm=gm: e.tensor_scalar(out=gm, in0=gs, scalar1=t8a[:, 3:4], scalar2=None, op0=ALU.is_ge),
                     reads=tr_, writes=tr_)
                P.op("dve", lambda e, gm=gm, pen=pen: e.tensor_scalar(out=pen, in0=gm, scalar1=-1.0, scalar2=1e9, op0=ALU.add, op1=ALU.mult),
                     reads=tr_, writes=tr_)
                P.op("dve", lambda e, bi=bi, pen=pen, msk=msk: e.tensor_tensor(out=g3(msk), in0=g3(bi), in1=bc_last(pen, 8), op=ALU.add),
                     reads=tr_, writes=tr_)
                P.op("dve", lambda e, msk=msk, t8b=t8b: e.max(out=t8b, in_=msk), reads=tr_, writes=tr_)
                P.op("dve", lambda e, msk=msk, t8b=t8b, sel=sel: e.tensor_scalar(out=sel, in0=msk, scalar1=t8b[:, 7:8], scalar2=None,
                                                                                 op0=ALU.is_ge), reads=tr_, writes=tr_)
                P.op("dve", lambda e, sc=sc, sel=sel, wv=wv: e.tensor_tensor(out=wv, in0=sc, in1=sel, op=ALU.mult), reads=tr_, writes=tr_)
                P.op("dve", lambda e, wv=wv, den=den: e.tensor_reduce(out=den[:, 0:1], in_=wv, axis=AX.X, op=ALU.add), reads=tr_, writes=tr_)
                P.op("dve", lambda e, den=den: e.reciprocal(out=den[:, 1:2], in_=den[:, 0:1]), reads=tr_, writes=tr_)
                P.op("dve", lambda e, wv=wv, den=den, gate=gate: e.tensor_scalar(out=gate, in0=wv, scalar1=den[:, 1:2], scalar2=2.5,
                                                                                 op0=ALU.mult, op1=ALU.mult), reads=tr_, writes=tr_)
                if "gate" in dbg:
                    P.dma("sp", lambda e, gate=gate, tsl=tsl: e.dma_start(out=dbg["gate"][tsl, :], in_=gate), reads=tr_, sem_tile=t_rt[par])
                P.op("pe", lambda e, gate=gate: e.transpose(out=PS[5][0:64, 128:256], in_=gate, identity=ident_f),
                     reads=tr_ + [t_ident], writes=[t_ps[5]])
                P.op("act", lambda e, par=par: e.activation(func=AF.Copy, out=gTs[par][0:64, :], in_=PS[5][0:64, 128:256]), reads=[t_ps[5]], writes=[t_gTs[par]])
                P.dma("sp", lambda e, par=par, tsl=tsl: e.dma_start(out=GT[:, tsl], in_=gTs[par][0:64, :]),
                      reads=[t_gTs[par]], writes=[t_GT[t]], sem_tile=t_gTs[par])
                for half in range(2):
                    hs = slice(half * 512, (half + 1) * 512)

                    def fPG(e, half=half, tsl=tsl, hs=hs):
                        ins = None
                        for kc in range(8):
                            ins = e.matmul(PS[6 + half], lhsT=x1T[:, kc, tsl], rhs=w_pgt[:, kc, hs], start=(kc == 0), stop=(kc == 7))
                        return ins
                    P.op("pe", fPG, reads=[t_x1T[t], t_wl[3]], writes=[t_ps[6 + half]])
                    P.op("act", lambda e, half=half, par=par, hs=hs: e.activation(out=sgp[par][:, hs], in_=PS[6 + half], func=AF.Sigmoid),
                         reads=[t_ps[6 + half]], writes=[t_sgp[par]])

                    def fPP(e, half=half, tsl=tsl, hs=hs):
                        ins = None
                        for kc in range(2):
                            ins = e.matmul(PS[6 + half], lhsT=pTb[:, kc, tsl], rhs=w_ppt[:, kc, hs], start=(kc == 0), stop=(kc == 1))
                        return ins
                    P.op("pe", fPP, reads=[t_wl[1], t_wl[2]], writes=[t_ps[6 + half]])
                    P.op("dve", lambda e, half=half, par=par, hs=hs: e.tensor_tensor(out=bst[par][:, hs], in0=PS[6 + half], in1=sgp[par][:, hs],
                                                                                     op=ALU.mult),
                         reads=[t_ps[6 + half], t_sgp[par]], writes=[t_bst[par]])
                P.op("dve", lambda e, par=par: e.scalar_tensor_tensor(out=bst[par], in0=x1t[par], scalar=ALPHA, in1=bst[par],
                                                                      op0=ALU.mult, op1=ALU.add),
                     reads=[t_x1t[par], t_bst[par]], writes=[t_bst[par]])
                P.dma("sp", lambda e, par=par, tsl=tsl: e.dma_start(out=base[tsl, :], in_=bst[par]),
                      reads=[t_bst[par]], writes=[t_base[t]], sem_tile=t_bst[par])

            stageA()
            m_pendA2.append(stageA2)
            if len(m_pendA2) > 1:
                m_pendA2.pop(0)()
                m_pending.append(m_stB.pop(0))
            m_stB.append(stageB)
            if len(m_pending) > 1:
                m_pending.pop(0)()
        while m_pendA2:
            m_pendA2.pop(0)()
            m_pending.append(m_stB.pop(0))
            if len(m_pending) > 1:
                m_pending.pop(0)()
        while m_pending:
            m_pending.pop(0)()
        P.barrier()
        A.release()
        if stage == "x1":
            P.final_wait("sp")
            P.emit_all()
            return nc

        y_acc = A.alloc([NT, 1024], F32); t_y = P.tiles_n(NT, "yacc")
        A.mark()
        m_Wgu = [A.alloc([8, 512], BF16) for _ in range(4)]; m_Wdn = [A.alloc([2, 1024], BF16) for _ in range(4)]
        t_mWgu = P.tiles_n(4, "mWgu"); t_mWdn = P.tiles_n(4, "mWdn")
        m_gbc = [A.alloc([TOK], F32) for _ in range(4)]; t_mgbc = P.tiles_n(4, "mgbc")
        m_act = A.alloc([4, TOK], BF16)
        t_mact = [P.tiles_n(4, "mact%d_" % k_) for k_ in range(4)]
        m_sg = [A.alloc([512], BF16) for _ in range(2)]; m_tt = [A.alloc([512], BF16) for _ in range(2)]
        t_msg = P.tiles_n(2, "msg"); t_mtt = P.tiles_n(2, "mtt")

        def m_load(ex):
            s_ = ex % 4
            src_gu = w_egu[ex] if ex < NEXP else w_sgu
            src_dn = w_edn[ex] if ex < NEXP else w_sdn
            P.dma("pool", lambda eng, s_=s_, src_gu=src_gu: eng.dma_start(
                out=m_Wgu[s_], in_=src_gu.rearrange("(kc k) n -> k kc n", k=128)), writes=[t_mWgu[s_]])
            P.dma("pool", lambda eng, s_=s_, src_dn=src_dn: eng.dma_start(
                out=m_Wdn[s_], in_=src_dn.rearrange("(kc k) n -> k kc n", k=128)), writes=[t_mWdn[s_]])
            if ex < NEXP:
                P.dma("sp", lambda eng, ex=ex: eng.dma_start(out=m_gbc[ex % 4], in_=GT[ex:ex + 1, :].partition_broadcast(128)),
                      reads=t_GT, writes=[t_mgbc[ex % 4]])

        m_groups = [(2 * g_, 2 * g_ + 1) for g_ in range(NEXP // 2)] + [(NEXP,)]
        for ex in m_groups[0]:
            m_load(ex)
        m_cnt = 0
        for gi, grp in enumerate(m_groups):
            if gi + 1 < len(m_groups):
                for ex in m_groups[gi + 1]:
                    m_load(ex)
            for eg, ex in enumerate(grp):
                s_ = ex % 4
                for tg in range(4):
                    tgs = slice(tg * 512, (tg + 1) * 512)
                    for j in range(2):
                        q_ = m_cnt % 2
                        m_cnt += 1
                        bg, bu = 2 * q_, 2 * q_ + 1

                        def fUp(eng, s_=s_, j=j, tgs=tgs, bg=bg, bu=bu):
                            ins = None
                            for kc in range(8):
                                eng.matmul(PS[bg], lhsT=m_Wgu[s_][:, kc, j * 128:(j + 1) * 128], rhs=x1T[:, kc, tgs],
                                           start=(kc == 0), stop=(kc == 7))
                            for kc in range(8):
                                ins = eng.matmul(PS[bu], lhsT=m_Wgu[s_][:, kc, 256 + j * 128:256 + (j + 1) * 128], rhs=x1T[:, kc, tgs],
                                                 start=(kc == 0), stop=(kc == 7))
                            return ins
                        P.op("pe", fUp, reads=[t_mWgu[s_]] + t_x1T[4 * tg:4 * tg + 4], writes=[t_ps[bg], t_ps[bu]])
                        P.op("act", lambda eng, q_=q_, bg=bg: eng.activation(out=m_sg[q_], in_=PS[bg], func=AF.Silu),
                             reads=[t_ps[bg]], writes=[t_msg[q_]])
                        if ex < NEXP:
                            P.op("dve", lambda eng, q_=q_, bu=bu, ex=ex, tgs=tgs: eng.tensor_tensor(
                                out=m_tt[q_], in0=PS[bu], in1=m_gbc[ex % 4][:, tgs], op=ALU.mult),
                                reads=[t_ps[bu], t_mgbc[ex % 4]], writes=[t_mtt[q_]])
                        else:
                            P.op("dve", lambda eng, q_=q_, bu=bu: eng.tensor_copy(out=m_tt[q_], in_=PS[bu]),
                                 reads=[t_ps[bu]], writes=[t_mtt[q_]])
                        P.op("pool", lambda eng, q_=q_, eg=eg, j=j, tgs=tgs: eng.tensor_tensor(
                            out=m_act[:, eg * 2 + j, tgs], in0=m_sg[q_], in1=m_tt[q_], op=ALU.mult),
                            reads=[t_msg[q_], t_mtt[q_]], writes=[t_mact[eg * 2 + j][tg]])
            for t in range(NT):
                yq = t % 2
                tsl = slice(t * 128, (t + 1) * 128)
                for half in range(2):
                    by = 4 + 2 * yq + half
                    hs = slice(half * 512, (half + 1) * 512)

                    def fDn(eng, grp=grp, tsl=tsl, by=by, hs=hs):
                        ins = None
                        n_ = len(grp) * 2
                        k_ = 0
                        for eg, ex in enumerate(grp):
                            for j in range(2):
                                ins = eng.matmul(PS[by], lhsT=m_act[:, eg * 2 + j, tsl], rhs=m_Wdn[ex % 4][:, j, hs],
                                                 start=(k_ == 0), stop=(k_ == n_ - 1))
                                k_ += 1
                        return ins
                    rd_ = [t_mact[eg * 2 + j][t // 4] for eg in range(len(grp)) for j in range(2)] + [t_mWdn[ex % 4] for ex in grp]
                    P.op("pe", fDn, reads=rd_, writes=[t_ps[by]])
                    if gi == 0:
                        P.op("dve", lambda eng, t=t, hs=hs, by=by: eng.tensor_copy(out=y_acc[:, t, hs], in_=PS[by]),
                             reads=[t_ps[by]], writes=[t_y[t]])
                    else:
                        P.op("dve", lambda eng, t=t, hs=hs, by=by: eng.tensor_tensor(out=y_acc[:, t, hs], in0=PS[by], in1=y_acc[:, t, hs],
                                                                                     op=ALU.add),
                             reads=[t_ps[by], t_y[t]], writes=[t_y[t]])
        P.barrier()
        A.release()
        g2 = A.alloc([1024], F32); b2 = A.alloc([1024], F32)
        t_f = P.tiles_n(2, "fgb")
        P.dma("sp", lambda eng: eng.dma_start(out=g2, in_=ln2_g.partition_broadcast(128)), writes=[t_f[0]])
        P.dma("sp", lambda eng: eng.dma_start(out=b2, in_=ln2_b.partition_broadcast(128)), writes=[t_f[1]])
        m_bt = [A.alloc([1024], F32) for _ in range(4)]; m_z2 = [A.alloc([1024], F32) for _ in range(4)]
        m_o = [A.alloc([1024], F32) for _ in range(4)]; m_st = [A.alloc([32], F32) for _ in range(4)]
        t_mbt = P.tiles_n(4, "mbt"); t_mz2 = P.tiles_n(4, "mz2"); t_mo = P.tiles_n(4, "mo"); t_mst = P.tiles_n(4, "mst")
        fin_pending = []
        for t in range(NT):
            par = t % 4
            tsl = slice(t * 128, (t + 1) * 128)
            P.dma("pool", lambda eng, par=par, tsl=tsl: eng.dma_start(out=m_bt[par], in_=base[tsl, :]),
                  reads=[t_base[t]], writes=[t_mbt[par]])
            P.op("pool", lambda eng, par=par, t=t: eng.tensor_tensor(out=m_z2[par], in0=y_acc[:, t, :], in1=m_bt[par], op=ALU.add),
                 reads=[t_y[t], t_mbt[par]], writes=[t_mz2[par]])
            def fin_tail(par=par, tsl=tsl):
                layer_norm(m_z2[par], t_mz2[par], m_o[par], t_mo[par], m_st[par], t_mst[par], g2, b2, t_f)
                P.dma("sp", lambda eng: eng.dma_start(out=out[tsl, :], in_=m_o[par]),
                      reads=[t_mo[par]], sem_tile=t_mo[par])
            fin_pending.append(fin_tail)
            if len(fin_pending) > 2:
                fin_pending.pop(0)()
        while fin_pending:
            fin_pending.pop(0)()
        P.final_wait("sp")
        P.emit_all()
        return nc


def _na_table(rpb, hf):
    base = 32 * hf
    tab = np.full((8, 128, NA_BLOCKS * 128), NEG, np.float32)
    kk = np.arange(128)
    qq = np.arange(128)
    for i in (0, 1, 2, 14, 15):
        kt0, nk, b0 = NA_TYPES[i]
        r = base + 2 * i + qq // 64
        c = qq % 64
        rs = np.clip(r - 4, 0, 56)
        cs = np.clip(c - 8, 0, 48)
        for idx in range(nk):
            kt = kt0 + idx
            kr = base - 4 + 2 * kt + kk // 64
            kc = kk % 64
            vr = (kr[:, None] >= rs[None, :]) & (kr[:, None] <= rs[None, :] + 7) & (kr[:, None] >= 0) & (kr[:, None] <= 63)
            vc = (kc[:, None] >= cs[None, :]) & (kc[:, None] <= cs[None, :] + 15)
            valid = vr & vc
            dr = np.clip(kr[:, None] - r[None, :] + 7, 0, 14)
            dc = np.clip(kc[:, None] - c[None, :] + 15, 0, 30)
            vals = rpb[:, dr, dc]
            blk = np.where(valid[None], vals, np.float32(NEG))
            tab[:, :, (b0 + idx) * 128:(b0 + idx + 1) * 128] = blk
    return tab


def _const_tables(hf):
    half = 32
    inv = (10000.0 ** (-np.arange(half, dtype=np.float32) / half)).astype(np.float32)
    j = np.arange(128)
    t = np.arange(NT)

    def rot(pos0):
        pos = (pos0 + t[None, :] * 128 + j[:, None]).astype(np.float32)
        ang = pos[:, :, None] * inv[None, None, :]
        cos = np.cos(ang).astype(np.float32)
        sin = np.sin(ang).astype(np.float32)
        cc = np.concatenate([cos, cos], -1).reshape(128, NT * 64)
        ss = np.concatenate([-sin, sin], -1).reshape(128, NT * 64)
        return np.ascontiguousarray(cc), np.ascontiguousarray(ss)
    cc_own, ss_own = rot(hf * TOK)
    cc_oth, ss_oth = rot((1 - hf) * TOK)
    m = t[None, :] * 128 + j[:, None]
    dist = (2047 - m if hf == 1 else m).astype(np.float32)
    ii = np.arange(128, dtype=np.float32)
    epos = np.maximum(ii[None, :] - ii[:, None], 0).astype(np.float32)
    eneg = np.maximum(ii[:, None] - ii[None, :], 0).astype(np.float32)
    iota1 = np.broadcast_to(ii[None, :] + 1, (128, 128)).astype(np.float32).copy()
    iota2 = np.broadcast_to(128 - ii[None, :], (128, 128)).astype(np.float32).copy()
    return dict(cc_own=cc_own, ss_own=ss_own, cc_oth=cc_oth, ss_oth=ss_oth, dist=np.ascontiguousarray(dist),
                epos=epos, eneg=eneg, iota1=iota1, iota2=iota2,
                c127=(127 - ii).reshape(128, 1).astype(np.float32), cj=ii.reshape(128, 1).copy(),
                flag=np.full((1, 1), float(hf), np.float32))


def make_in_maps(inputs, cores=range(NCORES)):
    f = lambda a: np.ascontiguousarray(np.asarray(a, dtype=np.float32))
    x = f(inputs["x"]); p = f(inputs["p"])[0]
    shared = dict(
        w_in=f(inputs["w_in"][0]), w_out=f(inputs["w_out"][0]), w_router=f(inputs["w_router"][0]),
        w_egu=f(inputs["w_expert_gu"][0]), w_edn=f(inputs["w_expert_down"][0]),
        w_sgu=f(inputs["w_shared_gu"][0]), w_sdn=f(inputs["w_shared_down"][0]),
        w_pp=f(inputs["w_ple_proj"][0]), w_pg=f(inputs["w_ple_gate"][0]),
        dec_f=f(inputs["ret_decay_fwd"]).reshape(1, 8), dec_b=f(inputs["ret_decay_bwd"]).reshape(1, 8),
        decfT=f(f(inputs["ret_decay_fwd"]).reshape(4, 2).T), decbT=f(f(inputs["ret_decay_bwd"]).reshape(4, 2).T),
        gn_gain=f(inputs["ret_gn_gain"]).reshape(1, 512),
        ln1_g=f(inputs["ln1_gain"]).reshape(1, D), ln1_b=f(inputs["ln1_bias"]).reshape(1, D),
        ln2_g=f(inputs["ln2_gain"]).reshape(1, D), ln2_b=f(inputs["ln2_bias"]).reshape(1, D),
        rbias=f(inputs["router_bias"]).reshape(1, NEXP))
    rpb = f(inputs["na_rpb"][0])
    per_hf = {}
    for hf in (0, 1):
        d = _const_tables(hf)
        d["natab"] = _na_table(rpb, hf)
        per_hf[hf] = d
    maps = []
    for c in cores:
        b, hf = divmod(c, 2)
        own = x[b, hf * TOK:(hf + 1) * TOK]
        oth = x[b, (1 - hf) * TOK:(2 - hf) * TOK]
        xh = np.zeros((512, D), np.float32)
        if hf == 0:
            xh[256:512] = oth[0:256]
        else:
            xh[0:256] = oth[TOK - 256:TOK]
        m = dict(shared)
        m.update(per_hf[hf])
        m.update(xT=f(own.T), xo=f(oth.T), xh=f(xh.T), xr=f(own), pT=f(p[b, hf * TOK:(hf + 1) * TOK].T))
        maps.append(m)
    return maps


_NC_CACHE = {}


def kernel(**inputs):
    if "full" not in _NC_CACHE:
        _NC_CACHE["full"] = build_program("full")
    nc = _NC_CACHE["full"]
    maps = make_in_maps(inputs)
    res = run_bass_kernel_spmd(nc, maps, core_ids=list(range(NCORES)))
    outp = np.empty((4, S, D), np.float32)
    for c in range(NCORES):
        b, hf = divmod(c, 2)
        outp[b, hf * TOK:(hf + 1) * TOK] = res.results[c]["out"]
    return outp
```

```python
import math
from contextlib import ExitStack

import numpy as np
import concourse.bass as bass
import concourse.mybir as mybir
from concourse.bass_utils import run_bass_kernel_spmd

F32 = mybir.dt.float32
BF16 = mybir.dt.bfloat16
U8 = mybir.dt.uint8
AF = mybir.ActivationFunctionType
ALU = mybir.AluOpType
AX = mybir.AxisListType
DT_SIZE = {F32: 4, BF16: 2, U8: 1}

NCORES = 8
D = 1024
S = 4096
TOK = 2048
NT = 16
NEXP = 64
ALPHA = 2.0 ** 0.25
LN_EPS = 1e-5
GN_EPS = 1e-6
NEG = -30000.0
NA_TYPES = {0: (0, 6, 0), 1: (1, 5, 6), 14: (14, 5, 16), 15: (14, 6, 21)}
for _i in range(2, 14):
    NA_TYPES[_i] = (_i, 5, 11)
NA_BLOCKS = 27


class Tile:
    __slots__ = ("name", "lw", "rd", "dsem", "excl")

    def __init__(self, name):
        self.name = name
        self.excl = False
        self.lw = None
        self.rd = []
        self.dsem = None


class Prog:
    ENGS = ("pe", "act", "dve", "pool", "sp")

    def __init__(self, nc, stack):
        self.nc = nc
        self.stack = stack
        self.streams = {e: [] for e in self.ENGS}
        self.sems = {}
        self.cnt = {}
        for e in self.ENGS:
            self.sems[e] = stack.enter_context(nc.semaphore("s_" + e))
            self.cnt[e] = 0
        self.seen = {e: {} for e in self.ENGS}
        self.ndsem = 0
        self.tiles = []

    def tile(self, name="t"):
        t = Tile(name)
        self.tiles.append(t)
        return t

    def tiles_n(self, n, name="t"):
        return [self.tile("%s%d" % (name, i)) for i in range(n)]

    def _dma_sem(self, t):
        if t.dsem is None:
            key = "d%d" % self.ndsem
            self.ndsem += 1
            self.sems[key] = self.stack.enter_context(self.nc.semaphore("s_" + key))
            self.cnt[key] = 0
            t.dsem = key
        return t.dsem

    def _waits(self, eng, reads, writes):
        need = {}

        def add(ev):
            if ev is None:
                return
            k, v = ev
            if need.get(k, 0) < v:
                need[k] = v
        for t in reads:
            add(t.lw)
            if t.excl:
                for ev in t.rd:
                    if ev[0] != eng:
                        add(ev)
        for t in writes:
            add(t.lw)
            for ev in t.rd:
                add(ev)
        out = []
        for k, v in need.items():
            if k == "pe" and eng == "pe":
                continue
            if self.seen[eng].get(k, 0) >= v:
                continue
            self.seen[eng][k] = v
            out.append((k, v))
        return out

    def op(self, eng, fn, reads=(), writes=()):
        waits = self._waits(eng, reads, writes)
        self.cnt[eng] += 1
        ev = (eng, self.cnt[eng])
        sems = self.sems

        def emit(e, waits=waits, fn=fn, semk=eng):
            for k, v in waits:
                e.wait_ge(sems[k], v)
            ins = fn(e)
            ins.then_inc(sems[semk], 1)
        self.streams[eng].append(emit)
        for t in reads:
            t.rd.append(ev)
        for t in writes:
            t.lw = ev
            t.rd = []
        return ev

    def dma(self, q, fn, reads=(), writes=(), sem_tile=None):
        st = sem_tile or (writes[0] if writes else reads[0])
        key = self._dma_sem(st)
        waits = self._waits(q, reads, writes)
        if self.cnt[key] > 0 and self.seen[q].get(key, 0) < self.cnt[key]:
            self.seen[q][key] = self.cnt[key]
            waits.append((key, self.cnt[key]))
        self.cnt[key] += 16
        ev = (key, self.cnt[key])
        sems = self.sems

        def emit(e, waits=waits, fn=fn, key=key):
            for k, v in waits:
                e.wait_ge(sems[k], v)
            ins = fn(e)
            ins.then_inc(sems[key], 16)
        self.streams[q].append(emit)
        for t in reads:
            t.rd.append(ev)
        for t in writes:
            t.lw = ev
            t.rd = []
        return ev

    def barrier(self):
        snap = {k: v for k, v in self.cnt.items() if v > 0}
        sems = self.sems
        for eng in self.ENGS:
            waits = []
            for k, v in snap.items():
                if k == eng:
                    continue
                if self.seen[eng].get(k, 0) >= v:
                    continue
                self.seen[eng][k] = v
                waits.append((k, v))

            def emit(e, waits=waits):
                for k, v in waits:
                    e.wait_ge(sems[k], v)
            self.streams[eng].append(emit)
        for t in self.tiles:
            t.rd = []

    def final_wait(self, eng="sp"):
        snap = {k: v for k, v in self.cnt.items() if v > 0}
        sems = self.sems

        def emit(e):
            for k, v in snap.items():
                if k == eng:
                    continue
                e.wait_ge(sems[k], v)
        self.streams[eng].append(emit)

    def emit_all(self):
        nc = self.nc
        streams = self.streams
        with nc.Block() as block:
            @block.tensor
            def _(e):
                for f in streams["pe"]:
                    f(e)

            @block.scalar
            def _(e):
                for f in streams["act"]:
                    f(e)

            @block.vector
            def _(e):
                for f in streams["dve"]:
                    f(e)

            @block.gpsimd
            def _(e):
                for f in streams["pool"]:
                    f(e)

            @block.sync
            def _(e):
                for f in streams["sp"]:
                    f(e)


class Arena:
    def __init__(self, nc, stack, nbytes):
        self.t = stack.enter_context(nc.sbuf_tensor("arena", [128, nbytes], U8))
        self.n = nbytes
        self.off = 0
        self.marks = []
        self.peak = 0

    def mark(self):
        self.marks.append(self.off)

    def release(self):
        self.off = self.marks.pop()

    def alloc(self, free_shape, dtype):
        n = int(np.prod(free_shape)) * DT_SIZE[dtype]
        n_al = (n + 63) // 64 * 64
        assert self.off + n_al <= self.n, ("SBUF arena overflow", self.off, n_al, self.n)
        ap = self.t[:, self.off:self.off + n].bitcast(dtype)
        self.off += n_al
        self.peak = max(self.peak, self.off)
        if len(free_shape) == 2:
            ap = ap.rearrange("p (a b) -> p a b", b=free_shape[1])
        elif len(free_shape) == 3:
            ap = ap.rearrange("p (a b c) -> p a b c", b=free_shape[1], c=free_shape[2])
        return ap


def bc_mid(ap2, n):
    p, f = ap2.shape
    return ap2.unsqueeze(1).to_broadcast([p, n, f])


def bc_last(ap2, n):
    p, a = ap2.shape
    return ap2.unsqueeze(2).to_broadcast([p, a, n])


def build_program(stage="full"):
    nc = bass.Bass("TRN2", target_bir_lowering=False)

    def din(name, shape):
        return nc.dram_tensor(name, list(shape), F32, kind="ExternalInput").ap()

    xT = din("xT", [D, TOK]); xo = din("xo", [D, TOK]); xh = din("xh", [D, 512])
    xr = din("xr", [TOK, D]); pT = din("pT", [256, TOK])
    w_in = din("w_in", [D, 3584]); w_out = din("w_out", [D, D]); w_router = din("w_router", [D, NEXP])
    w_egu = din("w_egu", [NEXP, D, 512]); w_edn = din("w_edn", [NEXP, 256, D])
    w_sgu = din("w_sgu", [D, 512]); w_sdn = din("w_sdn", [256, D])
    w_pp = din("w_pp", [256, D]); w_pg = din("w_pg", [D, D])
    cc_own = din("cc_own", [128, NT * 64]); ss_own = din("ss_own", [128, NT * 64])
    cc_oth = din("cc_oth", [128, NT * 64]); ss_oth = din("ss_oth", [128, NT * 64])
    dist = din("dist", [128, NT])
    dec_f = din("dec_f", [1, 8]); dec_b = din("dec_b", [1, 8])
    decfT = din("decfT", [2, 4]); decbT = din("decbT", [2, 4]); flag = din("flag", [1, 1])
    epos = din("epos", [128, 128]); eneg = din("eneg", [128, 128])
    iota1 = din("iota1", [128, 128]); iota2 = din("iota2", [128, 128])
    c127 = din("c127", [128, 1]); cj = din("cj", [128, 1])
    gn_gain = din("gn_gain", [1, 512]); natab = din("natab", [8, 128, NA_BLOCKS * 128])
    ln1_g = din("ln1_g", [1, D]); ln1_b = din("ln1_b", [1, D])
    ln2_g = din("ln2_g", [1, D]); ln2_b = din("ln2_b", [1, D]); rbias = din("rbias", [1, NEXP])
    out = nc.dram_tensor("out", [TOK, D], F32, kind="ExternalOutput").ap()
    GT = nc.dram_tensor("GT", [NEXP, TOK], F32, kind="Internal").ap()
    base = nc.dram_tensor("base", [TOK, D], F32, kind="Internal").ap()
    cat = nc.dram_tensor("cat", [TOK, D], BF16, kind="Internal").ap()
    dbg = {}
    if stage != "full":
        dbg["ret"] = nc.dram_tensor("dbg_ret", [TOK, 512], F32, kind="ExternalOutput").ap()
        dbg["na"] = nc.dram_tensor("dbg_na", [TOK, 512], F32, kind="ExternalOutput").ap()
        dbg["x1"] = nc.dram_tensor("dbg_x1", [TOK, D], F32, kind="ExternalOutput").ap()
        dbg["gate"] = nc.dram_tensor("dbg_gate", [TOK, NEXP], F32, kind="ExternalOutput").ap()
        dbg["misc"] = nc.dram_tensor("dbg_misc", [128, 64], F32, kind="ExternalOutput").ap()

    with ExitStack() as st:
        P = Prog(nc, st)
        A = Arena(nc, st, 206 * 1024)
        PS = [st.enter_context(nc.psum_tensor("ps%d" % i, [128, 512], F32))[:, :] for i in range(8)]
        PSB = [p.bitcast(BF16) for p in PS]
        t_ps = P.tiles_n(8, "ps")
        for t_ in t_ps:
            t_.excl = True

        ident_f = A.alloc([128], F32); ident_b = A.alloc([128], BF16)
        t_ident = P.tile("ident")

        P.op("pool", lambda e: e.memset(ident_f, 0.0), writes=[t_ident])
        P.op("pool", lambda e: e.affine_select(out=ident_f, in_=ident_f, pattern=[[-1, 128]], compare_op=ALU.not_equal,
                                               fill=1.0, base=0, channel_multiplier=1), reads=[t_ident], writes=[t_ident])
        P.op("dve", lambda e: e.tensor_copy(out=ident_b, in_=ident_f), reads=[t_ident], writes=[t_ident])

        cst = A.alloc([8], F32)
        t_cst = P.tile("cst")

        for (p0, p1, c0, c1, val) in ((0, 128, 0, 1, math.log(0.125)), (0, 128, 1, 2, 1.0), (0, 128, 2, 3, 0.0),
                                      (0, 128, 3, 4, GN_EPS), (0, 128, 4, 5, LN_EPS), (0, 64, 5, 6, 1.0),
                                      (64, 128, 5, 6, 0.0), (0, 64, 6, 7, 0.0), (64, 128, 6, 7, 1.0)):
            P.op("pool", lambda e, p0=p0, p1=p1, c0=c0, c1=c1, val=val: e.memset(cst[p0:p1, c0:c1], val), writes=[t_cst])

        t_cat = [P.tiles_n(NT, "catR"), P.tiles_n(NT, "catN")]

        A.mark()
        w_ret = A.alloc([8, 2048], BF16)
        t_wret = P.tiles_n(4, "wret")
        for g in range(4):
            P.dma("pool", lambda e, g=g: e.dma_start(
                out=w_ret[:, :, g * 512:(g + 1) * 512],
                in_=w_in[:, g * 512:(g + 1) * 512].rearrange("(kc k) n -> k kc n", k=128)), writes=[t_wret[g]])
        ccX = A.alloc([NT, 64], F32); ssX = A.alloc([NT, 64], F32)
        ccO, ssO = ccX, ssX
        t_rotl = P.tiles_n(2, "rot")

        def load_rot(c_src, s_src):
            for i_, (dst, src) in enumerate(((ccX, c_src), (ssX, s_src))):
                P.dma("sp", lambda e, dst=dst, src=src: e.dma_start(out=dst.rearrange("p a b -> p (a b)"), in_=src),
                      writes=[t_rotl[i_]])
        load_rot(cc_oth, ss_oth)
        small = A.alloc([256], F32)
        t_small = P.tile("small")
        sm = lambda a, b: small[:, a:b]
        lds = [(sm(0, 8), dec_f.partition_broadcast(128)), (sm(8, 16), dec_b.partition_broadcast(128)),
               (small[0:64, 16:20], decfT[0:1, :].partition_broadcast(64)),
               (small[64:128, 16:20], decfT[1:2, :].partition_broadcast(64)),
               (small[0:64, 20:24], decbT[0:1, :].partition_broadcast(64)),
               (small[64:128, 20:24], decbT[1:2, :].partition_broadcast(64)),
               (sm(24, 25), flag.partition_broadcast(128)), (sm(96, 97), c127), (sm(97, 98), cj),
               (sm(100, 116), dist)]
        t_smld = P.tiles_n(len(lds), "smld")
        for i_, (dst, src) in enumerate(lds):
            P.dma("sp", lambda e, dst=dst, src=src: e.dma_start(out=dst, in_=src), writes=[t_smld[i_]])
        eposT = A.alloc([128], F32); enegT = A.alloc([128], F32)
        io1 = A.alloc([128], F32); io2 = A.alloc([128], F32)
        gnb = A.alloc([512], F32)
        t_tabl = P.tiles_n(5, "tabl")
        t_gc = P.tile("gc")
        for i_, (dst, src) in enumerate(((eposT, epos), (enegT, eneg), (io1, iota1), (io2, iota2),
                                         (gnb, gn_gain.partition_broadcast(128)))):
            P.dma("sp", lambda e, dst=dst, src=src: e.dma_start(out=dst, in_=src), writes=[t_tabl[i_]])

        P.op("act", lambda e: e.activation(out=sm(32, 56), in_=sm(0, 24), func=AF.Exp, scale=-1.0),
             reads=t_smld, writes=[t_small])
        P.op("act", lambda e: e.activation(out=sm(32, 56), in_=sm(32, 56), func=AF.Ln, bias=cst[:, 1:2], scale=1.0),
             reads=[t_small, t_cst], writes=[t_small])
        P.op("dve", lambda e: e.tensor_scalar(out=sm(32, 56), in0=sm(32, 56), scalar1=-1.0, scalar2=None, op0=ALU.mult),
             reads=[t_small], writes=[t_small])
        P.op("dve", lambda e: e.tensor_scalar(out=sm(25, 26), in0=sm(24, 25), scalar1=-1.0, scalar2=1.0,
                                              op0=ALU.mult, op1=ALU.add), reads=[t_small], writes=[t_small])
        P.op("dve", lambda e: e.tensor_scalar(out=sm(56, 64), in0=sm(32, 40), scalar1=sm(24, 25), scalar2=None,
                                              op0=ALU.mult), reads=[t_small], writes=[t_small])
        P.op("dve", lambda e: e.scalar_tensor_tensor(out=sm(56, 64), in0=sm(40, 48), scalar=sm(25, 26), in1=sm(56, 64),
                                                     op0=ALU.mult, op1=ALU.add), reads=[t_small], writes=[t_small])
        P.op("act", lambda e: e.activation(out=sm(64, 72), in_=sm(32, 40), func=AF.Exp, scale=sm(96, 97)),
             reads=[t_small], writes=[t_small])
        P.op("act", lambda e: e.activation(out=sm(72, 80), in_=sm(40, 48), func=AF.Exp, scale=sm(97, 98)),
             reads=[t_small], writes=[t_small])
        P.op("act", lambda e: e.activation(out=sm(80, 88), in_=sm(48, 56), func=AF.Exp, scale=128.0),
             reads=[t_small], writes=[t_small])
        gcfB = A.alloc([4, 64], F32); gcbB = A.alloc([4, 64], F32)
        P.op("dve", lambda e: e.tensor_copy(out=gcfB, in_=bc_last(sm(80, 84), 64)), reads=[t_small], writes=[t_gc])
        P.op("dve", lambda e: e.tensor_copy(out=gcbB, in_=bc_last(sm(84, 88), 64)), reads=[t_small], writes=[t_gc])
        Mmask = A.alloc([8, 128], BF16)
        mtmp = A.alloc([128], F32)
        t_M = P.tile("M"); t_mtmp = P.tile("mtmp")
        for h in range(8):
            P.op("dve", lambda e, h=h: e.tensor_scalar(out=mtmp, in0=eposT, scalar1=sm(32 + h, 33 + h), scalar2=None,
                                                       op0=ALU.mult), reads=[t_small] + t_tabl, writes=[t_mtmp])
            P.op("dve", lambda e, h=h: e.scalar_tensor_tensor(out=mtmp, in0=enegT, scalar=sm(40 + h, 41 + h), in1=mtmp,
                                                              op0=ALU.mult, op1=ALU.add),
                 reads=[t_small, t_mtmp] + t_tabl, writes=[t_mtmp])
            P.op("act", lambda e, h=h: e.activation(out=Mmask[:, h, :], in_=mtmp, func=AF.Exp, bias=cst[:, 0:1], scale=1.0),
                 reads=[t_mtmp, t_cst], writes=[t_M])
        Df = A.alloc([4, 128], BF16); Db = A.alloc([4, 128], BF16)
        t_D = P.tile("D")
        for pr in range(4):
            P.op("act", lambda e, pr=pr: e.activation(out=Df[:, pr, :], in_=io1, func=AF.Exp, bias=cst[:, 0:1],
                                                      scale=sm(48 + pr, 49 + pr)), reads=[t_small, t_cst] + t_tabl, writes=[t_D])
            P.op("act", lambda e, pr=pr: e.activation(out=Db[:, pr, :], in_=io2, func=AF.Exp, bias=cst[:, 0:1],
                                                      scale=sm(52 + pr, 53 + pr)), reads=[t_small, t_cst] + t_tabl, writes=[t_D])
        if "misc" in dbg:
            P.dma("sp", lambda e: e.dma_start(out=dbg["misc"][:, 0:56], in_=sm(32, 88)), reads=[t_small], sem_tile=P.tile("dm"))

        if stage == "ret0":
            P.final_wait("sp")
            P.emit_all()
            return nc
        qT = A.alloc([4, TOK], BF16); kT = A.alloc([4, TOK], BF16)
        vS = A.alloc([NT, 512], BF16); GS = A.alloc([NT, 512], BF16)
        SfB = A.alloc([NT, 256], BF16); SbB = A.alloc([NT, 256], BF16)
        UbS = A.alloc([NT, 256], BF16)
        Sst = A.alloc([2, 256], F32)
        t_qT = P.tiles_n(NT, "qT"); t_kT = P.tiles_n(NT, "kT"); t_v = P.tiles_n(NT, "v"); t_G = P.tiles_n(NT, "G")
        t_SfB = P.tiles_n(NT, "SfB"); t_SbB = P.tiles_n(NT, "SbB"); t_Ub = P.tiles_n(NT, "Ub")
        t_Sf = P.tile("Sf"); t_Sb = P.tile("Sb")
        A.mark()
        xs = [A.alloc([8, 256], BF16) for _ in range(2)]; t_xs = P.tiles_n(2, "xs")
        q_tm = [A.alloc([512], BF16) for _ in range(2)]; k_tm = [A.alloc([512], BF16) for _ in range(2)]
        t_qtm = P.tiles_n(2, "qtm"); t_ktm = P.tiles_n(2, "ktm")
        rA = [A.alloc([512], F32) for _ in range(2)]; rB = [A.alloc([512], F32) for _ in range(2)]
        t_rA = P.tiles_n(2, "rA"); t_rB = P.tiles_n(2, "rB")
        vfb = [A.alloc([4, 256], BF16) for _ in range(2)]; t_vfb = P.tiles_n(2, "vfb")
        wo = [A.alloc([8], F32) for _ in range(2)]; t_wo = P.tiles_n(2, "wo")
        Ud = [A.alloc([4, 2, 64], F32) for _ in range(2)]; t_Ud = P.tiles_n(2, "Ud")
        Uf = [A.alloc([2, 512], F32) for _ in range(2)]; t_Uf = P.tiles_n(2, "Uf")

        def v3(ap):
            return ap.rearrange("p (h d) -> p h d", d=64)

        def inproj(ps_i, xs_ap, wcols, reads):
            def f(e):
                ins = None
                for kc in range(8):
                    ins = e.matmul(PS[ps_i], lhsT=xs_ap(kc), rhs=w_ret[:, kc, wcols * 512:(wcols + 1) * 512],
                                   start=(kc == 0), stop=(kc == 7))
                return ins
            P.op("pe", f, reads=reads, writes=[t_ps[ps_i]])

        def rotary(ps_i, cc, ss, t, dst, t_dst, par):
            src = v3(PS[ps_i])
            a3 = v3(rA[par]); b3 = v3(rB[par])
            P.op("dve", lambda e: e.tensor_tensor(out=a3, in0=src, in1=bc_mid(cc[:, t, :], 8), op=ALU.mult),
                 reads=[t_ps[ps_i]] + t_rotl, writes=[t_rA[par]])
            P.op("dve", lambda e: e.tensor_tensor(out=b3[:, :, 0:32], in0=src[:, :, 32:64],
                                                  in1=bc_mid(ss[:, t, 0:32], 8), op=ALU.mult),
                 reads=[t_ps[ps_i]] + t_rotl, writes=[t_rB[par]])
            P.op("dve", lambda e: e.tensor_tensor(out=b3[:, :, 32:64], in0=src[:, :, 0:32],
                                                  in1=bc_mid(ss[:, t, 32:64], 8), op=ALU.mult),
                 reads=[t_ps[ps_i]] + t_rotl, writes=[t_rB[par]])
            P.op("pool", lambda e: e.tensor_tensor(out=dst, in0=rA[par], in1=rB[par], op=ALU.add),
                 reads=[t_rA[par], t_rB[par]], writes=[t_dst])

        sinB = [PS[4 + pr][:, 0:128] for pr in range(4)]
        oth_pending = []
        for t in range(NT):
            ch, sub = divmod(t, 2)
            par = t % 2
            if sub == 0:
                P.dma("pool", lambda e, ch=ch: e.dma_start(
                    out=xs[ch % 2], in_=xo[:, ch * 256:(ch + 1) * 256].rearrange("(kc k) n -> k kc n", k=128)),
                    writes=[t_xs[ch % 2]])
            xs_ap = lambda kc, ch=ch, sub=sub: xs[ch % 2][:, kc, sub * 128:(sub + 1) * 128]
            inproj(0, xs_ap, 1, [t_xs[ch % 2], t_wret[1]])
            inproj(1, xs_ap, 2, [t_xs[ch % 2], t_wret[2]])
            while oth_pending:
                oth_pending.pop(0)()
            rotary(0, ccX, ssX, t, k_tm[par], t_ktm[par], par)
            P.op("act", lambda e, t=t, par=par: e.activation(out=wo[par], in_=sm(56, 64), func=AF.Exp,
                                                             scale=sm(100 + t, 101 + t)),
                 reads=[t_small], writes=[t_wo[par]])
            P.op("dve", lambda e, par=par: e.tensor_tensor(
                out=vfb[par].rearrange("p a b -> p (a b)")[:, 0:512].rearrange("p (h d) -> p h d", d=64),
                in0=v3(PS[1]), in1=bc_last(wo[par], 64), op=ALU.mult),
                reads=[t_ps[1], t_wo[par]], writes=[t_vfb[par]])

            def fU(e, t=t, par=par):
                ins = None
                vw = vfb[par].rearrange("p a b -> p (a b)")
                for pr in range(4):
                    ins = e.matmul(sinB[pr], lhsT=k_tm[par][:, pr * 128:(pr + 1) * 128],
                                   rhs=vw[:, pr * 128:(pr + 1) * 128], start=(t == 0), stop=(t == NT - 1))
                return ins
            oth_pending.append(lambda fU=fU, par=par: P.op("pe", fU, reads=[t_ktm[par], t_vfb[par]], writes=t_ps[4:8]))
        while oth_pending:
            oth_pending.pop(0)()
        S3 = Sst.rearrange("p a (b c) -> p a b c", c=64)
        for hp in range(2):
            pl, ph = hp * 64, hp * 64 + 64
            for pr in range(4):
                P.op("dve", lambda e, pl=pl, ph=ph, pr=pr: e.tensor_scalar(
                    out=S3[pl:ph, 0, pr, :], in0=sinB[pr][pl:ph, pl:ph], scalar1=small[pl:ph, 24:25], scalar2=None, op0=ALU.mult),
                    reads=[t_ps[4 + pr], t_small], writes=[t_Sf])
                P.op("dve", lambda e, pl=pl, ph=ph, pr=pr: e.tensor_scalar(
                    out=S3[pl:ph, 1, pr, :], in0=sinB[pr][pl:ph, pl:ph], scalar1=small[pl:ph, 25:26], scalar2=None, op0=ALU.mult),
                    reads=[t_ps[4 + pr], t_small], writes=[t_Sb])

        if stage == "ret1":
            P.final_wait("sp")
            P.emit_all()
            return nc
        load_rot(cc_own, ss_own)
        own_pending = []
        for t in range(NT):
            ch, sub = divmod(t, 2)
            par = t % 2
            if sub == 0:
                P.dma("pool", lambda e, ch=ch: e.dma_start(
                    out=xs[ch % 2], in_=xT[:, ch * 256:(ch + 1) * 256].rearrange("(kc k) n -> k kc n", k=128)),
                    writes=[t_xs[ch % 2]])
            xs_ap = lambda kc, ch=ch, sub=sub: xs[ch % 2][:, kc, sub * 128:(sub + 1) * 128]
            for g in range(4):
                inproj(g, xs_ap, g, [t_xs[ch % 2], t_wret[g]])
            rotary(0, ccO, ssO, t, q_tm[par], t_qtm[par], par)
            rotary(1, ccO, ssO, t, k_tm[par], t_ktm[par], par)
            P.op("act", lambda e, t=t: e.activation(func=AF.Copy, out=vS[:, t, :], in_=PS[2]), reads=[t_ps[2]], writes=[t_v[t]])
            vfl = vfb[par].rearrange("p a b -> p (a b)")
            for dr, col in ((0, 64), (1, 72)):
                P.op("dve", lambda e, dr=dr, col=col, vfl=vfl: e.tensor_tensor(
                    out=v3(vfl[:, dr * 512:(dr + 1) * 512]), in0=v3(PS[2]), in1=bc_last(small[:, col:col + 8], 64),
                    op=ALU.mult), reads=[t_ps[2], t_small], writes=[t_vfb[par]])
            P.op("act", lambda e, t=t: e.activation(out=GS[:, t, :], in_=PS[3], func=AF.Silu),
                 reads=[t_ps[3]], writes=[t_G[t]])
            P.op("pool", lambda e, t=t: e.tensor_tensor(out=GS[:, t, :], in0=GS[:, t, :], in1=gnb, op=ALU.mult),
                 reads=[t_G[t]] + t_tabl, writes=[t_G[t]])

            while own_pending:
                own_pending.pop(0)()

            def own_tail(t=t, par=par):
                def fT(e, par=par):
                    ins = None
                    for pr in range(4):
                        ins = e.transpose(out=PSB[4][:, pr * 128:(pr + 1) * 128], in_=q_tm[par][:, pr * 128:(pr + 1) * 128],
                                          identity=ident_b)
                    for pr in range(4):
                        ins = e.transpose(out=PSB[7][:, pr * 128:(pr + 1) * 128],
                                          in_=k_tm[par][:, pr * 128:(pr + 1) * 128], identity=ident_b)
                    return ins
                P.op("pe", fT, reads=[t_qtm[par], t_ktm[par], t_ident], writes=[t_ps[4], t_ps[7]])
                P.op("dve", lambda e, t=t: e.tensor_copy(out=qT[:, :, t * 128:(t + 1) * 128],
                                                         in_=PSB[4][:, 0:512].rearrange("p (a b) -> p a b", b=128)),
                     reads=[t_ps[4]], writes=[t_qT[t]])
                P.op("dve", lambda e, t=t: e.tensor_copy(out=kT[:, :, t * 128:(t + 1) * 128],
                                                         in_=PSB[7][:, 0:512].rearrange("p (a b) -> p a b", b=128)),
                     reads=[t_ps[7]], writes=[t_kT[t]])

                def fU2(e, par=par):
                    ins = None
                    vfl = vfb[par].rearrange("p a b -> p (a b)")
                    for pr in range(4):
                        bank = PS[5 + pr // 2]
                        for dr in range(2):
                            c0 = (pr % 2) * 256 + dr * 128
                            ins = e.matmul(bank[:, c0:c0 + 128], lhsT=k_tm[par][:, pr * 128:(pr + 1) * 128],
                                           rhs=vfl[:, dr * 512 + pr * 128:dr * 512 + (pr + 1) * 128], start=True, stop=True)
                    return ins
                P.op("pe", fU2, reads=[t_ktm[par], t_vfb[par]], writes=[t_ps[5], t_ps[6]])
                P.op("act", lambda e, t=t: e.activation(func=AF.Copy, out=SfB[:, t, :], in_=Sst[:, 0, :]), reads=[t_Sf], writes=[t_SfB[t]])
                P.op("dve", lambda e: e.tensor_tensor(out=Sst[:, 0, :], in0=Sst[:, 0, :],
                                                      in1=gcfB.rearrange("p a b -> p (a b)"), op=ALU.mult),
                     reads=[t_Sf, t_gc], writes=[t_Sf])
                for bk in range(2):
                    P.op("dve", lambda e, bk=bk, par=par: e.tensor_copy(out=Uf[par][:, bk, :], in_=PS[5 + bk]),
                         reads=[t_ps[5 + bk]], writes=[t_Uf[par]])
                    Ub4 = Uf[par][:, bk, :].rearrange("p (a s c) -> p a s c", s=2, c=128)
                    ud = Ud[par][:, 2 * bk:2 * bk + 2, :, :]
                    for a_ in range(2):
                        P.op("dve", lambda e, Ub4=Ub4, ud=ud, a_=a_: e.tensor_scalar(
                            out=ud[:, a_], in0=Ub4[:, a_, :, 64:128], scalar1=cst[:, 6:7], scalar2=None, op0=ALU.mult),
                            reads=[t_Uf[par], t_cst], writes=[t_Ud[par]])
                        P.op("dve", lambda e, Ub4=Ub4, ud=ud, a_=a_: e.scalar_tensor_tensor(
                            out=ud[:, a_], in0=Ub4[:, a_, :, 0:64], scalar=cst[:, 5:6], in1=ud[:, a_], op0=ALU.mult, op1=ALU.add),
                            reads=[t_Uf[par], t_cst, t_Ud[par]], writes=[t_Ud[par]])
                P.op("dve", lambda e, par=par: e.tensor_tensor(out=S3[:, 0], in0=S3[:, 0], in1=Ud[par][:, :, 0, :], op=ALU.add),
                     reads=[t_Sf, t_Ud[par]], writes=[t_Sf])
                P.op("pool", lambda e, par=par, t=t: e.tensor_copy(out=UbS[:, t, :].rearrange("p (a c) -> p a c", c=64),
                                                                   in_=Ud[par][:, :, 1, :]),
                     reads=[t_Ud[par]], writes=[t_Ub[t]])

            own_pending.append(own_tail)
        while own_pending:
            own_pending.pop(0)()
        for t in range(NT - 1, -1, -1):
            P.op("act", lambda e, t=t: e.activation(func=AF.Copy, out=SbB[:, t, :], in_=Sst[:, 1, :]), reads=[t_Sb], writes=[t_SbB[t]])
            if t > 0:
                P.op("dve", lambda e: e.tensor_tensor(out=Sst[:, 1, :], in0=Sst[:, 1, :],
                                                      in1=gcbB.rearrange("p a b -> p (a b)"), op=ALU.mult),
                     reads=[t_Sb, t_gc], writes=[t_Sb])
                P.op("dve", lambda e, t=t: e.tensor_tensor(out=Sst[:, 1, :], in0=Sst[:, 1, :], in1=UbS[:, t, :], op=ALU.add),
                     reads=[t_Sb, t_Ub[t]], writes=[t_Sb])

        P.barrier()
        A.release()
        if stage == "ret2":
            P.final_wait("sp")
            P.emit_all()
            return nc
        sT_sb = [A.alloc([8, 128], BF16) for _ in range(2)]; t_sT = P.tiles_n(2, "sT")
        qfT = [A.alloc([4, 128], BF16) for _ in range(2)]; qbT = [A.alloc([4, 128], BF16) for _ in range(2)]
        t_qf = P.tiles_n(2, "qf"); t_qb = P.tiles_n(2, "qb")
        sq = [A.alloc([512], F32) for _ in range(2)]; t_sq = P.tiles_n(2, "sq")
        gst = [A.alloc([64], F32) for _ in range(2)]; t_gst = P.tiles_n(2, "gst")
        yc = [A.alloc([512], F32) for _ in range(2)]; t_yc = P.tiles_n(2, "yc")
        retb = [A.alloc([512], BF16) for _ in range(2)]; t_retb = P.tiles_n(2, "retb")
        retf = [A.alloc([512], F32) for _ in range(2)] if "ret" in dbg else None
        ret_pending = []
        for t in range(NT):
            par = t % 2
            b0 = 4 * par
            tsl = slice(t * 128, (t + 1) * 128)

            def fS(e, b0=b0, tsl=tsl):
                ins = None
                for h in range(8):
                    pr, hp = divmod(h, 2)
                    pl, ph = hp * 64, hp * 64 + 64
                    ins = e.matmul(PS[b0 + hp][:, pr * 128:pr * 128 + 128], lhsT=kT[pl:ph, pr, tsl],
                                   rhs=qT[pl:ph, pr, tsl], start=True, stop=True)
                return ins
            P.op("pe", fS, reads=[t_kT[t], t_qT[t]], writes=[t_ps[b0], t_ps[b0 + 1]])
            for bk in range(2):
                P.op("dve", lambda e, bk=bk, b0=b0, par=par: e.tensor_tensor(
                    out=sT_sb[par].rearrange("p (a s) b -> p a s b", s=2)[:, :, bk, :],
                    in0=PS[b0 + bk].rearrange("p (a b) -> p a b", b=128),
                    in1=Mmask.rearrange("p (a s) b -> p a s b", s=2)[:, :, bk, :], op=ALU.mult),
                    reads=[t_ps[b0 + bk], t_M], writes=[t_sT[par]])
            P.op("pool", lambda e, par=par, tsl=tsl: e.tensor_tensor(out=qfT[par], in0=qT[:, :, tsl], in1=Df, op=ALU.mult),
                 reads=[t_qT[t], t_D], writes=[t_qf[par]])
            P.op("pool", lambda e, par=par, tsl=tsl: e.tensor_tensor(out=qbT[par], in0=qT[:, :, tsl], in1=Db, op=ALU.mult),
                 reads=[t_qT[t], t_D], writes=[t_qb[par]])

            def ret_tail(b0=b0, par=par, t=t, tsl=tsl):
                def fY(e, b0=b0, par=par, t=t):
                    ins = None
                    for h in range(8):
                        pr, hp = divmod(h, 2)
                        pl, ph = hp * 64, hp * 64 + 64
                        o = PS[b0 + 2][:, h * 64:(h + 1) * 64]
                        e.matmul(o, lhsT=sT_sb[par][:, h, :], rhs=vS[:, t, h * 64:(h + 1) * 64], start=True, stop=False)
                        e.matmul(o, lhsT=qfT[par][pl:ph, pr, :], rhs=SfB[pl:ph, t, pr * 64:(pr + 1) * 64], start=False, stop=False)
                        ins = e.matmul(o, lhsT=qbT[par][pl:ph, pr, :], rhs=SbB[pl:ph, t, pr * 64:(pr + 1) * 64], start=False, stop=True)
                    return ins
                P.op("pe", fY, reads=[t_sT[par], t_v[t], t_qf[par], t_qb[par], t_SfB[t], t_SbB[t]], writes=[t_ps[b0 + 2]])
                y3 = v3(PS[b0 + 2])
                g = gst[par]
                P.op("dve", lambda e, y3=y3, g=g: e.tensor_reduce(out=g[:, 0:8], in_=y3, axis=AX.X, op=ALU.add),
                     reads=[t_ps[b0 + 2]], writes=[t_gst[par]])
                P.op("act", lambda e, b0=b0, par=par: e.activation(out=sq[par], in_=PS[b0 + 2], func=AF.Square),
                     reads=[t_ps[b0 + 2]], writes=[t_sq[par]])
                P.op("dve", lambda e, g=g, par=par: e.tensor_reduce(out=g[:, 8:16], in_=v3(sq[par]), axis=AX.X, op=ALU.add),
                     reads=[t_sq[par]], writes=[t_gst[par]])
                P.op("dve", lambda e, g=g: e.tensor_scalar(out=g[:, 16:24], in0=g[:, 0:8], scalar1=1.0 / 64, scalar2=None,
                                                           op0=ALU.mult), reads=[t_gst[par]], writes=[t_gst[par]])
                P.op("dve", lambda e, g=g: e.tensor_tensor(out=g[:, 24:32], in0=g[:, 16:24], in1=g[:, 16:24], op=ALU.mult),
                     reads=[t_gst[par]], writes=[t_gst[par]])
                P.op("dve", lambda e, g=g: e.scalar_tensor_tensor(out=g[:, 32:40], in0=g[:, 8:16], scalar=1.0 / 64,
                                                                  in1=g[:, 24:32], op0=ALU.mult, op1=ALU.subtract),
                     reads=[t_gst[par]], writes=[t_gst[par]])
                P.op("act", lambda e, g=g: e.activation(out=g[:, 40:48], in_=g[:, 32:40], func=AF.Ln, bias=cst[:, 3:4], scale=1.0),
                     reads=[t_gst[par], t_cst], writes=[t_gst[par]])
                P.op("act", lambda e, g=g: e.activation(out=g[:, 40:48], in_=g[:, 40:48], func=AF.Exp, scale=-0.5),
                     reads=[t_gst[par]], writes=[t_gst[par]])
                P.op("dve", lambda e, g=g, y3=y3, par=par: e.tensor_tensor(out=v3(yc[par]), in0=y3, in1=bc_last(g[:, 16:24], 64),
                                                                           op=ALU.subtract),
                     reads=[t_ps[b0 + 2], t_gst[par]], writes=[t_yc[par]])
                P.op("pool", lambda e, g=g, par=par: e.tensor_tensor(out=v3(yc[par]), in0=v3(yc[par]), in1=bc_last(g[:, 40:48], 64),
                                                                     op=ALU.mult), reads=[t_yc[par], t_gst[par]], writes=[t_yc[par]])
                P.op("pool", lambda e, par=par, t=t: e.tensor_tensor(out=retb[par], in0=yc[par], in1=GS[:, t, :], op=ALU.mult),
                     reads=[t_yc[par], t_G[t]], writes=[t_retb[par]])
                if "ret" in dbg:
                    P.op("dve", lambda e, par=par: e.tensor_copy(out=retf[par], in_=retb[par]), reads=[t_retb[par]], writes=[t_yc[par]])
                    P.dma("sp", lambda e, par=par, tsl=tsl: e.dma_start(out=dbg["ret"][tsl, :], in_=retf[par]),
                          reads=[t_yc[par]], sem_tile=t_yc[par])

                P.dma("sp", lambda e, par=par, tsl=tsl: e.dma_start(out=cat[tsl, 0:512], in_=retb[par]),
                      reads=[t_retb[par]], writes=[t_cat[0][t]], sem_tile=t_retb[par])

            ret_pending.append(ret_tail)
            if len(ret_pending) > 1:
                ret_pending.pop(0)()
        while ret_pending:
            ret_pending.pop(0)()
        P.barrier()
        A.release()
        if stage == "ret":
            P.final_wait("sp")
            P.emit_all()
            return nc

        A.mark()
        NKB = 20
        w_na = A.alloc([8, 1536], BF16); t_wna = P.tiles_n(3, "wna")
        for g in range(3):
            P.dma("pool", lambda e, g=g: e.dma_start(
                out=w_na[:, :, g * 512:(g + 1) * 512],
                in_=w_in[:, 2048 + g * 512:2048 + (g + 1) * 512].rearrange("(kc k) n -> k kc n", k=128)), writes=[t_wna[g]])
        nqT = A.alloc([4, TOK], BF16); nkT = A.alloc([4, NKB * 128], BF16)
        Vaug = A.alloc([NKB, 8 * 65], BF16)
        na_sb = A.alloc([NT, 512], BF16)
        t_nq = P.tiles_n(NT, "nq"); t_nk = P.tiles_n(NKB, "nk"); t_V = P.tiles_n(NKB, "V"); t_na = P.tiles_n(NT, "na")
        V4 = Vaug.rearrange("p a (h c) -> p a h c", c=65)
        for kb in range(NKB):
            P.op("pool", lambda e, kb=kb: e.memset(V4[:, kb, :, 64:65], 1.0), writes=[t_V[kb]])
        nxs = [A.alloc([8, 256], BF16) for _ in range(2)]; t_nxs = P.tiles_n(2, "xsn")
        nq_tm = [A.alloc([512], BF16) for _ in range(2)]; nk_tm = [A.alloc([512], BF16) for _ in range(2)]
        t_nqtm = P.tiles_n(2, "nqtm"); t_nktm = P.tiles_n(2, "nktm")

        def na_inproj(ps_i, xs_ap, g, reads):
            def f(e):
                ins = None
                for kc in range(8):
                    ins = e.matmul(PS[ps_i], lhsT=xs_ap(kc), rhs=w_na[:, kc, g * 512:(g + 1) * 512],
                                   start=(kc == 0), stop=(kc == 7))
                return ins
            P.op("pe", f, reads=reads, writes=[t_ps[ps_i]])

        chunks = [xh[:, 0:256]] + [xT[:, c * 256:(c + 1) * 256] for c in range(8)] + [xh[:, 256:512]]
        for kb in range(NKB):
            ch, sub = divmod(kb, 2)
            par = kb % 2
            own = 2 <= kb < 18
            if sub == 0:
                P.dma("pool", lambda e, ch=ch: e.dma_start(
                    out=nxs[ch % 2], in_=chunks[ch].rearrange("(kc k) n -> k kc n", k=128)), writes=[t_nxs[ch % 2]])
            xs_ap = lambda kc, ch=ch, sub=sub: nxs[ch % 2][:, kc, sub * 128:(sub + 1) * 128]
            if own:
                t = kb - 2
                na_inproj(0, xs_ap, 0, [t_nxs[ch % 2], t_wna[0]])
                P.op("act", lambda e, par=par: e.activation(func=AF.Copy, out=nq_tm[par], in_=PS[0]), reads=[t_ps[0]], writes=[t_nqtm[par]])

                def fTq(e, par=par):
                    ins = None
                    for pr in range(4):
                        ins = e.transpose(out=PSB[3][:, pr * 128:(pr + 1) * 128], in_=nq_tm[par][:, pr * 128:(pr + 1) * 128],
                                          identity=ident_b)
                    return ins
                P.op("pe", fTq, reads=[t_nqtm[par], t_ident], writes=[t_ps[3]])
                P.op("dve", lambda e, t=t: e.tensor_copy(out=nqT[:, :, t * 128:(t + 1) * 128],
                                                         in_=PSB[3][:, 0:512].rearrange("p (a b) -> p a b", b=128)),
                     reads=[t_ps[3]], writes=[t_nq[t]])
            na_inproj(1, xs_ap, 1, [t_nxs[ch % 2], t_wna[1]])
            na_inproj(2, xs_ap, 2, [t_nxs[ch % 2], t_wna[2]])
            P.op("act", lambda e, par=par: e.activation(func=AF.Copy, out=nk_tm[par], in_=PS[1]), reads=[t_ps[1]], writes=[t_nktm[par]])

            def fTk(e, par=par):
                ins = None
                for pr in range(4):
                    ins = e.transpose(out=PSB[4][:, pr * 128:(pr + 1) * 128], in_=nk_tm[par][:, pr * 128:(pr + 1) * 128],
                                      identity=ident_b)
                return ins
            P.op("pe", fTk, reads=[t_nktm[par], t_ident], writes=[t_ps[4]])
            P.op("dve", lambda e, kb=kb: e.tensor_copy(out=nkT[:, :, kb * 128:(kb + 1) * 128],
                                                       in_=PSB[4][:, 0:512].rearrange("p (a b) -> p a b", b=128)),
                 reads=[t_ps[4]], writes=[t_nk[kb]])
            P.op("dve", lambda e, kb=kb: e.tensor_copy(out=V4[:, kb, :, 0:64], in_=v3(PS[2])),
                 reads=[t_ps[2]], writes=[t_V[kb]])
        Est = [A.alloc([NA_BLOCKS * 128], F32) for _ in range(2)]; t_Est = P.tiles_n(2, "Est")
        Ebf = [A.alloc([NA_BLOCKS, 128], BF16) for _ in range(2)]; t_Ebf = P.tiles_n(2, "Ebf")
        nes = [A.alloc([768], BF16) for _ in range(2)]; t_nes = P.tiles_n(2, "nes")
        pTt = [A.alloc([768], BF16) for _ in range(2)]; t_pT = P.tiles_n(2, "pT")
        rcp = [A.alloc([2], F32) for _ in range(4)]; t_rcp = P.tiles_n(4, "rcp")
        na_pending = []
        for h in range(8):
            hp2 = h % 2
            pr, hp = divmod(h, 2)
            pl, ph = hp * 64, hp * 64 + 64
            P.dma("sp", lambda e, h=h, hp2=hp2: e.dma_start(out=Est[hp2], in_=natab[h]), writes=[t_Est[hp2]])
            P.op("act", lambda e, hp2=hp2: e.activation(out=Ebf[hp2].rearrange("p a b -> p (a b)"), in_=Est[hp2], func=AF.Exp),
                 reads=[t_Est[hp2]], writes=[t_Ebf[hp2]])
            for i in range(NT):
                kt0, nk, b0 = NA_TYPES[i]
                n = h * NT + i
                par = n % 2
                bX, bY = 2 * par, 2 * par + 1
                bO = 4 + n % 4
                qsl = slice(i * 128, (i + 1) * 128)

                def fS(e, kt0=kt0, nk=nk, bX=bX, bY=bY, pl=pl, ph=ph, pr=pr, qsl=qsl):
                    ins = None
                    for idx in range(nk):
                        kt = kt0 + idx
                        bank = PS[bX] if idx < 4 else PS[bY]
                        ins = e.matmul(bank[:, (idx % 4) * 128:(idx % 4) * 128 + 128], lhsT=nkT[pl:ph, pr, kt * 128:(kt + 1) * 128],
                                       rhs=nqT[pl:ph, pr, qsl], start=True, stop=True)
                    return ins
                P.op("pe", fS, reads=[t_nq[i]] + t_nk[kt0:kt0 + nk], writes=[t_ps[bX], t_ps[bY]])
                P.op("act", lambda e, par=par, bX=bX: e.activation(out=nes[par][:, 0:512], in_=PS[bX], func=AF.Exp, scale=0.125),
                     reads=[t_ps[bX]], writes=[t_nes[par]])
                w2 = (nk - 4) * 128
                P.op("act", lambda e, par=par, bY=bY, w2=w2: e.activation(out=nes[par][:, 512:512 + w2], in_=PS[bY][:, 0:w2],
                                                                        func=AF.Exp, scale=0.125),
                     reads=[t_ps[bY]], writes=[t_nes[par]])
                P.op("pool", lambda e, par=par, nk=nk, b0=b0, hp2=hp2: e.tensor_tensor(
                    out=pTt[par][:, 0:nk * 128], in0=nes[par][:, 0:nk * 128],
                    in1=Ebf[hp2][:, b0:b0 + nk, :].rearrange("p a b -> p (a b)"), op=ALU.mult),
                    reads=[t_nes[par], t_Ebf[hp2]], writes=[t_pT[par]])

                def na_tail(kt0=kt0, nk=nk, par=par, bO=bO, h=h, n=n, i=i):
                    def fO(e):
                        ins = None
                        for idx in range(nk):
                            ins = e.matmul(PS[bO][:, 0:65], lhsT=pTt[par][:, idx * 128:(idx + 1) * 128],
                                           rhs=Vaug[:, kt0 + idx, h * 65:(h + 1) * 65], start=(idx == 0), stop=(idx == nk - 1))
                        return ins
                    P.op("pe", fO, reads=[t_pT[par]] + t_V[kt0:kt0 + nk], writes=[t_ps[bO]])
                    r4 = n % 4
                    P.op("dve", lambda e: e.reciprocal(out=rcp[r4][:, 0:1], in_=PS[bO][:, 64:65]),
                         reads=[t_ps[bO]], writes=[t_rcp[r4]])
                    P.op("dve", lambda e: e.tensor_scalar(
                        out=na_sb[:, i, h * 64:(h + 1) * 64], in0=PS[bO][:, 0:64], scalar1=rcp[r4][:, 0:1], scalar2=None, op0=ALU.mult),
                        reads=[t_ps[bO], t_rcp[r4]], writes=[t_na[i]])
                na_pending.append(na_tail)
                if len(na_pending) > 1:
                    na_pending.pop(0)()
        while na_pending:
            na_pending.pop(0)()
        naf = [A.alloc([512], F32) for _ in range(2)] if "na" in dbg else None
        t_naf = P.tiles_n(2, "naf")
        for i in range(NT):
            tsl = slice(i * 128, (i + 1) * 128)
            P.dma("sp", lambda e, i=i, tsl=tsl: e.dma_start(out=cat[tsl, 512:1024], in_=na_sb[:, i, :]),
                  reads=[t_na[i]], writes=[t_cat[1][i]], sem_tile=P.tile("nast%d" % (i % 4)) if i >= 4 else P.tile("nast%d" % i))
            if "na" in dbg:
                P.op("dve", lambda e, i=i: e.tensor_copy(out=naf[i % 2], in_=na_sb[:, i, :]), reads=[t_na[i]], writes=[t_naf[i % 2]])
                P.dma("sp", lambda e, i=i, tsl=tsl: e.dma_start(out=dbg["na"][tsl, :], in_=naf[i % 2]),
                      reads=[t_naf[i % 2]], sem_tile=t_naf[i % 2])
        P.barrier()
        A.release()
        if stage == "na":
            P.final_wait("sp")
            P.emit_all()
            return nc

        x1T = A.alloc([8, TOK], BF16)
        t_x1T = P.tiles_n(NT, "x1T")
        t_GT = P.tiles_n(NT, "GT"); t_base = P.tiles_n(NT, "base")
        A.mark()
        w_o = A.alloc([8, 1024], BF16); w_pgt = A.alloc([8, 1024], BF16); w_ppt = A.alloc([2, 1024], BF16)
        pTb = A.alloc([2, TOK], BF16)
        w_rt = A.alloc([8, 64], F32)
        g1 = A.alloc([1024], F32); b1 = A.alloc([1024], F32); rb = A.alloc([64], F32)
        t_wl = P.tiles_n(9, "mw")
        P.dma("pool", lambda e: e.dma_start(out=w_o, in_=w_out.rearrange("(kc k) n -> k kc n", k=128)), writes=[t_wl[0]])
        P.dma("pool", lambda e: e.dma_start(out=pTb, in_=pT.rearrange("(kc k) n -> k kc n", k=128)), writes=[t_wl[1]])
        P.dma("pool", lambda e: e.dma_start(out=w_ppt, in_=w_pp.rearrange("(kc k) n -> k kc n", k=128)), writes=[t_wl[2]])
        P.dma("pool", lambda e: e.dma_start(out=w_pgt, in_=w_pg.rearrange("(kc k) n -> k kc n", k=128)), writes=[t_wl[3]])
        P.dma("sp", lambda e: e.dma_start(out=w_rt, in_=w_router.rearrange("(kc k) n -> k kc n", k=128)), writes=[t_wl[4]])
        P.dma("sp", lambda e: e.dma_start(out=g1, in_=ln1_g.partition_broadcast(128)), writes=[t_wl[5]])
        P.dma("sp", lambda e: e.dma_start(out=b1, in_=ln1_b.partition_broadcast(128)), writes=[t_wl[6]])
        P.dma("sp", lambda e: e.dma_start(out=rb, in_=rbias.partition_broadcast(128)), writes=[t_wl[7]])
        catb = [A.alloc([1024], BF16) for _ in range(2)]; t_catb = P.tiles_n(2, "catb")
        catT = [A.alloc([8, 128], BF16) for _ in range(2)]; t_catT = P.tiles_n(2, "catT")
        xrt = [A.alloc([1024], F32) for _ in range(2)]; t_xrt = P.tiles_n(2, "xrt")
        zt = [A.alloc([1024], F32) for _ in range(2)]; t_zt = P.tiles_n(2, "zt")
        x1t = [A.alloc([1024], F32) for _ in range(2)]; t_x1t = P.tiles_n(2, "x1t")
        x1T32 = [A.alloc([8, 128], F32) for _ in range(2)]; t_x1T32 = P.tiles_n(2, "x1T32")
        lst = [A.alloc([32], F32) for _ in range(2)]; t_lst = P.tiles_n(2, "lst")
        rt = [A.alloc([768], F32) for _ in range(2)]; t_rt = P.tiles_n(2, "rt")
        gTs = [A.alloc([128], F32) for _ in range(2)]; t_gTs = P.tiles_n(2, "gTs")
        sgp = [A.alloc([1024], F32) for _ in range(2)]; t_sgp = P.tiles_n(2, "sgp")
        bst = [A.alloc([1024], F32) for _ in range(2)]; t_bst = P.tiles_n(2, "bst")

        def layer_norm(src, t_src, dst, t_dst, st, t_st, gain, bias, t_gb):
            for hf_ in range(2):
                P.op("dve", lambda e, hf_=hf_: e.bn_stats(out=st[:, hf_ * 6:(hf_ + 1) * 6], in_=src[:, hf_ * 512:(hf_ + 1) * 512]),
                     reads=[t_src], writes=[t_st])
            P.op("dve", lambda e: e.bn_aggr(out=st[:, 12:14], in_=st[:, 0:12]), reads=[t_st], writes=[t_st])
            P.op("act", lambda e: e.activation(out=st[:, 14:15], in_=st[:, 13:14], func=AF.Ln, bias=cst[:, 4:5], scale=1.0),
                 reads=[t_st, t_cst], writes=[t_st])
            P.op("act", lambda e: e.activation(out=st[:, 14:15], in_=st[:, 14:15], func=AF.Exp, scale=-0.5),
                 reads=[t_st], writes=[t_st])
            P.op("dve", lambda e: e.tensor_scalar(out=dst, in0=src, scalar1=st[:, 12:13], scalar2=st[:, 14:15],
                                                  op0=ALU.subtract, op1=ALU.mult), reads=[t_src, t_st], writes=[t_dst])
            P.op("pool", lambda e: e.tensor_tensor(out=dst, in0=dst, in1=gain, op=ALU.mult), reads=[t_dst] + t_gb, writes=[t_dst])
            P.op("pool", lambda e: e.tensor_tensor(out=dst, in0=dst, in1=bias, op=ALU.add), reads=[t_dst] + t_gb, writes=[t_dst])

        m_pending = []
        m_pendA2 = []
        m_stB = []
        for t in range(NT):
            par = t % 2
            tsl = slice(t * 128, (t + 1) * 128)
            def stageA(t=t, par=par, tsl=tsl):
                P.dma("pool", lambda e, par=par, tsl=tsl: e.dma_start(out=catb[par], in_=cat[tsl, :]),
                      reads=[t_cat[0][t], t_cat[1][t]], writes=[t_catb[par]])
                P.dma("pool", lambda e, par=par, tsl=tsl: e.dma_start(out=xrt[par], in_=xr[tsl, :]), writes=[t_xrt[par]])

                def fCT(e, par=par):
                    ins = None
                    for c in range(8):
                        ins = e.transpose(out=PSB[0][:, c * 128:(c + 1) * 128], in_=catb[par][:, c * 128:(c + 1) * 128], identity=ident_b)
                    return ins
                P.op("pe", fCT, reads=[t_catb[par], t_ident], writes=[t_ps[0]])
                P.op("dve", lambda e, par=par: e.tensor_copy(out=catT[par].rearrange("p a b -> p (a b)"), in_=PSB[0]),
                     reads=[t_ps[0]], writes=[t_catT[par]])
                for half in range(2):
                    def fM(e, par=par, half=half):
                        ins = None
                        for kc in range(8):
                            ins = e.matmul(PS[1 + half], lhsT=catT[par][:, kc, :], rhs=w_o[:, kc, half * 512:(half + 1) * 512],
                                           start=(kc == 0), stop=(kc == 7))
                        return ins
                    P.op("pe", fM, reads=[t_catT[par], t_wl[0]], writes=[t_ps[1 + half]])
                    P.op("dve", lambda e, par=par, half=half: e.scalar_tensor_tensor(
                        out=zt[par][:, half * 512:(half + 1) * 512], in0=xrt[par][:, half * 512:(half + 1) * 512], scalar=ALPHA,
                        in1=PS[1 + half], op0=ALU.mult, op1=ALU.add), reads=[t_xrt[par], t_ps[1 + half]], writes=[t_zt[par]])

            def stageA2(t=t, par=par, tsl=tsl):
                layer_norm(zt[par], t_zt[par], x1t[par], t_x1t[par], lst[par], t_lst[par], g1, b1, [t_wl[5], t_wl[6]])
                if "x1" in dbg:
                    P.dma("sp", lambda e, par=par, tsl=tsl: e.dma_start(out=dbg["x1"][tsl, :], in_=x1t[par]),
                          reads=[t_x1t[par]], sem_tile=t_x1t[par])
                for half in range(2):
                    def fXT(e, par=par, half=half):
                        ins = None
                        for c in range(4):
                            cc_ = half * 4 + c
                            ins = e.transpose(out=PS[3 + half][:, c * 128:(c + 1) * 128], in_=x1t[par][:, cc_ * 128:(cc_ + 1) * 128],
                                              identity=ident_f)
                        return ins
                    P.op("pe", fXT, reads=[t_x1t[par], t_ident], writes=[t_ps[3 + half]])
                    P.op("dve", lambda e, half=half, tsl=tsl: e.tensor_copy(
                        out=x1T[:, half * 4:half * 4 + 4, tsl], in_=PS[3 + half].rearrange("p (a b) -> p a b", b=128)),
                        reads=[t_ps[3 + half]], writes=[t_x1T[t]])
                    P.op("act", lambda e, half=half, par=par: e.copy(
                        out=x1T32[par].rearrange("p a b -> p (a b)")[:, half * 512:(half + 1) * 512], in_=PS[3 + half]),
                        reads=[t_ps[3 + half]], writes=[t_x1T32[par]])


            def stageB(t=t, par=par, tsl=tsl):
                def fR(e, par=par):
                    ins = None
                    for kc in range(8):
                        ins = e.matmul(PS[5][:, 0:64], lhsT=x1T32[par][:, kc, :], rhs=w_rt[:, kc, :], start=(kc == 0), stop=(kc == 7))
                    return ins
                P.op("pe", fR, reads=[t_x1T32[par], t_wl[4]], writes=[t_ps[5]])
                R = rt[par]
                sc, bi, eq, bi2 = R[:, 0:64], R[:, 64:128], R[:, 128:192], R[:, 192:256]
                m1, m2, gs, t8a, gm, pen = R[:, 256:264], R[:, 264:272], R[:, 272:280], R[:, 280:288], R[:, 288:296], R[:, 296:304]
                msk, t8b, sel, wv, den, gate = R[:, 320:384], R[:, 304:312], R[:, 384:448], R[:, 448:512], R[:, 312:314], R[:, 512:576]
                g3 = lambda ap: ap.rearrange("p (a b) -> p a b", b=8)
                tr_ = [t_rt[par]]
                P.op("act", lambda e, sc=sc: e.activation(out=sc, in_=PS[5][:, 0:64], func=AF.Sigmoid), reads=[t_ps[5]], writes=tr_)
                P.op("dve", lambda e, sc=sc, bi=bi: e.tensor_tensor(out=bi, in0=sc, in1=rb, op=ALU.add), reads=tr_ + [t_wl[7]], writes=tr_)
                P.op("dve", lambda e, bi=bi, m1=m1: e.tensor_reduce(out=m1, in_=g3(bi), axis=AX.X, op=ALU.max), reads=tr_, writes=tr_)
                P.op("dve", lambda e, bi=bi, m1=m1, eq=eq: e.tensor_tensor(out=g3(eq), in0=g3(bi), in1=bc_last(m1, 8), op=ALU.is_equal),
                     reads=tr_, writes=tr_)
                P.op("dve", lambda e, bi=bi, eq=eq, bi2=bi2: e.scalar_tensor_tensor(out=bi2, in0=eq, scalar=-1e9, in1=bi,
                                                                                    op0=ALU.mult, op1=ALU.add), reads=tr_, writes=tr_)
                P.op("dve", lambda e, bi2=bi2, m2=m2: e.tensor_reduce(out=m2, in_=g3(bi2), axis=AX.X, op=ALU.max), reads=tr_, writes=tr_)
                P.op("dve", lambda e, m1=m1, m2=m2, gs=gs: e.tensor_tensor(out=gs, in0=m1, in1=m2, op=ALU.add), reads=tr_, writes=tr_)
                P.op("dve", lambda e, gs=gs, t8a=t8a: e.max(out=t8a, in_=gs), reads=tr_, writes=tr_)
                P.op("dve", lambda e, gs=gs, t8a=t8a, gm=gm: e.tensor_scalar(out=gm, in0=gs, scalar1=t8a[:, 3:4], scalar2=None, op0=ALU.is_ge),
                     reads=tr_, writes=tr_)
                P.op("dve", lambda e, gm=gm, pen=pen: e.tensor_scalar(out=pen, in0=gm, scalar1=-1.0, scalar2=1e9, op0=ALU.add, op1=ALU.mult),
                     reads=tr_, writes=tr_)
                P.op("dve", lambda e, bi=bi, pen=pen, msk=msk: e.tensor_tensor(out=g3(msk), in0=g3(bi), in1=bc_last(pen, 8), op=ALU.add),
                     reads=tr_, writes=tr_)
                P.op("dve", lambda e, msk=msk, t8b=t8b: e.max(out=t8b, in_=msk), reads=tr_, writes=tr_)
                P.op("dve", lambda e, msk=msk, t8b=t8b, sel=sel: e.tensor_scalar(out=sel, in0=msk, scalar1=t8b[:, 7:8], scalar2=None,
                                                                                 op0=ALU.is_ge), reads=tr_, writes=tr_)
                P.op("dve", lambda e, sc=sc, sel=sel, wv=wv: e.tensor_tensor(out=wv, in0=sc, in1=sel, op=ALU.mult), reads=tr_, writes=tr_)
                P.op("dve", lambda e, wv=wv, den=den: e.tensor_reduce(out=den[:, 0:1], in_=wv, axis=AX.X, op=ALU.add), reads=tr_, writes=tr_)
                P.op("dve", lambda e, den=den: e.reciprocal(out=den[:, 1:2], in_=den[:, 0:1]), reads=tr_, writes=tr_)
                P.op("dve", lambda e, wv=wv, den=den, gate=gate: e.tensor_scalar(out=gate, in0=wv, scalar1=den[:, 1:2], scalar2=2.5,
                                                                                 op0=ALU.mult, op1=ALU.mult), reads=tr_, writes=tr_)
                if "gate" in dbg:
                    P.dma("sp", lambda e, gate=gate, tsl=tsl: e.dma_start(out=dbg["gate"][tsl, :], in_=gate), reads=tr_, sem_tile=t_rt[par])
                for half in range(2):
                    hs = slice(half * 512, (half + 1) * 512)

                    def fPG(e, half=half, tsl=tsl, hs=hs):
                        ins = None
                        for kc in range(8):
                            ins = e.matmul(PS[6 + half], lhsT=x1T[:, kc, tsl], rhs=w_pgt[:, kc, hs], start=(kc == 0), stop=(kc == 7))
                        return ins
                    P.op("pe", fPG, reads=[t_x1T[t], t_wl[3]], writes=[t_ps[6 + half]])
                    P.op("act", lambda e, half=half, par=par, hs=hs: e.activation(out=sgp[par][:, hs], in_=PS[6 + half], func=AF.Sigmoid),
                         reads=[t_ps[6 + half]], writes=[t_sgp[par]])

                    def fPP(e, half=half, tsl=tsl, hs=hs):
                        ins = None
                        for kc in range(2):
                            ins = e.matmul(PS[6 + half], lhsT=pTb[:, kc, tsl], rhs=w_ppt[:, kc, hs], start=(kc == 0), stop=(kc == 1))
                        return ins
                    P.op("pe", fPP, reads=[t_wl[1], t_wl[2]], writes=[t_ps[6 + half]])
                    P.op("dve", lambda e, half=half, par=par, hs=hs: e.tensor_tensor(out=bst[par][:, hs], in0=PS[6 + half], in1=sgp[par][:, hs],
                                                                                     op=ALU.mult),
                         reads=[t_ps[6 + half], t_sgp[par]], writes=[t_bst[par]])
                P.op("dve", lambda e, par=par: e.scalar_tensor_tensor(out=bst[par], in0=x1t[par], scalar=ALPHA, in1=bst[par],
                                                                      op0=ALU.mult, op1=ALU.add),
                     reads=[t_x1t[par], t_bst[par]], writes=[t_bst[par]])
                P.op("pe", lambda e, gate=gate: e.transpose(out=PS[5][0:64, 128:256], in_=gate, identity=ident_f),
                     reads=tr_ + [t_ident], writes=[t_ps[5]])
                P.op("act", lambda e, par=par: e.activation(func=AF.Copy, out=gTs[par][0:64, :], in_=PS[5][0:64, 128:256]), reads=[t_ps[5]], writes=[t_gTs[par]])
                P.dma("sp", lambda e, par=par, tsl=tsl: e.dma_start(out=GT[:, tsl], in_=gTs[par][0:64, :]),
                      reads=[t_gTs[par]], writes=[t_GT[t]], sem_tile=t_gTs[par])
                P.dma("sp", lambda e, par=par, tsl=tsl: e.dma_start(out=base[tsl, :], in_=bst[par]),
                      reads=[t_bst[par]], writes=[t_base[t]], sem_tile=t_bst[par])

            stageA()
            m_pendA2.append(stageA2)
            if len(m_pendA2) > 1:
                m_pendA2.pop(0)()
                m_pending.append(m_stB.pop(0))
            m_stB.append(stageB)
            if len(m_pending) > 1:
                m_pending.pop(0)()
        while m_pendA2:
            m_pendA2.pop(0)()
            m_pending.append(m_stB.pop(0))
            if len(m_pending) > 1:
                m_pending.pop(0)()
        while m_pending:
            m_pending.pop(0)()
        P.barrier()
        A.release()
        if stage == "x1":
            P.final_wait("sp")
            P.emit_all()
            return nc

        y_acc = A.alloc([NT, 1024], F32); t_y = P.tiles_n(NT, "yacc")
        A.mark()
        m_Wgu = [A.alloc([8, 512], BF16) for _ in range(4)]; m_Wdn = [A.alloc([2, 1024], BF16) for _ in range(4)]
        t_mWgu = P.tiles_n(4, "mWgu"); t_mWdn = P.tiles_n(4, "mWdn")
        m_gbc = [A.alloc([TOK], F32) for _ in range(4)]; t_mgbc = P.tiles_n(4, "mgbc")
        m_act = A.alloc([4, TOK], BF16)
        t_mact = [P.tiles_n(4, "mact%d_" % k_) for k_ in range(4)]
        m_sg = [A.alloc([512], BF16) for _ in range(2)]; m_tt = [A.alloc([512], BF16) for _ in range(2)]
        t_msg = P.tiles_n(2, "msg"); t_mtt = P.tiles_n(2, "mtt")

        def m_load(ex):
            s_ = ex % 4
            src_gu = w_egu[ex] if ex < NEXP else w_sgu
            src_dn = w_edn[ex] if ex < NEXP else w_sdn
            P.dma("pool", lambda eng, s_=s_, src_gu=src_gu: eng.dma_start(
                out=m_Wgu[s_], in_=src_gu.rearrange("(kc k) n -> k kc n", k=128)), writes=[t_mWgu[s_]])
            P.dma("pool", lambda eng, s_=s_, src_dn=src_dn: eng.dma_start(
                out=m_Wdn[s_], in_=src_dn.rearrange("(kc k) n -> k kc n", k=128)), writes=[t_mWdn[s_]])
            if ex < NEXP:
                P.dma("sp", lambda eng, ex=ex: eng.dma_start(out=m_gbc[ex % 4], in_=GT[ex:ex + 1, :].partition_broadcast(128)),
                      reads=t_GT, writes=[t_mgbc[ex % 4]])

        m_groups = [(2 * g_, 2 * g_ + 1) for g_ in range(NEXP // 2)] + [(NEXP,)]
        for ex in m_groups[0]:
            m_load(ex)
        m_cnt = 0
        for gi, grp in enumerate(m_groups):
            if gi + 1 < len(m_groups):
                for ex in m_groups[gi + 1]:
                    m_load(ex)
            for eg, ex in enumerate(grp):
                s_ = ex % 4
                for tg in range(4):
                    tgs = slice(tg * 512, (tg + 1) * 512)
                    for j in range(2):
                        q_ = m_cnt % 2
                        m_cnt += 1
                        bg, bu = 2 * q_, 2 * q_ + 1

                        def fUp(eng, s_=s_, j=j, tgs=tgs, bg=bg, bu=bu):
                            ins = None
                            for kc in range(8):
                                eng.matmul(PS[bg], lhsT=m_Wgu[s_][:, kc, j * 128:(j + 1) * 128], rhs=x1T[:, kc, tgs],
                                           start=(kc == 0), stop=(kc == 7))
                            for kc in range(8):
                                ins = eng.matmul(PS[bu], lhsT=m_Wgu[s_][:, kc, 256 + j * 128:256 + (j + 1) * 128], rhs=x1T[:, kc, tgs],
                                                 start=(kc == 0), stop=(kc == 7))
                            return ins
                        P.op("pe", fUp, reads=[t_mWgu[s_]] + t_x1T[4 * tg:4 * tg + 4], writes=[t_ps[bg], t_ps[bu]])
                        P.op("act", lambda eng, q_=q_, bg=bg: eng.activation(out=m_sg[q_], in_=PS[bg], func=AF.Silu),
                             reads=[t_ps[bg]], writes=[t_msg[q_]])
                        if ex < NEXP:
                            P.op("dve", lambda eng, q_=q_, bu=bu, ex=ex, tgs=tgs: eng.tensor_tensor(
                                out=m_tt[q_], in0=PS[bu], in1=m_gbc[ex % 4][:, tgs], op=ALU.mult),
                                reads=[t_ps[bu], t_mgbc[ex % 4]], writes=[t_mtt[q_]])
                        else:
                            P.op("dve", lambda eng, q_=q_, bu=bu: eng.tensor_copy(out=m_tt[q_], in_=PS[bu]),
                                 reads=[t_ps[bu]], writes=[t_mtt[q_]])
                        P.op("pool", lambda eng, q_=q_, eg=eg, j=j, tgs=tgs: eng.tensor_tensor(
                            out=m_act[:, eg * 2 + j, tgs], in0=m_sg[q_], in1=m_tt[q_], op=ALU.mult),
                            reads=[t_msg[q_], t_mtt[q_]], writes=[t_mact[eg * 2 + j][tg]])
            for t in range(NT):
                yq = t % 2
                tsl = slice(t * 128, (t + 1) * 128)
                for half in range(2):
                    by = 4 + 2 * yq + half
                    hs = slice(half * 512, (half + 1) * 512)

                    def fDn(eng, grp=grp, tsl=tsl, by=by, hs=hs):
                        ins = None
                        n_ = len(grp) * 2
                        k_ = 0
                        for eg, ex in enumerate(grp):
                            for j in range(2):
                                ins = eng.matmul(PS[by], lhsT=m_act[:, eg * 2 + j, tsl], rhs=m_Wdn[ex % 4][:, j, hs],
                                                 start=(k_ == 0), stop=(k_ == n_ - 1))
                                k_ += 1
                        return ins
                    rd_ = [t_mact[eg * 2 + j][t // 4] for eg in range(len(grp)) for j in range(2)] + [t_mWdn[ex % 4] for ex in grp]
                    P.op("pe", fDn, reads=rd_, writes=[t_ps[by]])
                    if gi == 0:
                        P.op("dve", lambda eng, t=t, hs=hs, by=by: eng.tensor_copy(out=y_acc[:, t, hs], in_=PS[by]),
                             reads=[t_ps[by]], writes=[t_y[t]])
                    else:
                        P.op("dve", lambda eng, t=t, hs=hs, by=by: eng.tensor_tensor(out=y_acc[:, t, hs], in0=PS[by], in1=y_acc[:, t, hs],
                                                                                     op=ALU.add),
                             reads=[t_ps[by], t_y[t]], writes=[t_y[t]])
        P.barrier()
        A.release()
        g2 = A.alloc([1024], F32); b2 = A.alloc([1024], F32)
        t_f = P.tiles_n(2, "fgb")
        P.dma("sp", lambda eng: eng.dma_start(out=g2, in_=ln2_g.partition_broadcast(128)), writes=[t_f[0]])
        P.dma("sp", lambda eng: eng.dma_start(out=b2, in_=ln2_b.partition_broadcast(128)), writes=[t_f[1]])
        m_bt = [A.alloc([1024], F32) for _ in range(4)]; m_z2 = [A.alloc([1024], F32) for _ in range(4)]
        m_o = [A.alloc([1024], F32) for _ in range(4)]; m_st = [A.alloc([32], F32) for _ in range(4)]
        t_mbt = P.tiles_n(4, "mbt"); t_mz2 = P.tiles_n(4, "mz2"); t_mo = P.tiles_n(4, "mo"); t_mst = P.tiles_n(4, "mst")
        fin_pending = []
        for t in range(NT):
            par = t % 4
            tsl = slice(t * 128, (t + 1) * 128)
            P.dma("pool", lambda eng, par=par, tsl=tsl: eng.dma_start(out=m_bt[par], in_=base[tsl, :]),
                  reads=[t_base[t]], writes=[t_mbt[par]])
            P.op("pool", lambda eng, par=par, t=t: eng.tensor_tensor(out=m_z2[par], in0=y_acc[:, t, :], in1=m_bt[par], op=ALU.add),
                 reads=[t_y[t], t_mbt[par]], writes=[t_mz2[par]])
            def fin_tail(par=par, tsl=tsl):
                layer_norm(m_z2[par], t_mz2[par], m_o[par], t_mo[par], m_st[par], t_mst[par], g2, b2, t_f)
                P.dma("sp", lambda eng: eng.dma_start(out=out[tsl, :], in_=m_o[par]),
                      reads=[t_mo[par]], sem_tile=t_mo[par])
            fin_pending.append(fin_tail)
            if len(fin_pending) > 2:
                fin_pending.pop(0)()
        while fin_pending:
            fin_pending.pop(0)()
        P.final_wait("sp")
        P.emit_all()
        return nc


def _na_table(rpb, hf):
    base = 32 * hf
    tab = np.full((8, 128, NA_BLOCKS * 128), NEG, np.float32)
    kk = np.arange(128)
    qq = np.arange(128)
    for i in (0, 1, 2, 14, 15):
        kt0, nk, b0 = NA_TYPES[i]
        r = base + 2 * i + qq // 64
        c = qq % 64
        rs = np.clip(r - 4, 0, 56)
        cs = np.clip(c - 8, 0, 48)
        for idx in range(nk):
            kt = kt0 + idx
            kr = base - 4 + 2 * kt + kk // 64
            kc = kk % 64
            vr = (kr[:, None] >= rs[None, :]) & (kr[:, None] <= rs[None, :] + 7) & (kr[:, None] >= 0) & (kr[:, None] <= 63)
            vc = (kc[:, None] >= cs[None, :]) & (kc[:, None] <= cs[None, :] + 15)
            valid = vr & vc
            dr = np.clip(kr[:, None] - r[None, :] + 7, 0, 14)
            dc = np.clip(kc[:, None] - c[None, :] + 15, 0, 30)
            vals = rpb[:, dr, dc]
            blk = np.where(valid[None], vals, np.float32(NEG))
            tab[:, :, (b0 + idx) * 128:(b0 + idx + 1) * 128] = blk
    return tab


def _const_tables(hf):
    half = 32
    inv = (10000.0 ** (-np.arange(half, dtype=np.float32) / half)).astype(np.float32)
    j = np.arange(128)
    t = np.arange(NT)

    def rot(pos0):
        pos = (pos0 + t[None, :] * 128 + j[:, None]).astype(np.float32)
        ang = pos[:, :, None] * inv[None, None, :]
        cos = np.cos(ang).astype(np.float32)
        sin = np.sin(ang).astype(np.float32)
        cc = np.concatenate([cos, cos], -1).reshape(128, NT * 64)
        ss = np.concatenate([-sin, sin], -1).reshape(128, NT * 64)
        return np.ascontiguousarray(cc), np.ascontiguousarray(ss)
    cc_own, ss_own = rot(hf * TOK)
    cc_oth, ss_oth = rot((1 - hf) * TOK)
    m = t[None, :] * 128 + j[:, None]
    dist = (2047 - m if hf == 1 else m).astype(np.float32)
    ii = np.arange(128, dtype=np.float32)
    epos = np.maximum(ii[None, :] - ii[:, None], 0).astype(np.float32)
    eneg = np.maximum(ii[:, None] - ii[None, :], 0).astype(np.float32)
    iota1 = np.broadcast_to(ii[None, :] + 1, (128, 128)).astype(np.float32).copy()
    iota2 = np.broadcast_to(128 - ii[None, :], (128, 128)).astype(np.float32).copy()
    return dict(cc_own=cc_own, ss_own=ss_own, cc_oth=cc_oth, ss_oth=ss_oth, dist=np.ascontiguousarray(dist),
                epos=epos, eneg=eneg, iota1=iota1, iota2=iota2,
                c127=(127 - ii).reshape(128, 1).astype(np.float32), cj=ii.reshape(128, 1).copy(),
                flag=np.full((1, 1), float(hf), np.float32))


def make_in_maps(inputs, cores=range(NCORES)):
    f = lambda a: np.ascontiguousarray(np.asarray(a, dtype=np.float32))
    x = f(inputs["x"]); p = f(inputs["p"])[0]
    shared = dict(
        w_in=f(inputs["w_in"][0]), w_out=f(inputs["w_out"][0]), w_router=f(inputs["w_router"][0]),
        w_egu=f(inputs["w_expert_gu"][0]), w_edn=f(inputs["w_expert_down"][0]),
        w_sgu=f(inputs["w_shared_gu"][0]), w_sdn=f(inputs["w_shared_down"][0]),
        w_pp=f(inputs["w_ple_proj"][0]), w_pg=f(inputs["w_ple_gate"][0]),
        dec_f=f(inputs["ret_decay_fwd"]).reshape(1, 8), dec_b=f(inputs["ret_decay_bwd"]).reshape(1, 8),
        decfT=f(f(inputs["ret_decay_fwd"]).reshape(4, 2).T), decbT=f(f(inputs["ret_decay_bwd"]).reshape(4, 2).T),
        gn_gain=f(inputs["ret_gn_gain"]).reshape(1, 512),
        ln1_g=f(inputs["ln1_gain"]).reshape(1, D), ln1_b=f(inputs["ln1_bias"]).reshape(1, D),
        ln2_g=f(inputs["ln2_gain"]).reshape(1, D), ln2_b=f(inputs["ln2_bias"]).reshape(1, D),
        rbias=f(inputs["router_bias"]).reshape(1, NEXP))
    rpb = f(inputs["na_rpb"][0])
    per_hf = {}
    for hf in (0, 1):
        d = _const_tables(hf)
        d["natab"] = _na_table(rpb, hf)
        per_hf[hf] = d
    maps = []
    for c in cores:
        b, hf = divmod(c, 2)
        own = x[b, hf * TOK:(hf + 1) * TOK]
        oth = x[b, (1 - hf) * TOK:(2 - hf) * TOK]
        xh = np.zeros((512, D), np.float32)
        if hf == 0:
            xh[256:512] = oth[0:256]
        else:
            xh[0:256] = oth[TOK - 256:TOK]
        m = dict(shared)
        m.update(per_hf[hf])
        m.update(xT=f(own.T), xo=f(oth.T), xh=f(xh.T), xr=f(own), pT=f(p[b, hf * TOK:(hf + 1) * TOK].T))
        maps.append(m)
    return maps


_NC_CACHE = {}


def kernel(**inputs):
    if "full" not in _NC_CACHE:
        _NC_CACHE["full"] = build_program("full")
    nc = _NC_CACHE["full"]
    maps = make_in_maps(inputs)
    res = run_bass_kernel_spmd(nc, maps, core_ids=list(range(NCORES)))
    outp = np.empty((4, S, D), np.float32)
    for c in range(NCORES):
        b, hf = divmod(c, 2)
        outp[b, hf * TOK:(hf + 1) * TOK] = res.results[c]["out"]
    return outp
```
